# Optimizing a Trainium2 kernel written in Bass

```python
import math
import jax
import jax.numpy as jnp
from jax import lax
import numpy as np

D_MODEL = 1024
BATCH = 16
SEQ = 2048
DEPTH = 2

HEAD_DIM = 64
BLOCK = 128
GMLP_GROUPS = 8
GMLP_CHUNK = 128
GMLP_WIDTH = GMLP_GROUPS * HEAD_DIM
SWA_Q_HEADS = 8
SWA_KV_HEADS = 2
SWA_WINDOW = 128
FOX_HEADS = 8
CONV_CH = 512
CONV_TAPS = 31
N_BRANCH = 4
BRANCH_WIDTH = 512
ROPE_THETA = 10000.0
D_FF = 3584
N_EXPERTS = 8
TOP_K = 2
N_DENSE = (DEPTH + 1) // 2
N_MOE = DEPTH // 2
EPS = 1e-6
NEG_INF = -1e30
IN_SPLIT = (GMLP_WIDTH, GMLP_WIDTH,
            SWA_Q_HEADS * HEAD_DIM, SWA_KV_HEADS * HEAD_DIM, SWA_KV_HEADS * HEAD_DIM,
            FOX_HEADS * HEAD_DIM, FOX_HEADS * HEAD_DIM, FOX_HEADS * HEAD_DIM, FOX_HEADS,
            CONV_CH, CONV_CH, N_BRANCH * D_MODEL)
N_IN = sum(IN_SPLIT)

kernel_name = 'hybrid_gated_gmlp_swa_fox_conv_moe'


def _rms_norm(x, g):
    xf = x.astype(jnp.float32)
    y = xf * lax.rsqrt(jnp.mean(xf * xf, axis=-1, keepdims=True) + EPS)
    return (y * g.astype(jnp.float32)).astype(x.dtype)


def _layer_norm(x, g, b):
    xf = x.astype(jnp.float32)
    xc = xf - jnp.mean(xf, axis=-1, keepdims=True)
    y = xc * lax.rsqrt(jnp.mean(xc * xc, axis=-1, keepdims=True) + EPS)
    return (y * g.astype(jnp.float32) + b.astype(jnp.float32)).astype(x.dtype)


def _rope_tables(positions):
    inv_freq = jnp.exp(-math.log(ROPE_THETA) * jnp.arange(0, HEAD_DIM, 2, dtype=jnp.float32) / HEAD_DIM)
    ang = positions.astype(jnp.float32)[..., None] * inv_freq
    return jnp.cos(ang)[:, :, None, :], jnp.sin(ang)[:, :, None, :]


def _apply_rope(t, cos, sin):
    tf = t.astype(jnp.float32)
    t1, t2 = tf[..., :HEAD_DIM // 2], tf[..., HEAD_DIM // 2:]
    return jnp.concatenate([t1 * cos - t2 * sin, t2 * cos + t1 * sin], axis=-1).astype(t.dtype)


def _to_blocks(t):
    b, s = t.shape[0], t.shape[1]
    t = t.reshape((b, s // BLOCK, BLOCK) + t.shape[2:])
    return jnp.moveaxis(t, 1, 0)


def _from_blocks(t):
    t = jnp.moveaxis(t, 0, 1)
    return t.reshape((t.shape[0], t.shape[1] * t.shape[2]) + t.shape[3:])


def _gmlp_mixer(zu, zv, ln_g, ln_b, ws, bs):
    b, s, _ = zu.shape
    u = jax.nn.gelu(zu)
    v = _layer_norm(jax.nn.gelu(zv), ln_g, ln_b)
    v = v.reshape(b, s // GMLP_CHUNK, GMLP_CHUNK, GMLP_GROUPS, HEAD_DIM)
    causal = jnp.tril(jnp.ones((GMLP_CHUNK, GMLP_CHUNK), dtype=bool))
    w_s = jnp.where(causal, ws, 0.0).astype(v.dtype)
    mixed = jnp.einsum('gts,bnsgc->bntgc', w_s, v) + bs.T.astype(v.dtype)[None, None, :, :, None]
    return u * mixed.reshape(b, s, GMLP_WIDTH)


def _swa_mixer(q, k, v, sinks):
    b, s = q.shape[0], q.shape[1]
    nb = s // BLOCK
    groups = SWA_Q_HEADS // SWA_KV_HEADS
    qb = _to_blocks(q.reshape(b, s, SWA_KV_HEADS, groups, HEAD_DIM))
    kb, vb = _to_blocks(k), _to_blocks(v)
    shift = lambda t: jnp.concatenate([jnp.zeros_like(t[:1]), t[:-1]], axis=0)
    kk = jnp.concatenate([shift(kb), kb], axis=2)
    vv = jnp.concatenate([shift(vb), vb], axis=2)
    qi = jnp.arange(BLOCK)[:, None]
    kj = jnp.arange(2 * BLOCK)[None, :]
    band = (kj > qi + BLOCK - SWA_WINDOW) & (kj <= qi + BLOCK)
    sink = sinks.astype(jnp.float32).reshape(1, SWA_KV_HEADS, groups, 1, 1)
    scale = HEAD_DIM ** -0.5

    def block_attn(args):
        qblk, kblk, vblk, idx = args
        sc = jnp.einsum('bqhgd,bkhd->bhgqk', qblk, kblk, preferred_element_type=jnp.float32) * scale
        valid = band & (idx * BLOCK - BLOCK + kj >= 0)
        sc = jnp.where(valid, sc, NEG_INF)
        m = jnp.maximum(jnp.max(sc, axis=-1, keepdims=True), sink)
        p = jnp.exp(sc - m)
        p = p / (jnp.sum(p, axis=-1, keepdims=True) + jnp.exp(sink - m))
        return jnp.einsum('bhgqk,bkhd->bqhgd', p.astype(vblk.dtype), vblk)

    out = lax.map(block_attn, (qb, kk, vv, jnp.arange(nb)))
    return _from_blocks(out).reshape(b, s, SWA_Q_HEADS * HEAD_DIM)


def _fox_mixer(q, k, v, f_logit):
    b, s = q.shape[0], q.shape[1]
    nb = s // BLOCK
    cum = jnp.cumsum(jax.nn.log_sigmoid(f_logit.astype(jnp.float32)), axis=1)
    cum_k = jnp.transpose(cum, (0, 2, 1))[:, :, None, :]
    kpos = jnp.arange(s)
    scale = HEAD_DIM ** -0.5

    def block_attn(args):
        qblk, cq, idx = args
        sc = jnp.einsum('bqhd,bkhd->bhqk', qblk, k, preferred_element_type=jnp.float32) * scale
        sc = sc + jnp.transpose(cq, (0, 2, 1))[..., None] - cum_k
        qpos = idx * BLOCK + jnp.arange(BLOCK)
        sc = jnp.where(kpos[None, :] <= qpos[:, None], sc, NEG_INF)
        p = jax.nn.softmax(sc, axis=-1)
        return jnp.einsum('bhqk,bkhd->bqhd', p.astype(v.dtype), v)

    out = lax.map(block_attn, (_to_blocks(q), _to_blocks(cum), jnp.arange(nb)))
    return _from_blocks(out).reshape(b, s, FOX_HEADS * HEAD_DIM)


def _conv_mixer(za, zg, w, bias, ln_g, ln_b):
    y = za * jax.nn.sigmoid(zg)
    y = lax.conv_general_dilated(y, w[:, None, :].astype(y.dtype), window_strides=(1,),
                                 padding=((CONV_TAPS - 1, 0),),
                                 dimension_numbers=('NWC', 'WIO', 'NWC'),
                                 feature_group_count=CONV_CH)
    y = y + bias.astype(y.dtype)
    return jax.nn.silu(_layer_norm(y, ln_g, ln_b))


def _mixer_layer(h, cos, sin, w_in, gmlp_ln_g, gmlp_ln_b, gmlp_ws, gmlp_bs, swa_sink, fox_bf,
                 conv_w, conv_b, conv_ln_g, conv_ln_b, w_branch, w_out):
    b, s, _ = h.shape
    z = jnp.einsum('bsd,dn->bsn', h, w_in)
    parts, off = [], 0
    for n in IN_SPLIT:
        parts.append(z[..., off:off + n])
        off += n
    zu, zv, sq, sk, sv, fq, fk, fv, ff, ca, cg, zgate = parts
    heads = lambda t, n: t.reshape(b, s, n, HEAD_DIM)
    o_a = _gmlp_mixer(zu, zv, gmlp_ln_g, gmlp_ln_b, gmlp_ws, gmlp_bs)
    o_b = _swa_mixer(_apply_rope(heads(sq, SWA_Q_HEADS), cos, sin),
                     _apply_rope(heads(sk, SWA_KV_HEADS), cos, sin),
                     heads(sv, SWA_KV_HEADS), swa_sink)
    o_c = _fox_mixer(heads(fq, FOX_HEADS), heads(fk, FOX_HEADS), heads(fv, FOX_HEADS),
                     ff + fox_bf.astype(ff.dtype))
    o_d = _conv_mixer(ca, cg, conv_w, conv_b, conv_ln_g, conv_ln_b)
    branches = jnp.stack([o_a, o_b, o_c, o_d], axis=2)
    proj = jnp.einsum('bsnc,ncd->bsnd', branches, w_branch)
    gates = jax.nn.sigmoid(zgate.reshape(b, s, N_BRANCH, D_MODEL))
    merged = jnp.sum(gates * proj, axis=2)
    return jnp.einsum('bsd,de->bse', merged, w_out)


def _swiglu(h, wg, wu, wd):
    return (jax.nn.silu(h @ wg) * (h @ wu)) @ wd


def _moe(h, router_w, wg, wu, wd):
    b, s, d = h.shape
    t = h.reshape(b * s, d)
    logits = jnp.einsum('td,de->te', t, router_w, preferred_element_type=jnp.float32)
    vals, idx = lax.top_k(logits, TOP_K)
    w = jax.nn.softmax(vals, axis=-1)
    gate = jnp.sum(jax.nn.one_hot(idx, N_EXPERTS, dtype=jnp.float32) * w[..., None], axis=1)
    y = jnp.zeros_like(t)
    for e in range(N_EXPERTS):
        y = y + gate[:, e:e + 1].astype(t.dtype) * _swiglu(t, wg[e], wu[e], wd[e])
    return y.reshape(b, s, d)


def setup_inputs(seed: int = 0) -> dict:
    key = jax.random.key(seed)
    ks = jax.random.split(key, 26)
    f32 = jnp.float32
    nrm = lambda k, shape, sc: jax.random.normal(k, shape, f32) * sc
    offset = jax.random.randint(ks[1], (BATCH, 1), 0, 4096, dtype=jnp.int32)
    positions = (offset + jnp.arange(SEQ, dtype=jnp.int32)[None, :]).astype(jnp.int32)
    return {
        'x': nrm(ks[0], (BATCH, SEQ, D_MODEL), 1.0),
        'positions': positions,
        'norm_mix_g': 1.0 + nrm(ks[2], (DEPTH, D_MODEL), 0.05),
        'w_in': nrm(ks[3], (DEPTH, D_MODEL, N_IN), D_MODEL ** -0.5),
        'gmlp_ln_g': 1.0 + nrm(ks[4], (DEPTH, GMLP_WIDTH), 0.05),
        'gmlp_ln_b': nrm(ks[5], (DEPTH, GMLP_WIDTH), 0.05),
        'gmlp_ws': nrm(ks[6], (DEPTH, GMLP_GROUPS, GMLP_CHUNK, GMLP_CHUNK), GMLP_CHUNK ** -0.5),
        'gmlp_bs': 1.0 + nrm(ks[7], (DEPTH, GMLP_GROUPS, GMLP_CHUNK), 0.05),
        'swa_sink': nrm(ks[8], (DEPTH, SWA_Q_HEADS), 0.5),
        'fox_bf': 2.0 + nrm(ks[9], (DEPTH, FOX_HEADS), 0.5),
        'conv_w': nrm(ks[10], (DEPTH, CONV_TAPS, CONV_CH), CONV_TAPS ** -0.5),
        'conv_b': nrm(ks[11], (DEPTH, CONV_CH), 0.02),
        'conv_ln_g': 1.0 + nrm(ks[12], (DEPTH, CONV_CH), 0.05),
        'conv_ln_b': nrm(ks[13], (DEPTH, CONV_CH), 0.05),
        'w_branch': nrm(ks[14], (DEPTH, N_BRANCH, BRANCH_WIDTH, D_MODEL), BRANCH_WIDTH ** -0.5),
        'w_out': nrm(ks[15], (DEPTH, D_MODEL, D_MODEL), D_MODEL ** -0.5),
        'norm_ffn_g': 1.0 + nrm(ks[16], (DEPTH, D_MODEL), 0.05),
        'ffn_w_gate': nrm(ks[17], (N_DENSE, D_MODEL, D_FF), D_MODEL ** -0.5),
        'ffn_w_up': nrm(ks[18], (N_DENSE, D_MODEL, D_FF), D_MODEL ** -0.5),
        'ffn_w_down': nrm(ks[19], (N_DENSE, D_FF, D_MODEL), D_FF ** -0.5),
        'router_w': nrm(ks[20], (N_MOE, D_MODEL, N_EXPERTS), D_MODEL ** -0.5),
        'exp_w_gate': nrm(ks[21], (N_MOE, N_EXPERTS, D_MODEL, D_FF), D_MODEL ** -0.5),
        'exp_w_up': nrm(ks[22], (N_MOE, N_EXPERTS, D_MODEL, D_FF), D_MODEL ** -0.5),
        'exp_w_down': nrm(ks[23], (N_MOE, N_EXPERTS, D_FF, D_MODEL), D_FF ** -0.5),
        'norm_final_g': 1.0 + nrm(ks[24], (D_MODEL,), 0.05),
    }


def reference(x, positions, norm_mix_g, w_in, gmlp_ln_g, gmlp_ln_b, gmlp_ws, gmlp_bs, swa_sink, fox_bf,
              conv_w, conv_b, conv_ln_g, conv_ln_b, w_branch, w_out, norm_ffn_g,
              ffn_w_gate, ffn_w_up, ffn_w_down, router_w, exp_w_gate, exp_w_up, exp_w_down,
              norm_final_g):
    cos, sin = _rope_tables(positions)
    for l in range(DEPTH):
        h = _rms_norm(x, norm_mix_g[l])
        x = x + _mixer_layer(h, cos, sin, w_in[l], gmlp_ln_g[l], gmlp_ln_b[l], gmlp_ws[l], gmlp_bs[l],
                             swa_sink[l], fox_bf[l], conv_w[l], conv_b[l], conv_ln_g[l], conv_ln_b[l],
                             w_branch[l], w_out[l])
        h = _rms_norm(x, norm_ffn_g[l])
        j = l // 2
        if l % 2 == 0:
            x = x + _swiglu(h, ffn_w_gate[j], ffn_w_up[j], ffn_w_down[j])
        else:
            x = x + _moe(h, router_w[j], exp_w_gate[j], exp_w_up[j], exp_w_down[j])
    return _rms_norm(x, norm_final_g)
```

```python
import math
import numpy as np
from contextlib import ExitStack
import concourse.bass as bass
import concourse.mybir as mybir
from concourse.bass_utils import run_bass_kernel_spmd

F32 = mybir.dt.float32
BF16 = mybir.dt.bfloat16
I32 = mybir.dt.int32
AF = mybir.ActivationFunctionType
ALU = mybir.AluOpType

CE = ('pe', 'act', 'dve', 'pool')
NSLOT = 8


class Res:
    __slots__ = ('w', 'rd', 'psum', 'extra')

    def __init__(self, psum=False):
        self.w = None
        self.rd = []
        self.psum = psum
        self.extra = []


class Tok:
    __slots__ = ('key', 'val', 'clock')

    def __init__(self, key, val, clock):
        self.key = key
        self.val = val
        self.clock = clock


class FW:
    def __init__(self, nc, es):
        self.nc = nc
        self.eng = ('pe', 'act', 'dve', 'pool', 'sp')
        self.sem = {}
        self.cnt = {}
        for e in CE:
            self.sem[e] = es.enter_context(nc.semaphore('c_' + e))
            self.cnt[e] = 0
        self.dq = ('sp', 'act', 'pool')
        self.slot_i = {q: 0 for q in self.dq}
        self.nslot = {'sp': NSLOT, 'act': NSLOT, 'pool': 2 * NSLOT}
        for q in self.dq:
            for s in range(self.nslot[q]):
                k = 'd_%s_%d' % (q, s)
                self.sem[k] = es.enter_context(nc.semaphore(k))
                self.cnt[k] = 0
        self.seen = {e: {} for e in self.eng}
        self.prog = {e: [] for e in self.eng}
        self.all_res = []
        self.pes = None
        self.n_inst = 0
        self.n_wait = 0
        self.uid = 0
        self.cond = None

    def res(self, psum=False):
        r = Res(psum)
        self.all_res.append(r)
        return r

    def begin(self):
        self.pes = ExitStack()
        self.all_res = []

    def sb(self, name, shape, dt):
        self.uid += 1
        return self.pes.enter_context(self.nc.sbuf_tensor("%s_%d" % (name, self.uid), list(shape), dt))

    def ps(self, name, shape, dt=F32):
        self.uid += 1
        return self.pes.enter_context(self.nc.psum_tensor("%s_%d" % (name, self.uid), list(shape), dt))

    def _deps(self, e, reads, writes):
        deps = []
        for r in reads:
            if r.w is not None:
                deps.append(r.w)
            if r.psum:
                deps.extend(t for t in r.rd if t.key != e)
            deps.extend(r.extra)
        for w in writes:
            if w.w is not None:
                deps.append(w.w)
            deps.extend(w.rd)
            deps.extend(w.extra)
        return deps

    def _wait(self, e, deps, skip_self=False):
        seen = self.seen[e]
        for t in sorted(deps, key=lambda t: -t.val):
            if skip_self and t.key == e:
                continue
            if seen.get(t.key, 0) >= t.val:
                continue
            self.prog[e].append(('w', self.sem[t.key], t.val))
            self.n_wait += 1
            seen[t.key] = t.val
            for k, v in t.clock.items():
                if seen.get(k, 0) < v:
                    seen[k] = v

    def _commit(self, tok, reads, writes):
        for r in reads:
            r.rd.append(tok)
            if len(r.rd) > 48:
                best = {}
                for t in r.rd:
                    if t.key not in best or best[t.key].val < t.val:
                        best[t.key] = t
                r.rd = list(best.values())
        for w in writes:
            if self.cond is not None:
                ex = w.extra + ([w.w] if w.w is not None else []) + w.rd
                best = {}
                for t in ex:
                    if t.key not in best or best[t.key].val < t.val:
                        best[t.key] = t
                w.extra = list(best.values())
            else:
                w.extra = []
            w.w = tok
            w.rd = []

    def op(self, e, name, reads=(), writes=(), **kw):
        deps = self._deps(e, reads, writes)
        self._wait(e, deps, skip_self=(e == 'pe'))
        self.cnt[e] += 1
        self.prog[e].append(('o', name, kw, self.sem[e], 1))
        self.n_inst += 1
        base = self.cond[e][0] if self.cond is not None else self.seen[e]
        clock = {k: v for k, v in base.items() if k in CE}
        tok = Tok(e, self.cnt[e], clock)
        self._commit(tok, reads, writes)
        return tok

    def dma(self, q, out, in_, reads=(), writes=(), _meth='dma_start', **kw):
        deps = self._deps(q, reads, writes)
        i = self.slot_i[q]
        self.slot_i[q] = (i + 1) % self.nslot[q]
        k = 'd_%s_%d' % (q, i)
        if self.cnt[k] > 0:
            deps.append(Tok(k, self.cnt[k], {}))
        self._wait(q, deps)
        self.cnt[k] += 16
        kw = dict(kw)
        kw['out'] = out
        kw['in_'] = in_
        self.prog[q].append(('o', _meth, kw, self.sem[k], 16))
        self.n_inst += 1
        base = self.cond[q][0] if self.cond is not None else self.seen[q]
        clock = {kk: v for kk, v in base.items() if kk in CE}
        tok = Tok(k, self.cnt[k], clock)
        self._commit(tok, reads, writes)
        return tok

    def cond_begin(self, flag_ap, r_flag):
        assert self.cond is None
        self.cond = {}
        for e in self.eng:
            deps = [r_flag.w] if r_flag.w is not None else []
            self._wait(e, deps)
            self.prog[e].append(('cb', flag_ap))
            self.cond[e] = (dict(self.seen[e]), dict(self.cnt))

    def cond_end(self):
        for e in self.eng:
            seen0, cnt0 = self.cond[e]
            fix = []
            if e in CE and self.cnt[e] != cnt0[e]:
                fix.append((self.sem[e], self.cnt[e] - cnt0[e]))
            if e in self.dq:
                for s in range(self.nslot[e]):
                    k = 'd_%s_%d' % (e, s)
                    if self.cnt[k] != cnt0[k]:
                        fix.append((self.sem[k], self.cnt[k] - cnt0[k]))
            self.prog[e].append(('ce', fix))
            self.seen[e] = seen0
        self.cond = None

    def end(self):
        deps = []
        for q in self.dq:
            for s in range(self.nslot[q]):
                k = 'd_%s_%d' % (q, s)
                if self.cnt[k] > 0:
                    deps.append(Tok(k, self.cnt[k], {}))
        self._wait('sp', deps)
        nc = self.nc
        prog = self.prog
        E_of = {'pe': nc.tensor, 'act': nc.scalar, 'dve': nc.vector, 'pool': nc.gpsimd, 'sp': nc.sync}

        def run(E, items):
            i = 0
            n = len(items)
            while i < n:
                it = items[i]
                if it[0] == 'w':
                    E.wait_ge(it[1], it[2])
                elif it[0] == 'o':
                    getattr(E, it[1])(**it[2]).then_inc(it[3], it[4])
                elif it[0] == 'cb':
                    j = i + 1
                    while items[j][0] != 'ce':
                        j += 1
                    inner = items[i + 1:j]
                    fix = items[j][1]
                    if any(x[0] == 'o' for x in inner):
                        val = E.value_load(it[1], min_val=0, max_val=1)
                        with E.If(val > 0):
                            run(E, inner)
                        with E.Else():
                            for (sem, amt) in fix:
                                E.drain().then_inc(sem, amt)
                    i = j
                i += 1

        with nc.Block() as block:
            @block.tensor
            def _(E):
                run(E, prog['pe'])

            @block.scalar
            def _(E):
                run(E, prog['act'])

            @block.vector
            def _(E):
                run(E, prog['dve'])

            @block.gpsimd
            def _(E):
                run(E, prog['pool'])

            @block.sync
            def _(E):
                run(E, prog['sp'])
        self.prog = {e: [] for e in self.eng}
        full = dict(self.cnt)
        for e in self.eng:
            self.seen[e] = dict(full)
        self.pes.close()
        self.pes = None
        self.all_res = []


D = 1024
KD = 8
HD = 64
OFF_ZU, OFF_ZV, OFF_SQ, OFF_SK, OFF_SV, OFF_FQ, OFF_FK, OFF_FV, OFF_FF, OFF_CA, OFF_CG, OFF_G = (
    0, 512, 1024, 1536, 1664, 1792, 2304, 2816, 3328, 3336, 3848, 4360)
N_IN = 8456
EPS = 1e-6
TAPS = 31
TWO_PI = 2.0 * math.pi


def build(NSEQ=2, S=2048, DFF=3584, NE=8, DEPTH=2, BS=None):
    NT = S // 128
    NTT = NSEQ * NT
    NTOK = NSEQ * S
    if BS is None:
        BS = 768 if S >= 1536 else min(1024, S // 2)
    SB = BS // 128
    NBLK = (2 * NTOK + NE * (BS - 1)) // BS
    JMAX = (NTOK + BS - 1) // BS
    TBW = 384 if BS == 768 else min(512, BS)
    IOA = bass.IndirectOffsetOnAxis
    NB = S // 512
    NR = DFF // 512
    n_dense = (DEPTH + 1) // 2
    n_moe = DEPTH // 2
    sparse = (n_moe == 1 and DEPTH % 2 == 0)
    NR2 = NR * 2
    nc = bass.Bass("TRN2", target_bir_lowering=False)
    dt = lambda name, shape, d=F32: nc.dram_tensor(name, list(shape), d, kind="ExternalInput").ap()
    x_d = dt("x", [NSEQ, S, D])
    pos_d = dt("positions", [NSEQ, S], I32)
    norm_mix_g = dt("norm_mix_g", [DEPTH, D])
    w_in = dt("w_in", [DEPTH, D, N_IN])
    gmlp_ln_g = dt("gmlp_ln_g", [DEPTH, 512])
    gmlp_ln_b = dt("gmlp_ln_b", [DEPTH, 512])
    gmlp_ws = dt("gmlp_ws", [DEPTH, 8, 128, 128])
    gmlp_bs = dt("gmlp_bs", [DEPTH, 8, 128])
    swa_sink = dt("swa_sink", [DEPTH, 8])
    fox_bf = dt("fox_bf", [DEPTH, 8])
    conv_w = dt("conv_w", [DEPTH, TAPS, 512])
    conv_b = dt("conv_b", [DEPTH, 512])
    conv_ln_g = dt("conv_ln_g", [DEPTH, 512])
    conv_ln_b = dt("conv_ln_b", [DEPTH, 512])
    w_branch = dt("w_branch", [DEPTH, 4, 512, D])
    w_out = dt("w_out", [DEPTH, D, D])
    norm_ffn_g = dt("norm_ffn_g", [DEPTH, D])
    ffn_w_gate = dt("ffn_w_gate", [n_dense, D, DFF])
    ffn_w_up = dt("ffn_w_up", [n_dense, D, DFF])
    ffn_w_down = dt("ffn_w_down", [n_dense, DFF, D])
    router_w = dt("router_w", [max(n_moe, 1), D, NE])
    if sparse:
        exp_w_gate = dt("exp_w_gate", [NE * 128 * NR2, 2048])
        exp_w_up = dt("exp_w_up", [NE * 128 * NR2, 2048])
        exp_w_down = dt("exp_w_down", [NE * 128 * NR2, 2048])
    else:
        exp_w_gate = dt("exp_w_gate", [max(n_moe, 1), NE, D, DFF])
        exp_w_up = dt("exp_w_up", [max(n_moe, 1), NE, D, DFF])
        exp_w_down = dt("exp_w_down", [max(n_moe, 1), NE, DFF, D])
    norm_final_g = dt("norm_final_g", [1, D])
    out_d = nc.dram_tensor("out", [NSEQ, S, D], F32, kind="ExternalOutput").ap()
    OT = nc.dram_tensor("ot_scr", [4, 128, 4, S], BF16, kind="Internal").ap()
    SG = nc.dram_tensor("sg_scr", [S, 4096], BF16, kind="Internal").ap()
    CS = nc.dram_tensor("cs_scr", [2, 64, S], F32, kind="Internal").ap()
    HTK = nc.dram_tensor("htk_scr", [NTOK, D], BF16, kind="Internal").ap()
    HS = nc.dram_tensor("hs_scr", [NBLK * BS, D], BF16, kind="Internal").ap()
    YS = nc.dram_tensor("ys_scr", [NBLK * BS, D], F32, kind="Internal").ap()
    XS = nc.dram_tensor("xs_scr", [NSEQ, S, D], F32, kind="Internal").ap()

    with ExitStack() as es:
        fw = FW(nc, es)
        sbp = lambda name, shape, d: es.enter_context(nc.sbuf_tensor(name, list(shape), d))
        xs = sbp("xs", [128, NT, D], F32)
        hT = sbp("hT", [128, KD, S], BF16)
        ident = sbp("ident", [128, 128], BF16)
        identf = sbp("identf", [128, 128], F32)
        onesf = sbp("onesf", [128, 128], F32)
        triu = sbp("triu", [128, 128], F32)
        mdiag = sbp("mdiag", [128, 128], BF16)
        mprev = sbp("mprev", [128, 128], BF16)
        mask2 = sbp("mask2", [128, 256], BF16)
        epsc = sbp("epsc", [128, 1], F32)
        rec_dummy = sbp("rec_dummy", [128, 1], F32)
        negpi = sbp("negpi", [128, 1], F32)
        Gt = sbp("Gt", [128, NTT * 8], F32)
        Mf = sbp("Mf", [128, NTT * 8], F32)
        sAi = sbp("sAi", [128, NTT], I32)
        sBi = sbp("sBi", [128, NTT], I32)
        gA = sbp("gA", [128, NTT], F32)
        gB = sbp("gB", [128, NTT], F32)
        idxw = sbp("idxw", [128, NBLK * NR2], I32)
        ncum = sbp("ncum", [128, NT, 8], F32)
        tot = sbp("tot", [128, NT, 8], F32)
        negtot = sbp("negtot", [128, NT, 8], F32)
        KML = sbp("KML", [128, NT, 8, 6], BF16)
        QML = sbp("QML", [128, NT, 8, 6], BF16)
        mbias = sbp("mbias", [128, 128], F32)

        fw.begin()
        r = fw.res()
        fw.op('pool', 'memset', ap=ident[:], constant=0.0, writes=[r])
        fw.op('pool', 'affine_select', out=ident[:], in_=ident[:], pattern=[[-1, 128]], compare_op=ALU.not_equal,
              fill=1.0, base=0, channel_multiplier=1, reads=[r], writes=[r])
        r = fw.res()
        fw.op('pool', 'memset', ap=identf[:], constant=0.0, writes=[r])
        fw.op('pool', 'affine_select', out=identf[:], in_=identf[:], pattern=[[-1, 128]], compare_op=ALU.not_equal,
              fill=1.0, base=0, channel_multiplier=1, reads=[r], writes=[r])
        fw.op('dve', 'memset', ap=onesf[:], constant=1.0, writes=[fw.res()])
        r = fw.res()
        fw.op('pool', 'memset', ap=triu[:], constant=1.0, writes=[r])
        fw.op('pool', 'affine_select', out=triu[:], in_=triu[:], pattern=[[1, 128]], compare_op=ALU.is_ge,
              fill=0.0, base=0, channel_multiplier=-1, reads=[r], writes=[r])
        r = fw.res()
        fw.op('pool', 'memset', ap=mdiag[:], constant=1.0, writes=[r])
        fw.op('pool', 'affine_select', out=mdiag[:], in_=mdiag[:], pattern=[[1, 128]], compare_op=ALU.is_ge,
              fill=0.0, base=0, channel_multiplier=-1, reads=[r], writes=[r])
        r_md = r
        r = fw.res()
        fw.op('pool', 'memset', ap=mprev[:], constant=1.0, writes=[r])
        fw.op('pool', 'affine_select', out=mprev[:], in_=mprev[:], pattern=[[-1, 128]], compare_op=ALU.is_gt,
              fill=0.0, base=0, channel_multiplier=1, reads=[r], writes=[r])
        r_mp = r
        r = fw.res()
        fw.op('pool', 'memset', ap=mbias[:], constant=0.0, writes=[r])
        fw.op('pool', 'affine_select', out=mbias[:], in_=mbias[:], pattern=[[1, 128]], compare_op=ALU.is_ge,
              fill=-30000.0, base=0, channel_multiplier=-1, reads=[r], writes=[r])
        r_m2 = fw.res()
        fw.op('pool', 'tensor_copy', out=mask2[:, 128:256], in_=mdiag[:], reads=[r_md], writes=[r_m2])
        fw.op('pool', 'tensor_copy', out=mask2[:, 0:128], in_=mprev[:], reads=[r_mp], writes=[r_m2])
        fw.op('dve', 'memset', ap=epsc[:], constant=EPS, writes=[fw.res()])
        fw.op('dve', 'memset', ap=negpi[:], constant=-math.pi, writes=[fw.res()])
        fw.end()

        def load_w(dst, src, res, q='pool'):
            fw.dma(q, dst, src.rearrange("(k p) n -> p k n", p=128), writes=[res])

        def phase_norm(g_row, moe_router=None, seq=0, g_row2=None):
            fw.begin()
            sp_ = sparse and moe_router is not None
            gT = fw.sb("gT", [128, KD], F32)
            r_g = fw.res()
            fw.dma('sp', gT[:], g_row.rearrange("(k p) -> p k", p=128), writes=[r_g], allow_slow_non_contiguous=True)
            ss = fw.sb("ss", [128, NT], F32)
            rstd = fw.sb("rstd", [128, NT], F32)
            junk = [fw.sb("junk%d" % i, [128, D], BF16) for i in range(2)]
            r_junk = [fw.res() for _ in range(2)]
            xn = [fw.sb("xn%d" % i, [128, D], BF16) for i in range(2)]
            r_xn = [fw.res() for _ in range(2)]
            pT = [fw.ps("pT%d" % i, [128, KD, 128], BF16) for i in range(2)]
            r_pT = [fw.res(True) for _ in range(2)]
            r_ss = [fw.res() for _ in range(NT)]
            r_hT = [fw.res() for _ in range(NT)]
            if moe_router is not None:
                wr = fw.sb("wr", [128, KD, NE], F32)
                r_wr = fw.res()
                fw.dma('sp', wr[:], moe_router.rearrange("(k p) e -> p k e", p=128), writes=[r_wr])
                for k in range(KD):
                    fw.op('dve', 'tensor_scalar', out=wr[:, k, :], in0=wr[:, k, :], scalar1=gT[:, k:k + 1], scalar2=None,
                          op0=ALU.mult, reads=[r_wr, r_g], writes=[r_wr])
                xf = [fw.sb("xf%d" % i, [128, D], F32) for i in range(2)]
                r_xf = [fw.res() for _ in range(2)]
                xfT = [fw.sb("xfT%d" % i, [128, KD, 128], F32) for i in range(2)]
                r_xfT = [fw.res() for _ in range(2)]
                pTf = [fw.ps("pTf%d" % i, [128, 4, 128], F32) for i in range(2)]
                r_pTf = [fw.res(True) for _ in range(2)]
                pL = fw.ps("pL", [128, NE], F32)
                r_pL = fw.res(True)
                lg = fw.sb("lg", [128, 8], F32)
                top = fw.sb("top", [128, 8], F32)
                nm1 = fw.sb("nm1", [128, 1], F32)
                ex = fw.sb("ex", [128, 8], F32)
                dd = fw.sb("dd", [128, 1], F32)
                msk = fw.sb("msk", [128, 8], F32)
                r_s = fw.res()
            if sp_:
                gfb_ = fw.sb("gfb_", [128, D], F32)
                fw.dma('sp', gfb_[:], g_row2.partition_broadcast(128), writes=[r_g])
                hrow = [fw.sb("hrow%d" % i, [128, D], BF16) for i in range(2)]
                r_hrow = [fw.res() for _ in range(2)]
            for t in range(NT):
                b = t % 2
                r_x = r_xs[t]
                fw.op('act', 'activation', out=junk[b][:], in_=xs[:, t, :], func=AF.Square, accum_out=ss[:, t:t + 1],
                      reads=[r_x], writes=[r_junk[b], r_ss[t]])
                fw.op('act', 'activation', out=rstd[:, t:t + 1], in_=ss[:, t:t + 1], func=AF.Ln, scale=1.0 / D,
                      bias=epsc[:, 0:1], reads=[r_ss[t]], writes=[r_ss[t]])
                fw.op('act', 'activation', out=rstd[:, t:t + 1], in_=rstd[:, t:t + 1], func=AF.Exp, scale=-0.5,
                      reads=[r_ss[t]], writes=[r_ss[t]])
                fw.op('act', 'activation', out=xn[b][:], in_=xs[:, t, :], func=AF.Copy, scale=rstd[:, t:t + 1],
                      reads=[r_x, r_ss[t]], writes=[r_xn[b]])
                for k in range(0 if sp_ else KD):
                    fw.op('pe', 'transpose', out=pT[b][:, k, :], in_=xn[b][:, k * 128:(k + 1) * 128], identity=ident[:],
                          reads=[r_xn[b]], writes=[r_pT[b]])
                for k in range(0 if sp_ else KD):
                    fw.op('dve', 'tensor_scalar', out=hT[:, k, t * 128:(t + 1) * 128], in0=pT[b][:, k, :],
                          scalar1=gT[:, k:k + 1], scalar2=None, op0=ALU.mult, reads=[r_pT[b], r_g], writes=[r_hT[t]])
                if sp_:
                    fw.op('pool', 'tensor_tensor', out=hrow[b][:], in0=xn[b][:], in1=gfb_[:], op=ALU.mult,
                          reads=[r_xn[b], r_g], writes=[r_hrow[b]])
                    fw.dma('sp', HTK[seq * S + t * 128:seq * S + (t + 1) * 128, :], hrow[b][:], reads=[r_hrow[b]], writes=[fw.res()])
                if moe_router is not None:
                    fw.op('act', 'activation', out=xf[b][:], in_=xs[:, t, :], func=AF.Copy, scale=rstd[:, t:t + 1],
                          reads=[r_x, r_ss[t]], writes=[r_xf[b]])
                    for hh in range(2):
                        for k4 in range(4):
                            k = hh * 4 + k4
                            fw.op('pe', 'matmul', out=pTf[hh][:, k4, :], lhsT=xf[b][:, k * 128:(k + 1) * 128], rhs=identf[:],
                                  start=True, stop=True, reads=[r_xf[b]], writes=[r_pTf[hh]])
                        fw.op('act', 'activation', out=xfT[b][:, hh * 4:(hh + 1) * 4, :], in_=pTf[hh][:], func=AF.Copy,
                              reads=[r_pTf[hh]], writes=[r_xfT[b]])
                    for k in range(KD):
                        fw.op('pe', 'matmul', out=pL[:], lhsT=xfT[b][:, k, :], rhs=wr[:, k, :], start=(k == 0), stop=(k == KD - 1),
                              reads=[r_xfT[b], r_wr], writes=[r_pL])
                    fw.op('dve', 'tensor_copy', out=lg[:, 0:NE], in_=pL[:], reads=[r_pL], writes=[r_s])
                    fw.op('dve', 'max', out=top[:], in_=lg[:, 0:NE], reads=[r_s], writes=[r_s])
                    fw.op('dve', 'tensor_scalar', out=nm1[:], in0=top[:, 0:1], scalar1=-1.0, scalar2=None, op0=ALU.mult,
                          reads=[r_s], writes=[r_s])
                    fw.op('act', 'activation', out=ex[:, 0:NE], in_=lg[:, 0:NE], func=AF.Exp, bias=nm1[:, 0:1], reads=[r_s], writes=[r_s])
                    fw.op('act', 'activation', out=dd[:], in_=top[:, 1:2], func=AF.Exp, bias=nm1[:, 0:1], reads=[r_s], writes=[r_s])
                    fw.op('dve', 'tensor_scalar', out=dd[:], in0=dd[:], scalar1=1.0, scalar2=None, op0=ALU.add, reads=[r_s], writes=[r_s])
                    fw.op('dve', 'reciprocal', out=dd[:], in_=dd[:], reads=[r_s], writes=[r_s])
                    fw.op('dve', 'tensor_scalar', out=msk[:, 0:NE], in0=lg[:, 0:NE], scalar1=top[:, 1:2], scalar2=None, op0=ALU.is_ge,
                          reads=[r_s], writes=[r_s])
                    tt_ = seq * NT + t if sparse else t
                    fw.op('dve', 'scalar_tensor_tensor', out=Gt[:, tt_ * 8:tt_ * 8 + NE], in0=ex[:, 0:NE], scalar=dd[:, 0:1], in1=msk[:, 0:NE],
                          op0=ALU.mult, op1=ALU.mult, reads=[r_s], writes=[r_s])
                    fw.op('dve', 'tensor_copy', out=Mf[:, tt_ * 8:tt_ * 8 + NE], in_=msk[:, 0:NE], reads=[r_s], writes=[r_s])
            fw.end()

        def phase_gmlp(l):
            fw.begin()
            wA = fw.sb("wA", [128, KD, 1024], BF16)
            r_wA = fw.res()
            load_w(wA[:, :, 0:512], w_in[l, :, OFF_ZU:OFF_ZU + 512], r_wA)
            load_w(wA[:, :, 512:1024], w_in[l, :, OFF_ZV:OFF_ZV + 512], r_wA)
            wsb = fw.sb("wsb", [128, 8, 128], BF16)
            r_ws = fw.res()
            fw.dma('pool', wsb[:], gmlp_ws[l].rearrange("g t s -> t g s"), writes=[r_ws])
            pW = fw.ps("pW", [128, 8, 128], BF16)
            r_pW = fw.res(True)
            for g in range(8):
                fw.op('pe', 'transpose', out=pW[:, g, :], in_=wsb[:, g, :], identity=ident[:], reads=[r_ws], writes=[r_pW])
            wsT = fw.sb("wsT", [128, 8, 128], BF16)
            r_wsT = fw.res()
            fw.op('dve', 'tensor_copy', out=wsT[:], in_=pW[:], reads=[r_pW], writes=[r_wsT])
            fw.op('pool', 'affine_select', out=wsT[:], in_=wsT[:], pattern=[[0, 8], [1, 128]], compare_op=ALU.is_ge, fill=0.0,
                  base=0, channel_multiplier=-1, reads=[r_wsT], writes=[r_wsT])
            bsT = fw.sb("bsT", [128, 8], F32)
            r_c = fw.res()
            fw.dma('sp', bsT[:], gmlp_bs[l].rearrange("g t -> t g"), writes=[r_c], allow_slow_non_contiguous=True)
            lng = fw.sb("lng", [128, 512], F32)
            lnb = fw.sb("lnb", [128, 512], F32)
            fw.dma('sp', lng[:], gmlp_ln_g[l:l + 1, :].partition_broadcast(128), writes=[r_c])
            fw.dma('sp', lnb[:], gmlp_ln_b[l:l + 1, :].partition_broadcast(128), writes=[r_c])
            psUV = [fw.ps("psUV%d" % i, [128, 1024]) for i in range(2)]
            psU = [p[:, 0:512] for p in psUV]
            psV = [p[:, 512:1024] for p in psUV]
            psM = fw.ps("psM", [128, 512])
            pT2 = pW[:, 0:4, :]
            r_psU = [fw.res(True) for _ in range(2)]
            r_psV = r_psU
            r_psM = fw.res(True)
            r_pT2 = r_pW
            u = [fw.sb("u%d" % i, [128, 512], F32) for i in range(3)]
            gv = [fw.sb("gv%d" % i, [128, 512], F32) for i in range(3)]
            sq = [fw.sb("sq%d" % i, [128, 512], BF16) for i in range(3)]
            vn = [fw.sb("vn%d" % i, [128, 512], F32) for i in range(3)]
            vb = [fw.sb("vb%d" % i, [128, 512], BF16) for i in range(3)]
            oa = [fw.sb("oa%d" % i, [128, 512], BF16) for i in range(3)]
            st = [fw.sb("st%d" % i, [128, 8], F32) for i in range(3)]
            r_u = [fw.res() for _ in range(3)]
            r_gv = [fw.res() for _ in range(3)]
            r_sq = [fw.res() for _ in range(3)]
            r_vn = [fw.res() for _ in range(3)]
            r_vb = [fw.res() for _ in range(3)]
            r_oa = [fw.res() for _ in range(3)]
            r_st = [fw.res() for _ in range(3)]
            oT = [fw.sb("oT%d" % i, [128, 4, 512], BF16) for i in range(2)]
            r_oT = [fw.res() for _ in range(2)]
            r_OT = fw.res()
            gx = [fw.sb("gx%d" % i, [128, 1024], F32) for i in range(2)]
            gt = [fw.sb("gt%d" % i, [128, 1024], F32) for i in range(2)]
            r_gx = [fw.res() for _ in range(2)]
            r_gt = [fw.res() for _ in range(2)]
            gcnt = [0]

            def gelu2(b, bb, acc_ap, r_acc):
                i = gcnt[0] % 2
                gcnt[0] += 1
                fw.op('act', 'activation', out=gx[i][:, 0:512], in_=psU[b], func=AF.Copy, reads=[r_psU[b]], writes=[r_gx[i]])
                fw.op('act', 'activation', out=gx[i][:, 512:1024], in_=psV[b], func=AF.Copy, reads=[r_psU[b]], writes=[r_gx[i]])
                fw.op('act', 'activation', out=gt[i][:], in_=gx[i][:], func=AF.Square, reads=[r_gx[i]], writes=[r_gt[i]])
                fw.op('dve', 'tensor_scalar', out=gt[i][:], in0=gt[i][:], scalar1=0.044715, scalar2=1.0, op0=ALU.mult, op1=ALU.add,
                      reads=[r_gt[i]], writes=[r_gt[i]])
                fw.op('dve', 'tensor_tensor', out=gt[i][:], in0=gt[i][:], in1=gx[i][:], op=ALU.mult, reads=[r_gt[i], r_gx[i]], writes=[r_gt[i]])
                fw.op('act', 'activation', out=gt[i][:], in_=gt[i][:], func=AF.Exp, scale=-1.5957691216057308, reads=[r_gt[i]], writes=[r_gt[i]])
                fw.op('dve', 'tensor_scalar', out=gt[i][:], in0=gt[i][:], scalar1=1.0, scalar2=None, op0=ALU.add, reads=[r_gt[i]], writes=[r_gt[i]])
                fw.op('dve', 'reciprocal', out=gt[i][:], in_=gt[i][:], reads=[r_gt[i]], writes=[r_gt[i]])
                fw.op('dve', 'tensor_tensor', out=u[bb][:], in0=gx[i][:, 0:512], in1=gt[i][:, 0:512], op=ALU.mult,
                      reads=[r_gx[i], r_gt[i]], writes=[r_u[bb]])
                fw.op('dve', 'scalar_tensor_tensor', out=gv[bb][:], in0=gx[i][:, 512:1024], scalar=1.0, in1=gt[i][:, 512:1024], op0=ALU.mult,
                      op1=ALU.mult, accum_out=acc_ap, reads=[r_gx[i], r_gt[i]], writes=[r_gv[bb], r_acc])

            def part1(t):
                b = t % 2
                bb = t % 3
                tok = slice(t * 128, (t + 1) * 128)
                for k in range(KD):
                    fw.op('pe', 'matmul', out=psU[b][:], lhsT=hT[:, k, tok], rhs=wA[:, k, 0:512], start=(k == 0), stop=(k == KD - 1),
                          reads=[r_wA], writes=[r_psU[b]])
                for k in range(KD):
                    fw.op('pe', 'matmul', out=psV[b][:], lhsT=hT[:, k, tok], rhs=wA[:, k, 512:1024], start=(k == 0), stop=(k == KD - 1),
                          reads=[r_wA], writes=[r_psV[b]])
                s = st[bb]
                gelu2(b, bb, s[:, 0:1], r_st[bb])
                fw.op('act', 'activation', out=sq[bb][:], in_=gv[bb][:], func=AF.Square, accum_out=s[:, 1:2],
                      reads=[r_gv[bb]], writes=[r_sq[bb], r_st[bb]])
                rs = [r_st[bb]]
                fw.op('dve', 'tensor_scalar', out=s[:, 2:3], in0=s[:, 0:1], scalar1=1.0 / 512, scalar2=None, op0=ALU.mult, reads=rs, writes=rs)
                fw.op('dve', 'tensor_tensor', out=s[:, 3:4], in0=s[:, 2:3], in1=s[:, 2:3], op=ALU.mult, reads=rs, writes=rs)
                fw.op('dve', 'scalar_tensor_tensor', out=s[:, 4:5], in0=s[:, 1:2], scalar=1.0 / 512, in1=s[:, 3:4], op0=ALU.mult,
                      op1=ALU.subtract, reads=rs, writes=rs)
                fw.op('act', 'activation', out=s[:, 5:6], in_=s[:, 4:5], func=AF.Ln, bias=epsc[:, 0:1], reads=rs, writes=rs)
                fw.op('act', 'activation', out=s[:, 5:6], in_=s[:, 5:6], func=AF.Exp, scale=-0.5, reads=rs, writes=rs)
                fw.op('dve', 'tensor_scalar', out=vn[bb][:], in0=gv[bb][:], scalar1=s[:, 2:3], scalar2=s[:, 5:6], op0=ALU.subtract,
                      op1=ALU.mult, reads=[r_gv[bb], r_st[bb]], writes=[r_vn[bb]])
                fw.op('pool', 'tensor_tensor', out=vn[bb][:], in0=vn[bb][:], in1=lng[:], op=ALU.mult, reads=[r_vn[bb], r_c], writes=[r_vn[bb]])
                fw.op('pool', 'tensor_tensor', out=vb[bb][:], in0=vn[bb][:], in1=lnb[:], op=ALU.add, reads=[r_vn[bb], r_c], writes=[r_vb[bb]])
            def part2(t):
                bb = t % 3
                for g in range(8):
                    gs = slice(g * 64, (g + 1) * 64)
                    fw.op('pe', 'matmul', out=psM[:, gs], lhsT=wsT[:, g, :], rhs=vb[bb][:, gs], start=True, stop=True,
                          reads=[r_wsT, r_vb[bb]], writes=[r_psM])
                for g in range(8):
                    gs = slice(g * 64, (g + 1) * 64)
                    fw.op('dve', 'scalar_tensor_tensor', out=oa[bb][:, gs], in0=psM[:, gs], scalar=bsT[:, g:g + 1], in1=u[bb][:, gs],
                          op0=ALU.add, op1=ALU.mult, reads=[r_psM, r_c, r_u[bb]], writes=[r_oa[bb]])
                for c in range(4):
                    fw.op('pe', 'transpose', out=pT2[:, c, :], in_=oa[bb][:, c * 128:(c + 1) * 128], identity=ident[:],
                          reads=[r_oa[bb]], writes=[r_pT2])
                blk = t // 4
                ob = blk % 2
                fw.op('act', 'activation', out=oT[ob][:, :, (t % 4) * 128:(t % 4 + 1) * 128], in_=pT2[:], func=AF.Copy,
                      reads=[r_pT2], writes=[r_oT[ob]])
                if t % 4 == 3:
                    fw.dma('sp', OT[0, :, :, blk * 512:(blk + 1) * 512], oT[ob][:], reads=[r_oT[ob]], writes=[r_OT])
            gu = iter(())
            per = 0
            for t in range(NT):
                part1(t)
                for _ in range(per):
                    next(gu, None)
                if t > 1:
                    part2(t - 2)
            part2(NT - 2)
            part2(NT - 1)
            for _ in gu:
                pass
            fw.end()

        def phase_conv(l):
            fw.begin()
            wD = fw.sb("wD", [128, KD, 1024], BF16)
            r_wD = fw.res()
            load_w(wD[:, :, 0:512], w_in[l, :, OFF_CA:OFF_CA + 512], r_wD)
            load_w(wD[:, :, 512:1024], w_in[l, :, OFF_CG:OFF_CG + 512], r_wD)
            cw = fw.sb("cw", [128, 4, TAPS], F32)
            cb = fw.sb("cb", [128, 4], F32)
            cg_ = fw.sb("cg_", [128, 4], F32)
            cbe = fw.sb("cbe", [128, 4], F32)
            r_c = fw.res()
            for c in range(4):
                fw.dma('sp', cw[:, c, :], conv_w[l, :, c * 128:(c + 1) * 128].rearrange("j p -> p j"), writes=[r_c],
                       allow_slow_non_contiguous=True)
            fw.dma('sp', cb[:], conv_b[l].rearrange("(k p) -> p k", p=128), writes=[r_c], allow_slow_non_contiguous=True)
            fw.dma('sp', cg_[:], conv_ln_g[l].rearrange("(k p) -> p k", p=128), writes=[r_c], allow_slow_non_contiguous=True)
            fw.dma('sp', cbe[:], conv_ln_b[l].rearrange("(k p) -> p k", p=128), writes=[r_c], allow_slow_non_contiguous=True)
            PAD = TAPS - 1
            yT = [[fw.sb("yT%d_%d" % (c, i), [128, PAD + 512], BF16) for i in range(2)] for c in range(4)]
            dg = fw.sb("dg", [128, 4, TAPS, 128], BF16)
            r_dg = fw.res()
            for c in range(4):
                for j in range(TAPS):
                    fw.op('dve', 'tensor_scalar', out=dg[:, c, j, :], in0=ident[:], scalar1=cw[:, c, j:j + 1], scalar2=None, op0=ALU.mult,
                          reads=[r_c], writes=[r_dg])
            psK = [fw.ps("psK%d" % i, [128, 512]) for i in range(2)]
            r_psK = [fw.res(True) for _ in range(2)]
            r_yT = [[fw.res() for i in range(2)] for c in range(4)]
            acc = [fw.sb("acc%d" % c, [128, 512], F32) for c in range(4)]
            r_acc = [fw.res() for _ in range(4)]
            psA = [fw.ps("psA%d" % i, [128, 512]) for i in range(2)]
            psG = [fw.ps("psG%d" % i, [128, 512]) for i in range(2)]
            r_psA = [fw.res(True) for _ in range(2)]
            r_psG = [fw.res(True) for _ in range(2)]
            ps1 = fw.ps("ps1", [128, 512])
            ps2 = fw.ps("ps2", [128, 512])
            r_ps1 = fw.res(True)
            r_ps2 = fw.res(True)
            sig = [fw.sb("sig%d" % i, [128, 512], F32) for i in range(2)]
            r_sig = [fw.res() for _ in range(2)]
            sqb = [fw.sb("sqb%d" % i, [128, 512], F32) for i in range(2)]
            r_sqb = [fw.res() for _ in range(2)]
            mean = fw.sb("mean", [128, 512], F32)
            msq = fw.sb("msq", [128, 512], F32)
            rsd = fw.sb("rsd", [128, 512], F32)
            r_m = fw.res()
            tmp = [fw.sb("tmp%d" % i, [128, 512], F32) for i in range(2)]
            r_tmp = [fw.res() for _ in range(2)]
            od = [fw.sb("od%d" % i, [128, 4, 512], BF16) for i in range(2)]
            r_od = [fw.res() for _ in range(2)]
            r_OT = fw.res()
            def unit_proj(n):
                bk, c = divmod(n, 4)
                yb = bk % 2
                b = n % 2
                tok = slice(bk * 512, (bk + 1) * 512)
                y = yT[c][yb]
                ry = r_yT[c][yb]
                if bk == 0:
                    fw.op('pool', 'memset', ap=y[:, 0:PAD], constant=0.0, writes=[ry])
                else:
                    fw.op('pool', 'tensor_copy', out=y[:, 0:PAD], in_=yT[c][1 - yb][:, 512:512 + PAD], reads=[r_yT[c][1 - yb]], writes=[ry])
                for k in range(KD):
                    fw.op('pe', 'matmul', out=psA[b][:], lhsT=wD[:, k, c * 128:(c + 1) * 128], rhs=hT[:, k, tok],
                          start=(k == 0), stop=(k == KD - 1), reads=[r_wD], writes=[r_psA[b]])
                for k in range(KD):
                    fw.op('pe', 'matmul', out=psG[b][:], lhsT=wD[:, k, 512 + c * 128:512 + (c + 1) * 128], rhs=hT[:, k, tok],
                          start=(k == 0), stop=(k == KD - 1), reads=[r_wD], writes=[r_psG[b]])
                fw.op('act', 'activation', out=sig[b][:], in_=psG[b][:], func=AF.Sigmoid, reads=[r_psG[b]], writes=[r_sig[b]])
                fw.op('dve', 'tensor_tensor', out=y[:, PAD:PAD + 512], in0=psA[b][:], in1=sig[b][:],
                      op=ALU.mult, reads=[r_psA[b], r_sig[b]], writes=[ry])

            def unit_taps(n):
                bk, c = divmod(n, 4)
                yb = bk % 2
                b = n % 2
                y = yT[c][yb]
                ry = r_yT[c][yb]
                for j in range(TAPS):
                    fw.op('pe', 'matmul', out=psK[b][:], lhsT=dg[:, c, j, :], rhs=y[:, j:j + 512], start=(j == 0), stop=(j == TAPS - 1),
                          reads=[ry, r_dg], writes=[r_psK[b]])
                fw.op('dve', 'tensor_scalar', out=acc[c][:], in0=psK[b][:], scalar1=cb[:, c:c + 1], scalar2=None, op0=ALU.add,
                      reads=[r_psK[b], r_c], writes=[r_acc[c]])

            def block_ln(bk):
                tok = slice(bk * 512, (bk + 1) * 512)
                for c in range(4):
                    fw.op('pe', 'matmul', out=ps1[:], lhsT=onesf[:], rhs=acc[c][:], start=(c == 0), stop=(c == 3),
                          reads=[r_acc[c]], writes=[r_ps1])
                for c in range(4):
                    sb_ = c % 2
                    fw.op('act', 'activation', out=sqb[sb_][:], in_=acc[c][:], func=AF.Square, reads=[r_acc[c]], writes=[r_sqb[sb_]])
                    fw.op('pe', 'matmul', out=ps2[:], lhsT=onesf[:], rhs=sqb[sb_][:], start=(c == 0), stop=(c == 3),
                          reads=[r_sqb[sb_]], writes=[r_ps2])
                fw.op('dve', 'tensor_scalar', out=mean[:], in0=ps1[:], scalar1=1.0 / 512, scalar2=None, op0=ALU.mult,
                      reads=[r_ps1], writes=[r_m])
                fw.op('dve', 'tensor_tensor', out=msq[:], in0=mean[:], in1=mean[:], op=ALU.mult, reads=[r_m], writes=[r_m])
                fw.op('dve', 'scalar_tensor_tensor', out=rsd[:], in0=ps2[:], scalar=1.0 / 512, in1=msq[:], op0=ALU.mult, op1=ALU.subtract,
                      reads=[r_ps2, r_m], writes=[r_m])
                fw.op('act', 'activation', out=rsd[:], in_=rsd[:], func=AF.Sqrt, bias=epsc[:, 0:1], reads=[r_m], writes=[r_m])
                fw.op('dve', 'reciprocal', out=rsd[:], in_=rsd[:], reads=[r_m], writes=[r_m])
                ob = bk % 2
                for c in range(4):
                    b = c % 2
                    fw.op('dve', 'tensor_tensor', out=tmp[b][:], in0=acc[c][:], in1=mean[:], op=ALU.subtract,
                          reads=[r_acc[c], r_m], writes=[r_tmp[b]])
                    fw.op('dve', 'tensor_tensor', out=tmp[b][:], in0=tmp[b][:], in1=rsd[:], op=ALU.mult,
                          reads=[r_tmp[b], r_m], writes=[r_tmp[b]])
                    fw.op('dve', 'tensor_scalar', out=tmp[b][:], in0=tmp[b][:], scalar1=cg_[:, c:c + 1], scalar2=cbe[:, c:c + 1],
                          op0=ALU.mult, op1=ALU.add, reads=[r_tmp[b], r_c], writes=[r_tmp[b]])
                    fw.op('act', 'activation', out=od[ob][:, c, :], in_=tmp[b][:], func=AF.Silu, reads=[r_tmp[b]], writes=[r_od[ob]])
                fw.dma('sp', OT[3, :, :, tok], od[ob][:], reads=[r_od[ob]], writes=[r_OT])

            NU = NB * 4
            for n in range(NU + 1):
                if n < NU:
                    unit_proj(n)
                if n > 0:
                    unit_taps(n - 1)
                    if (n - 1) % 4 == 3:
                        block_ln((n - 1) // 4)
            fw.end()

        def attention(banks, r_bk, nheads, kd, qT, kT, kv_of, vx, r_q, r_k, r_v, bias_of, r_bias, prev_only, add_sink, dst_idx):
            LA = 2
            psS = [banks[4 + i] for i in range(3)]
            r_psS = [r_bk[4 + i] for i in range(3)]
            psO = [banks[7][:, 0:128], banks[3][:, 0:128]]
            r_psO = [r_bk[7], r_bk[3]]
            pt = [fw.sb("pt%d" % i, [128, 512], BF16) for i in range(4)]
            r_pt = [fw.res() for _ in range(4)]
            rec = [fw.sb("rec%d" % i, [128, 128], F32) for i in range(2)]
            r_rec = [fw.res() for _ in range(2)]
            oTt = fw.sb("oTt", [128, S], BF16)
            r_oTt = fw.res()
            r_OT = fw.res()
            groups = []
            for h in range(nheads):
                for i in range(NT):
                    js = [j for j in ((i - 1, i) if prev_only else range(i + 1)) if j >= 0]
                    chunks = [js[c:c + 4] for c in range(0, len(js), 4)]
                    for ci, ch in enumerate(chunks):
                        groups.append((h, i, ch, ci == 0, ci == len(chunks) - 1))
            pending = []

            def flush_one():
                gi, (h, i, ch, first, last) = pending.pop(0)
                hk = kv_of(h)
                pb = gi % 4
                ob = (h * NT + i) % 2
                half = h % 2
                qs = slice(i * 128, (i + 1) * 128)
                for jj, j in enumerate(ch):
                    fw.op('pe', 'matmul', out=psO[ob], lhsT=vx[:, j, hk, :], rhs=pt[pb][:, jj * 128:(jj + 1) * 128],
                          start=(first and jj == 0), stop=(last and jj == len(ch) - 1), reads=[r_v, r_pt[pb]], writes=[r_psO[ob]])
                if last:
                    if add_sink is not None:
                        fw.op('dve', 'tensor_scalar', out=rec[ob][64:128, :], in0=psO[ob][64:128, :],
                              scalar1=add_sink[0][64:128, add_sink[1] + h:add_sink[1] + h + 1],
                              scalar2=None, op0=ALU.add, reads=[r_psO[ob], r_bias], writes=[r_rec[ob]])
                        fw.op('dve', 'reciprocal', out=rec[ob][64:128, :], in_=rec[ob][64:128, :], reads=[r_rec[ob]], writes=[r_rec[ob]])
                    else:
                        fw.op('dve', 'reciprocal', out=rec[ob][64:128, :], in_=psO[ob][64:128, :], reads=[r_psO[ob]], writes=[r_rec[ob]])
                    fw.op('dve', 'tensor_tensor', out=oTt[half * 64:(half + 1) * 64, qs], in0=psO[ob][0:64, :], in1=rec[ob][64:128, :],
                          op=ALU.mult, reads=[r_psO[ob], r_rec[ob]], writes=[r_oTt])
                    if half == 1 and i == NT - 1:
                        hc = dst_idx[1] + h // 2
                        fw.dma('sp', OT[dst_idx[0], :, hc, :], oTt[:], reads=[r_oTt], writes=[r_OT])

            for gi, g in enumerate(groups):
                h, i, ch, first, last = g
                hk = kv_of(h)
                sb_ = gi % 3
                pb = gi % 4
                qs = slice(i * 128, (i + 1) * 128)
                n = len(ch)
                for jj, j in enumerate(ch):
                    ks = slice(j * 128, (j + 1) * 128)
                    fw.op('pe', 'matmul', out=psS[sb_][:, jj * 128:(jj + 1) * 128], lhsT=kT[0:kd, hk, ks], rhs=qT[0:kd, h, qs],
                          start=True, stop=True, reads=[r_q, r_k], writes=[r_psS[sb_]])
                kw = {}
                rds = [r_psS[sb_]]
                bias = bias_of(i, h)
                if bias is not None:
                    kw['bias'] = bias
                    rds.append(r_bias)
                fw.op('act', 'activation', out=pt[pb][:, 0:n * 128], in_=psS[sb_][:, 0:n * 128], func=AF.Exp, scale=0.125,
                      reads=rds, writes=[r_pt[pb]], **kw)
                if prev_only and n == 2:
                    fw.op('pool', 'tensor_tensor', out=pt[pb][:, 0:256], in0=pt[pb][:, 0:256], in1=mask2[:], op=ALU.mult,
                          reads=[r_pt[pb]], writes=[r_pt[pb]])
                else:
                    for jj, j in enumerate(ch):
                        if j == i:
                            fw.op('pool', 'tensor_tensor', out=pt[pb][:, jj * 128:(jj + 1) * 128], in0=pt[pb][:, jj * 128:(jj + 1) * 128],
                                  in1=mdiag[:], op=ALU.mult, reads=[r_pt[pb]], writes=[r_pt[pb]])
                        elif prev_only:
                            fw.op('pool', 'tensor_tensor', out=pt[pb][:, jj * 128:(jj + 1) * 128], in0=pt[pb][:, jj * 128:(jj + 1) * 128],
                                  in1=mprev[:], op=ALU.mult, reads=[r_pt[pb]], writes=[r_pt[pb]])
                pending.append((gi, g))
                if len(pending) > LA:
                    flush_one()
            while pending:
                flush_one()

        def attention_blk(banks, r_bk, nheads, kd, qT, kT, vx, r_q, r_k, r_v, dst_idx):
            LA = 2
            psS = [banks[4 + i] for i in range(3)]
            r_psS = [r_bk[4 + i] for i in range(3)]
            psO = [banks[7], banks[3]]
            r_psO = [r_bk[7], r_bk[3]]
            pt = [fw.sb("pt%d" % i, [128, 512], BF16) for i in range(4)]
            r_pt = [fw.res() for _ in range(4)]
            rec = [fw.sb("rec%d" % i, [128, 512], F32) for i in range(2)]
            r_rec = [fw.res() for _ in range(2)]
            oTt = fw.sb("oTt", [128, S], BF16)
            r_oTt = fw.res()
            r_OT = fw.res()
            groups = []
            for h in range(nheads):
                for B in range(NB):
                    nj = 4 * B + 4
                    for j in range(nj):
                        groups.append((h, B, j, j == 0, j == nj - 1))
            pending = []

            def geom(B, j):
                if j < 4 * B:
                    return 0, 512
                jr = j - 4 * B
                return jr * 128, (4 - jr) * 128

            def flush_one():
                gi, (h, B, j, first, last) = pending.pop(0)
                pb = gi % 4
                ob = (h * NB + B) % 2
                half = h % 2
                c0, ncols = geom(B, j)
                fw.op('pe', 'matmul', out=psO[ob][:, c0:c0 + ncols], lhsT=vx[:, j, h, :], rhs=pt[pb][:, 0:ncols],
                      start=first, stop=last, reads=[r_v, r_pt[pb]], writes=[r_psO[ob]])
                if last:
                    qb = slice(B * 512, (B + 1) * 512)
                    fw.op('dve', 'reciprocal', out=rec[ob][64:128, :], in_=psO[ob][64:128, :], reads=[r_psO[ob]], writes=[r_rec[ob]])
                    fw.op('dve', 'tensor_tensor', out=oTt[half * 64:(half + 1) * 64, qb], in0=psO[ob][0:64, :], in1=rec[ob][64:128, :],
                          op=ALU.mult, reads=[r_psO[ob], r_rec[ob]], writes=[r_oTt])
                    if half == 1 and B == NB - 1:
                        hc = dst_idx[1] + h // 2
                        fw.dma('sp', OT[dst_idx[0], :, hc, :], oTt[:], reads=[r_oTt], writes=[r_OT])

            for gi, g in enumerate(groups):
                h, B, j, first, last = g
                sb_ = gi % 3
                pb = gi % 4
                c0, ncols = geom(B, j)
                q0 = B * 512 + c0
                ks = slice(j * 128, (j + 1) * 128)
                fw.op('pe', 'matmul', out=psS[sb_][:, 0:ncols], lhsT=kT[0:kd, h, ks], rhs=qT[0:kd, h, q0:q0 + ncols],
                      start=True, stop=True, reads=[r_q, r_k], writes=[r_psS[sb_]])
                if j >= 4 * B:
                    fw.op('dve', 'tensor_tensor', out=psS[sb_][:, 0:128], in0=psS[sb_][:, 0:128], in1=mbias[:], op=ALU.add,
                          reads=[r_psS[sb_]], writes=[r_psS[sb_]])
                fw.op('act', 'activation', out=pt[pb][:, 0:ncols], in_=psS[sb_][:, 0:ncols], func=AF.Exp, scale=0.125,
                      reads=[r_psS[sb_]], writes=[r_pt[pb]])
                pending.append((gi, g))
                if len(pending) > LA:
                    flush_one()
            while pending:
                flush_one()

        def proj_heads(banks, r_bk, dst, w, nh, r_w, r_dst, rope=None, w_rot=None):
            psA = [banks[i][0:64, :] for i in range(2)]
            r_psA = [r_bk[i] for i in range(2)]
            if rope is not None:
                psB = [banks[2][0:64, :], banks[3][0:64, :]]
                r_psB = [r_bk[2], r_bk[3]]
                t1 = [fw.sb("rt1_%d" % i, [64, 512], F32) for i in range(2)]
                t2 = [fw.sb("rt2_%d" % i, [64, 512], F32) for i in range(2)]
                r_t1 = [fw.res() for _ in range(2)]
                r_t2 = [fw.res() for _ in range(2)]
                cos2, sin2, r_cs = rope
            n = 0
            for h in range(nh):
                for bk in range(NB):
                    b = n % 2
                    n += 1
                    tok = slice(bk * 512, (bk + 1) * 512)
                    for k in range(KD):
                        fw.op('pe', 'matmul', out=psA[b], lhsT=w[:, k, h * 64:(h + 1) * 64], rhs=hT[:, k, tok], start=(k == 0),
                              stop=(k == KD - 1), reads=[r_w], writes=[r_psA[b]])
                    if rope is None:
                        fw.op('act', 'activation', out=dst[:, h, tok], in_=psA[b], func=AF.Copy, reads=[r_psA[b]], writes=[r_dst])
                    else:
                        for k in range(KD):
                            fw.op('pe', 'matmul', out=psB[b], lhsT=w_rot[:, k, h * 64:(h + 1) * 64], rhs=hT[:, k, tok], start=(k == 0),
                                  stop=(k == KD - 1), reads=[r_w], writes=[r_psB[b]])
                        fw.op('dve', 'tensor_tensor', out=t1[b][:], in0=psA[b], in1=cos2[:, tok], op=ALU.mult,
                              reads=[r_psA[b], r_cs], writes=[r_t1[b]])
                        fw.op('dve', 'tensor_tensor', out=t2[b][:], in0=psB[b], in1=sin2[:, tok], op=ALU.mult,
                              reads=[r_psB[b], r_cs], writes=[r_t2[b]])
                        fw.op('pool', 'tensor_tensor', out=dst[:, h, tok], in0=t1[b][:], in1=t2[b][:], op=ALU.add,
                              reads=[r_t1[b], r_t2[b]], writes=[r_dst])

        def proj_v(banks, r_bk, vx, w, nh, r_w, r_vx):
            psV = [banks[i][:, 0:nh * 64] for i in range(2)]
            r_psV = [r_bk[i] for i in range(2)]
            fw.op('pool', 'memset', ap=vx[:], constant=1.0, writes=[r_vx])
            for t in range(NT):
                b = t % 2
                tok = slice(t * 128, (t + 1) * 128)
                for k in range(KD):
                    fw.op('pe', 'matmul', out=psV[b], lhsT=hT[:, k, tok], rhs=w[:, k, 0:nh * 64], start=(k == 0), stop=(k == KD - 1),
                          reads=[r_w], writes=[r_psV[b]])
                for h in range(nh):
                    fw.op('act', 'activation', out=vx[:, t, h, 0:64], in_=psV[b][:, h * 64:(h + 1) * 64], func=AF.Copy,
                          reads=[r_psV[b]], writes=[r_vx])

        def load_rot(dst, src_cols, nh, res):
            sv = src_cols.rearrange("(k p) (h two f) -> p k h two f", p=128, two=2, f=32)
            dv = dst.rearrange("p k (h two f) -> p k h two f", two=2, f=32)
            for k in range(KD):
                fw.dma('pool', dv[:, k, :, 0, :], sv[:, k, :, 1, :], writes=[res])
                fw.dma('pool', dv[:, k, :, 1, :], sv[:, k, :, 0, :], writes=[res])

        def phase_rope_tables(seq):
            fw.begin()
            posi = fw.sb("posi", [64, S], I32)
            ang = fw.sb("ang", [64, S], F32)
            ang2 = fw.sb("ang2", [64, S], F32)
            ni = fw.sb("ni", [64, S], I32)
            nf = fw.sb("nf", [64, S], F32)
            invf = fw.sb("invf", [64, 1], F32)
            sgn = fw.sb("sgn", [64, 1], F32)
            r_cs = fw.res()
            fw.dma('sp', posi[:], pos_d[seq:seq + 1, :].partition_broadcast(64), writes=[r_cs])
            fw.op('pool', 'iota', out=invf[0:32, :], pattern=[[0, 1]], base=0, channel_multiplier=1,
                  allow_small_or_imprecise_dtypes=True, writes=[r_cs])
            fw.op('pool', 'iota', out=invf[32:64, :], pattern=[[0, 1]], base=0, channel_multiplier=1,
                  allow_small_or_imprecise_dtypes=True, reads=[r_cs], writes=[r_cs])
            fw.op('act', 'activation', out=invf[:], in_=invf[:], func=AF.Exp, scale=-math.log(10000.0) / 32.0, reads=[r_cs], writes=[r_cs])
            fw.op('dve', 'memset', ap=sgn[0:32, :], constant=-1.0, reads=[r_cs], writes=[r_cs])
            fw.op('dve', 'memset', ap=sgn[32:64, :], constant=1.0, reads=[r_cs], writes=[r_cs])
            fw.op('dve', 'tensor_copy', out=ang[:], in_=posi[:], reads=[r_cs], writes=[r_cs])
            fw.op('dve', 'tensor_scalar', out=ang[:], in0=ang[:], scalar1=invf[:, 0:1], scalar2=None, op0=ALU.mult, reads=[r_cs], writes=[r_cs])

            def sin_of(dst, shift, sign_ap):
                fw.op('dve', 'tensor_scalar', out=ang2[:], in0=ang[:], scalar1=shift, scalar2=None, op0=ALU.add, reads=[r_cs], writes=[r_cs])
                fw.op('dve', 'tensor_scalar', out=nf[:], in0=ang2[:], scalar1=1.0 / TWO_PI, scalar2=None, op0=ALU.mult, reads=[r_cs], writes=[r_cs])
                fw.op('dve', 'tensor_copy', out=ni[:], in_=nf[:], reads=[r_cs], writes=[r_cs])
                fw.op('dve', 'tensor_copy', out=nf[:], in_=ni[:], reads=[r_cs], writes=[r_cs])
                fw.op('dve', 'scalar_tensor_tensor', out=ang2[:], in0=nf[:], scalar=-TWO_PI, in1=ang2[:], op0=ALU.mult, op1=ALU.add,
                      reads=[r_cs], writes=[r_cs])
                fw.op('dve', 'tensor_scalar', out=nf[:], in0=ang2[:], scalar1=math.pi, scalar2=-TWO_PI, op0=ALU.is_gt, op1=ALU.mult,
                      reads=[r_cs], writes=[r_cs])
                fw.op('dve', 'tensor_tensor', out=ang2[:], in0=ang2[:], in1=nf[:], op=ALU.add, reads=[r_cs], writes=[r_cs])
                fw.op('dve', 'tensor_scalar', out=nf[:], in0=ang2[:], scalar1=-math.pi, scalar2=TWO_PI, op0=ALU.is_lt, op1=ALU.mult,
                      reads=[r_cs], writes=[r_cs])
                fw.op('dve', 'tensor_tensor', out=ang2[:], in0=ang2[:], in1=nf[:], op=ALU.add, reads=[r_cs], writes=[r_cs])
                fw.op('act', 'activation', out=dst[:], in_=ang2[:], func=AF.Sin, reads=[r_cs], writes=[r_cs])
                if sign_ap is not None:
                    fw.op('dve', 'tensor_scalar', out=dst[:], in0=dst[:], scalar1=sign_ap, scalar2=None, op0=ALU.mult, reads=[r_cs], writes=[r_cs])

            cos2 = fw.sb("cos2", [64, S], F32)
            sin2 = fw.sb("sin2", [64, S], F32)
            sin_of(cos2, math.pi / 2.0, None)
            sin_of(sin2, 0.0, sgn[:, 0:1])
            r_CS = fw.res()
            fw.dma('sp', CS[0], cos2[:], reads=[r_cs], writes=[r_CS])
            fw.dma('sp', CS[1], sin2[:], reads=[r_cs], writes=[r_CS])
            fw.end()

        def rot_copy(dst, src_t, r_w):
            sv = src_t.rearrange("p k (h two f) -> p (k h) two f", two=2, f=32)
            dv = dst.rearrange("p k (h two f) -> p (k h) two f", two=2, f=32)
            fw.op('pool', 'tensor_copy', out=dv[:, :, 0, :], in_=sv[:, :, 1, :], reads=[r_w], writes=[r_w])
            fw.op('pool', 'tensor_copy', out=dv[:, :, 1, :], in_=sv[:, :, 0, :], reads=[r_w], writes=[r_w])

        def phase_swa(l, grp):
            fw.begin()
            banks = [fw.ps("bk%d" % i, [128, 512]) for i in range(8)]
            r_bk = [fw.res(True) for _ in range(8)]
            wq = fw.sb("wq", [128, KD, 256], BF16)
            wqr = fw.sb("wqr", [128, KD, 256], BF16)
            wk = fw.sb("wk", [128, KD, 64], BF16)
            wkr = fw.sb("wkr", [128, KD, 64], BF16)
            wv = fw.sb("wv", [128, KD, 64], BF16)
            r_wq = fw.res()
            r_wk = fw.res()
            r_wv = fw.res()
            q0 = OFF_SQ + grp * 256
            k0 = OFF_SK + grp * 64
            v0 = OFF_SV + grp * 64
            load_w(wq[:], w_in[l, :, q0:q0 + 256], r_wq)
            load_w(wk[:], w_in[l, :, k0:k0 + 64], r_wk)
            load_w(wv[:], w_in[l, :, v0:v0 + 64], r_wv)
            rot_copy(wqr[:], wq[:], r_wq)
            rot_copy(wkr[:], wk[:], r_wk)
            cos2 = fw.sb("cos2", [64, S], F32)
            sin2 = fw.sb("sin2", [64, S], F32)
            esk = fw.sb("esk", [128, 8], F32)
            r_cs = fw.res()
            r_es = fw.res()
            fw.dma('sp', cos2[:], CS[0], writes=[r_cs])
            fw.dma('sp', sin2[:], CS[1], writes=[r_cs])
            fw.dma('sp', esk[:], swa_sink[l:l + 1, :].partition_broadcast(128), writes=[r_es])
            fw.op('act', 'activation', out=esk[:], in_=esk[:], func=AF.Exp, reads=[r_es], writes=[r_es])
            qT = fw.sb("qT", [64, 4, S], BF16)
            kT = fw.sb("kT", [64, 1, S], BF16)
            vx = fw.sb("vx", [128, NT, 1, 128], BF16)
            r_q = fw.res()
            r_k = fw.res()
            r_v = fw.res()
            proj_heads(banks, r_bk, kT, wk, 1, r_wk, r_k, rope=(cos2, sin2, r_cs), w_rot=wkr)
            proj_v(banks, r_bk, vx, wv, 1, r_wv, r_v)
            proj_heads(banks, r_bk, qT, wq, 4, r_wq, r_q, rope=(cos2, sin2, r_cs), w_rot=wqr)
            attention(banks, r_bk, 4, 64, qT, kT, lambda h: 0, vx, r_q, r_k, r_v, lambda i, h: None, r_es, True, (esk, grp * 4), (1, grp * 2))
            fw.end()

        def phase_fox_cum(l):
            fw.begin()
            wff = fw.sb("wff", [128, KD, 8], BF16)
            r_w = fw.res()
            load_w(wff[:], w_in[l, :, OFF_FF:OFF_FF + 8], r_w)
            bfb = fw.sb("bfb", [128, 8], F32)
            r_c = fw.res()
            fw.dma('sp', bfb[:], fox_bf[l:l + 1, :].partition_broadcast(128), writes=[r_c])
            psF = [fw.ps("psF%d" % i, [128, 8]) for i in range(2)]
            r_psF = [fw.res(True) for _ in range(2)]
            psC = [fw.ps("psC%d" % i, [128, 8]) for i in range(2)]
            r_psC = [fw.res(True) for _ in range(2)]
            psT = [fw.ps("psT%d" % i, [128, 8]) for i in range(2)]
            r_psT = [fw.res(True) for _ in range(2)]
            nls = fw.sb("nls", [128, NT, 8], F32)
            r_nls = [fw.res() for _ in range(NT)]
            r_cum = [fw.res() for _ in range(NT)]
            for t in range(NT):
                b = t % 2
                tok = slice(t * 128, (t + 1) * 128)
                for k in range(KD):
                    fw.op('pe', 'matmul', out=psF[b][:], lhsT=hT[:, k, tok], rhs=wff[:, k, :], start=(k == 0), stop=(k == KD - 1),
                          reads=[r_w], writes=[r_psF[b]])
                fw.op('dve', 'tensor_tensor', out=nls[:, t, :], in0=psF[b][:], in1=bfb[:], op=ALU.add, reads=[r_psF[b], r_c], writes=[r_nls[t]])
                fw.op('act', 'activation', out=nls[:, t, :], in_=nls[:, t, :], func=AF.Exp, scale=-1.0, reads=[r_nls[t]], writes=[r_nls[t]])
                fw.op('dve', 'tensor_scalar', out=nls[:, t, :], in0=nls[:, t, :], scalar1=1.0, scalar2=None, op0=ALU.add,
                      reads=[r_nls[t]], writes=[r_nls[t]])
                fw.op('act', 'activation', out=nls[:, t, :], in_=nls[:, t, :], func=AF.Ln, reads=[r_nls[t]], writes=[r_nls[t]])
                fw.op('pe', 'matmul', out=psC[b][:], lhsT=triu[:], rhs=nls[:, t, :], start=True, stop=True, reads=[r_nls[t]], writes=[r_psC[b]])
                fw.op('pe', 'matmul', out=psT[b][:], lhsT=onesf[:], rhs=nls[:, t, :], start=True, stop=True, reads=[r_nls[t]], writes=[r_psT[b]])
                if t == 0:
                    fw.op('dve', 'tensor_copy', out=ncum[:, t, :], in_=psC[b][:], reads=[r_psC[b]], writes=[r_cum[t]])
                    fw.op('dve', 'tensor_copy', out=tot[:, t, :], in_=psT[b][:], reads=[r_psT[b]], writes=[r_cum[t]])
                else:
                    fw.op('dve', 'tensor_tensor', out=ncum[:, t, :], in0=psC[b][:], in1=tot[:, t - 1, :], op=ALU.add,
                          reads=[r_psC[b], r_cum[t - 1]], writes=[r_cum[t]])
                    fw.op('dve', 'tensor_tensor', out=tot[:, t, :], in0=psT[b][:], in1=tot[:, t - 1, :], op=ALU.add,
                          reads=[r_psT[b], r_cum[t - 1]], writes=[r_cum[t]])
            r1 = fw.sb("r1", [128, NT, 8], F32)
            n8 = fw.sb("n8", [128, NT, 8], F32)
            r_h = fw.res()
            rr = [r_cum[NT - 1], r_h]
            fw.op('dve', 'memset', ap=KML[:, :, :, 3:6], constant=1.0, writes=[r_h])
            fw.op('dve', 'memset', ap=QML[:, :, :, 0:3], constant=8.0, reads=[r_h], writes=[r_h])
            fw.op('dve', 'tensor_copy', out=KML[:, :, :, 0], in_=ncum[:], reads=rr, writes=[r_h])
            fw.op('dve', 'tensor_tensor', out=r1[:], in0=ncum[:], in1=KML[:, :, :, 0], op=ALU.subtract, reads=rr, writes=[r_h])
            fw.op('dve', 'tensor_copy', out=KML[:, :, :, 1], in_=r1[:], reads=rr, writes=[r_h])
            fw.op('dve', 'tensor_tensor', out=r1[:], in0=r1[:], in1=KML[:, :, :, 1], op=ALU.subtract, reads=rr, writes=[r_h])
            fw.op('dve', 'tensor_copy', out=KML[:, :, :, 2], in_=r1[:], reads=rr, writes=[r_h])
            fw.op('dve', 'tensor_scalar', out=n8[:], in0=ncum[:], scalar1=-8.0, scalar2=None, op0=ALU.mult, reads=rr, writes=[r_h])
            fw.op('dve', 'tensor_copy', out=QML[:, :, :, 3], in_=n8[:], reads=rr, writes=[r_h])
            fw.op('dve', 'tensor_tensor', out=r1[:], in0=n8[:], in1=QML[:, :, :, 3], op=ALU.subtract, reads=rr, writes=[r_h])
            fw.op('dve', 'tensor_copy', out=QML[:, :, :, 4], in_=r1[:], reads=rr, writes=[r_h])
            fw.op('dve', 'tensor_tensor', out=r1[:], in0=r1[:], in1=QML[:, :, :, 4], op=ALU.subtract, reads=rr, writes=[r_h])
            fw.op('dve', 'tensor_copy', out=QML[:, :, :, 5], in_=r1[:], reads=rr, writes=[r_h])
            fw.end()

        def phase_fox(l, grp):
            h0 = grp * 4
            fw.begin()
            banks = [fw.ps("bk%d" % i, [128, 512]) for i in range(8)]
            r_bk = [fw.res(True) for _ in range(8)]
            wfq = fw.sb("wfq", [128, KD, 256], BF16)
            wfk = fw.sb("wfk", [128, KD, 256], BF16)
            wfv = fw.sb("wfv", [128, KD, 256], BF16)
            r_wq = fw.res()
            r_wk = fw.res()
            r_wv = fw.res()
            load_w(wfk[:], w_in[l, :, OFF_FK + h0 * 64:OFF_FK + h0 * 64 + 256], r_wk)
            load_w(wfv[:], w_in[l, :, OFF_FV + h0 * 64:OFF_FV + h0 * 64 + 256], r_wv)
            load_w(wfq[:], w_in[l, :, OFF_FQ + h0 * 64:OFF_FQ + h0 * 64 + 256], r_wq)
            qT = fw.sb("qT", [128, 4, S], BF16)
            kT = fw.sb("kT", [128, 4, S], BF16)
            vx = fw.sb("vx", [128, NT, 4, 128], BF16)
            r_q = fw.res()
            r_k = fw.res()
            r_ka = fw.res()
            r_v = fw.res()
            r_qa = fw.res()
            n = 0
            for (ML, dstT, r_a) in ((KML, kT, r_ka), (QML, qT, r_qa)):
                for h in range(4):
                    for tb in range(NB):
                        bb = 4 + n % 3
                        n += 1
                        for tt in range(4):
                            t = tb * 4 + tt
                            fw.op('pe', 'matmul', out=banks[bb][0:6, tt * 128:(tt + 1) * 128], lhsT=ML[:, t, h0 + h, :], rhs=ident[:],
                                  start=True, stop=True, writes=[r_bk[bb]])
                        fw.op('dve', 'tensor_copy', out=dstT[64:70, h, tb * 512:(tb + 1) * 512], in_=banks[bb][0:6, :],
                              reads=[r_bk[bb]], writes=[r_a])
            proj_heads(banks, r_bk, kT[0:64], wfk, 4, r_wk, r_k)
            proj_v(banks, r_bk, vx, wfv, 4, r_wv, r_v)
            proj_heads(banks, r_bk, qT[0:64], wfq, 4, r_wq, r_q)

            r_kk = fw.res()
            r_qq = fw.res()
            fw.op('pool', 'memset', ap=rec_dummy[:], constant=0.0, reads=[r_k, r_ka, r_q, r_qa], writes=[r_kk, r_qq])
            attention_blk(banks, r_bk, 4, 70, qT, kT, vx, r_qq, r_kk, r_v, (2, grp * 2))
            fw.end()

        def phase_gates(l):
            fw.begin()
            wG = [fw.sb("wG%d" % i, [128, KD, 512], BF16) for i in range(2)]
            r_wG = [fw.res() for _ in range(2)]
            psZ = [fw.ps("psZ%d" % i, [128, 512]) for i in range(4)]
            r_psZ = [fw.res(True) for _ in range(4)]
            sg = [fw.sb("sg%d" % i, [128, 512], BF16) for i in range(4)]
            r_sg = [fw.res() for _ in range(4)]
            r_SG = fw.res()
            n = 0
            for c in range(8):
                wb = c % 2
                load_w(wG[wb][:], w_in[l, :, OFF_G + c * 512:OFF_G + (c + 1) * 512], r_wG[wb])
                for t in range(NT):
                    b = n % 4
                    n += 1
                    tok = slice(t * 128, (t + 1) * 128)
                    for k in range(KD):
                        fw.op('pe', 'matmul', out=psZ[b][:], lhsT=hT[:, k, tok], rhs=wG[wb][:, k, :], start=(k == 0), stop=(k == KD - 1),
                              reads=[r_wG[wb]], writes=[r_psZ[b]])
                    fw.op('act', 'activation', out=sg[b][:], in_=psZ[b][:], func=AF.Sigmoid, reads=[r_psZ[b]], writes=[r_sg[b]])
                    fw.dma('sp', SG[tok, c * 512:(c + 1) * 512], sg[b][:], reads=[r_sg[b]], writes=[r_SG])
            fw.end()

        def phase_merge(l):
            fw.begin()
            wbr = fw.sb("wbr", [128, 16, D], BF16)
            wo = fw.sb("wo", [128, KD, D], BF16)
            r_w = fw.res()
            for n_ in range(4):
                fw.dma('pool', wbr[:, n_ * 4:(n_ + 1) * 4, :], w_branch[l, n_].rearrange("(k p) d -> p k d", p=128), writes=[r_w])
            load_w(wo[:], w_out[l], r_w)
            sgt = [fw.sb("sgt%d" % i, [128, 4096], BF16) for i in range(2)]
            r_sgt = [fw.res() for _ in range(2)]
            ot = [fw.sb("ot%d" % i, [128, 16, 128], BF16) for i in range(2)]
            r_ot = [fw.res() for _ in range(2)]
            psP = [fw.ps("psP%d" % i, [128, 512]) for i in range(3)]
            r_psP = [fw.res(True) for _ in range(3)]
            pT = fw.ps("pTm", [128, KD, 128], BF16)
            r_pT = fw.res(True)
            psY = [fw.ps("psY%d" % i, [128, 512]) for i in range(2)]
            r_psY = [fw.res(True) for _ in range(2)]
            mg = [fw.sb("mg%d" % i, [128, D], F32) for i in range(2)]
            r_mg = [fw.res() for _ in range(2)]
            tm = [fw.sb("tm%d" % i, [128, 512], F32) for i in range(2)]
            r_tm = [fw.res() for _ in range(2)]
            mb = [fw.sb("mb%d" % i, [128, D], BF16) for i in range(2)]
            r_mb = [fw.res() for _ in range(2)]
            mT = [fw.sb("mT%d" % i, [128, KD, 128], BF16) for i in range(2)]
            r_mT = [fw.res() for _ in range(2)]
            cnt = {'np': 0}

            def stageA(t):
                np_ = cnt['np']
                b = t % 2
                tok = slice(t * 128, (t + 1) * 128)
                fw.dma('sp', sgt[b][:], SG[tok, :], writes=[r_sgt[b]])
                for n_ in range(4):
                    fw.dma('sp', ot[b][:, n_ * 4:(n_ + 1) * 4, :], OT[n_, :, :, tok], writes=[r_ot[b]])
                for half in range(2):
                    hs = slice(half * 512, (half + 1) * 512)
                    for n_ in range(4):
                        pb = np_ % 3
                        np_ += 1
                        for k in range(4):
                            fw.op('pe', 'matmul', out=psP[pb][:], lhsT=ot[b][:, n_ * 4 + k, :], rhs=wbr[:, n_ * 4 + k, hs], start=(k == 0),
                                  stop=(k == 3), reads=[r_ot[b], r_w], writes=[r_psP[pb]])
                        gsl = slice(n_ * 1024 + half * 512, n_ * 1024 + (half + 1) * 512)
                        if n_ == 0:
                            fw.op('dve', 'tensor_tensor', out=mg[b][:, hs], in0=psP[pb][:], in1=sgt[b][:, gsl], op=ALU.mult,
                                  reads=[r_psP[pb], r_sgt[b]], writes=[r_mg[b]])
                        else:
                            tb = np_ % 2
                            fw.op('dve', 'tensor_tensor', out=tm[tb][:], in0=psP[pb][:], in1=sgt[b][:, gsl], op=ALU.mult,
                                  reads=[r_psP[pb], r_sgt[b]], writes=[r_tm[tb]])
                            if n_ < 3:
                                fw.op('dve', 'tensor_tensor', out=mg[b][:, hs], in0=mg[b][:, hs], in1=tm[tb][:], op=ALU.add,
                                      reads=[r_tm[tb], r_mg[b]], writes=[r_mg[b]])
                            else:
                                fw.op('dve', 'tensor_tensor', out=mb[b][:, hs], in0=mg[b][:, hs], in1=tm[tb][:], op=ALU.add,
                                      reads=[r_tm[tb], r_mg[b]], writes=[r_mb[b]])
                cnt['np'] = np_

            def stageB(t):
                b = t % 2
                for k in range(KD):
                    fw.op('pe', 'transpose', out=pT[:, k, :], in_=mb[b][:, k * 128:(k + 1) * 128], identity=ident[:],
                          reads=[r_mb[b]], writes=[r_pT])
                fw.op('act', 'activation', out=mT[b][:], in_=pT[:], func=AF.Copy, reads=[r_pT], writes=[r_mT[b]])
                for half in range(2):
                    hs = slice(half * 512, (half + 1) * 512)
                    for k in range(KD):
                        fw.op('pe', 'matmul', out=psY[half][:], lhsT=mT[b][:, k, :], rhs=wo[:, k, hs], start=(k == 0), stop=(k == KD - 1),
                              reads=[r_mT[b], r_w], writes=[r_psY[half]])
                    fw.op('dve', 'tensor_tensor', out=xs[:, t, hs], in0=psY[half][:], in1=xs[:, t, hs], op=ALU.add,
                          reads=[r_psY[half], r_xs[t]], writes=[r_xs[t]])
            stageA(0)
            for t in range(1, NT):
                stageA(t)
                stageB(t - 1)
            stageB(NT - 1)
            fw.end()

        def phase_ffn(wg_d, wu_d, wd_d, ne, gated):
            fw.begin()
            wg = [fw.sb("wg%d" % i, [128, KD, 512], BF16) for i in range(2)]
            wu = [fw.sb("wu%d" % i, [128, KD, 512], BF16) for i in range(2)]
            wd = [fw.sb("wd%d" % i, [128, 4, D], BF16) for i in range(2)]
            r_wgt = [fw.res() for _ in range(2)]
            psG = [fw.ps("fpG%d" % i, [128, 512]) for i in range(2)]
            psU = [fw.ps("fpU%d" % i, [128, 512]) for i in range(2)]
            psY = [fw.ps("fpY%d" % i, [128, 512]) for i in range(3)]
            r_psG = [fw.res(True) for _ in range(2)]
            r_psU = [fw.res(True) for _ in range(2)]
            r_psY = [fw.res(True) for _ in range(3)]
            sl = [fw.sb("sl%d" % i, [128, 512], F32) for i in range(2)]
            r_sl = [fw.res() for _ in range(2)]
            act = [fw.sb("act%d" % i, [128, 4, 512], BF16) for i in range(2)]
            r_act = [fw.res() for _ in range(2)]
            nw = 0
            nc_ = 0
            nb_ = 0
            ny = 0
            for e in range(ne):
                for r in range(NR):
                    w = nw % 2
                    nw += 1
                    fs = slice(r * 512, (r + 1) * 512)
                    load_w(wg[w][:], wg_d[e, :, fs], r_wgt[w])
                    load_w(wu[w][:], wu_d[e, :, fs], r_wgt[w])
                    fw.dma('pool', wd[w][:], wd_d[e, fs, :].rearrange("(k p) d -> p k d", p=128), writes=[r_wgt[w]])
                    for bk in range(NB):
                        ab = nb_ % 2
                        nb_ += 1
                        tokb = slice(bk * 512, (bk + 1) * 512)
                        for c in range(4):
                            pb = nc_ % 2
                            nc_ += 1
                            cs = slice(c * 128, (c + 1) * 128)
                            for k in range(KD):
                                fw.op('pe', 'matmul', out=psG[pb][:], lhsT=wg[w][:, k, cs], rhs=hT[:, k, tokb], start=(k == 0), stop=(k == KD - 1),
                                      reads=[r_wgt[w]], writes=[r_psG[pb]])
                            for k in range(KD):
                                fw.op('pe', 'matmul', out=psU[pb][:], lhsT=wu[w][:, k, cs], rhs=hT[:, k, tokb], start=(k == 0), stop=(k == KD - 1),
                                      reads=[r_wgt[w]], writes=[r_psU[pb]])
                            fw.op('act', 'activation', out=sl[pb][:], in_=psG[pb][:], func=AF.Silu, reads=[r_psG[pb]], writes=[r_sl[pb]])
                            fw.op('dve', 'tensor_tensor', out=act[ab][:, c, :], in0=psU[pb][:], in1=sl[pb][:], op=ALU.mult,
                                  reads=[r_psU[pb], r_sl[pb]], writes=[r_act[ab]])
                        for tt in range(4):
                            t = bk * 4 + tt
                            for half in range(2):
                                yb = ny % 3
                                ny += 1
                                hs = slice(half * 512, (half + 1) * 512)
                                for c in range(4):
                                    fw.op('pe', 'matmul', out=psY[yb][:], lhsT=act[ab][:, c, tt * 128:(tt + 1) * 128], rhs=wd[w][:, c, hs],
                                          start=(c == 0), stop=(c == 3), reads=[r_act[ab], r_wgt[w]], writes=[r_psY[yb]])
                                if gated:
                                    fw.op('dve', 'scalar_tensor_tensor', out=xs[:, t, hs], in0=psY[yb][:], scalar=Gt[:, t * 8 + e:t * 8 + e + 1], in1=xs[:, t, hs],
                                          op0=ALU.mult, op1=ALU.add, reads=[r_psY[yb], r_xs[t]], writes=[r_xs[t]])
                                else:
                                    fw.op('dve', 'tensor_tensor', out=xs[:, t, hs], in0=psY[yb][:], in1=xs[:, t, hs], op=ALU.add,
                                          reads=[r_psY[yb], r_xs[t]], writes=[r_xs[t]])
            fw.end()

        def phase_stash(seq):
            fw.begin()
            for t in range(NT):
                fw.dma(('sp', 'act')[t % 2], XS[seq, t * 128:(t + 1) * 128, :], xs[:, t, :], writes=[fw.res()])
            fw.end()

        def phase_route():
            fw.begin()
            X_ = mybir.AxisListType.X
            W8 = NTT * 8
            v3 = lambda ap: ap.rearrange("p (t e) -> p t e", e=8)
            Mb = fw.sb("Mb", [128, W8], BF16)
            onesb = fw.sb("onesb", [128, 128], BF16)
            r_c = fw.res()
            fw.op('pool', 'memset', ap=onesb[:], constant=1.0, writes=[r_c])
            r_M = fw.res()
            fw.op('dve', 'tensor_copy', out=Mb[:], in_=Mf[:], writes=[r_M])
            ps_rank = fw.ps("ps_rank", [128, W8])
            ps_tot = fw.ps("ps_tot", [128, W8])
            r_pr = fw.res(True)
            r_pt = fw.res(True)
            fw.op('pe', 'matmul', out=ps_rank[:], lhsT=mdiag[:], rhs=Mb[:], start=True, stop=True, reads=[r_M], writes=[r_pr])
            fw.op('pe', 'matmul', out=ps_tot[:], lhsT=onesb[:], rhs=Mb[:], start=True, stop=True, reads=[r_M, r_c], writes=[r_pt])
            tot_sb = fw.sb("tot_sb", [128, W8], F32)
            tp = fw.sb("tp", [128, W8 + 8], F32)
            r_t = fw.res()
            D_ = lambda name, **kw: fw.op('dve', name, reads=[r_t], writes=[r_t], **kw)
            fw.op('dve', 'tensor_copy', out=tot_sb[:], in_=ps_tot[:], reads=[r_pt], writes=[r_t])
            D_('memset', ap=tp[:, 0:8], constant=0.0)
            for tt in range(NTT):
                D_('tensor_tensor', out=tp[:, (tt + 1) * 8:(tt + 2) * 8], in0=tp[:, tt * 8:(tt + 1) * 8],
                   in1=tot_sb[:, tt * 8:(tt + 1) * 8], op=ALU.add)
            cnt = tp[:, W8:W8 + 8]
            nb = fw.sb("nb", [128, 8], F32)
            tmp8 = fw.sb("tmp8", [128, 8], F32)
            D_('memset', ap=nb[:], constant=0.0)
            for j in range(JMAX):
                D_('tensor_scalar', out=tmp8[:], in0=cnt, scalar1=float(j * BS), scalar2=None, op0=ALU.is_gt)
                D_('tensor_tensor', out=nb[:], in0=nb[:], in1=tmp8[:], op=ALU.add)
            bend = fw.sb("bend", [128, 8], F32)
            off = fw.sb("off", [128, 8], F32)
            D_('tensor_copy', out=bend[:, 0:1], in_=nb[:, 0:1])
            for e in range(1, 8):
                D_('tensor_tensor', out=bend[:, e:e + 1], in0=bend[:, e - 1:e], in1=nb[:, e:e + 1], op=ALU.add)
            D_('tensor_tensor', out=off[:], in0=bend[:], in1=nb[:], op=ALU.subtract)
            D_('tensor_scalar', out=off[:], in0=off[:], scalar1=float(BS), scalar2=None, op0=ALU.mult)
            slot = fw.sb("slot", [128, W8], F32)
            slotM = fw.sb("slotM", [128, W8], F32)
            valA = fw.sb("valA", [128, W8], F32)
            fw.op('dve', 'tensor_tensor', out=slot[:], in0=ps_rank[:], in1=Mf[:], op=ALU.subtract, reads=[r_pr, r_t], writes=[r_t])
            D_('tensor_tensor', out=slot[:], in0=slot[:], in1=tp[:, 0:W8], op=ALU.add)
            for tt in range(NTT):
                D_('tensor_tensor', out=slot[:, tt * 8:(tt + 1) * 8], in0=slot[:, tt * 8:(tt + 1) * 8], in1=off[:], op=ALU.add)
            D_('tensor_tensor', out=slotM[:], in0=slot[:], in1=Mf[:], op=ALU.mult)
            D_('tensor_scalar', out=valA[:], in0=Mf[:], scalar1=-1.0e6, scalar2=1.0e6, op0=ALU.mult, op1=ALU.add)
            D_('tensor_tensor', out=valA[:], in0=valA[:], in1=slot[:], op=ALU.add)
            sAf = fw.sb("sAf", [128, NTT], F32)
            sBf = fw.sb("sBf", [128, NTT], F32)
            eq = fw.sb("eq", [128, NTT], F32)
            D_('tensor_reduce', out=sAf[:], in_=v3(valA[:]), axis=X_, op=ALU.min)
            D_('tensor_reduce', out=sBf[:], in_=v3(slotM[:]), axis=X_, op=ALU.max)
            D_('tensor_copy', out=sAi[:], in_=sAf[:])
            D_('tensor_copy', out=sBi[:], in_=sBf[:])
            D_('tensor_reduce', out=gA[:], in_=v3(Gt[:]), axis=X_, op=ALU.add)
            D_('memset', ap=gB[:], constant=0.0)
            for e in range(NE):
                D_('tensor_tensor', out=eq[:], in0=v3(slotM[:])[:, :, e], in1=sBf[:], op=ALU.is_equal)
                D_('tensor_tensor', out=eq[:], in0=eq[:], in1=v3(Gt[:])[:, :, e], op=ALU.mult)
                D_('tensor_tensor', out=gB[:], in0=gB[:], in1=eq[:], op=ALU.add)
            D_('tensor_tensor', out=gA[:], in0=gA[:], in1=gB[:], op=ALU.subtract)
            jidx = fw.sb("jidx", [128, NBLK], F32)
            ej = fw.sb("ej", [128, NBLK], F32)
            tmpj = fw.sb("tmpj", [128, NBLK], F32)
            base14 = fw.sb("base14", [128, NR2], F32)
            idxf = fw.sb("idxf", [128, NBLK * NR2], F32)
            r_i = fw.res()
            fw.op('pool', 'iota', out=jidx[:], pattern=[[1, NBLK]], base=0, channel_multiplier=0,
                  allow_small_or_imprecise_dtypes=True, writes=[r_i])
            fw.op('pool', 'iota', out=base14[:], pattern=[[1, NR2]], base=0, channel_multiplier=NR2,
                  allow_small_or_imprecise_dtypes=True, writes=[r_i])
            D_('memset', ap=ej[:], constant=0.0)
            for e in range(NE):
                fw.op('dve', 'tensor_scalar', out=tmpj[:], in0=jidx[:], scalar1=bend[:, e:e + 1], scalar2=None, op0=ALU.is_ge,
                      reads=[r_t, r_i], writes=[r_t])
                D_('tensor_tensor', out=ej[:], in0=ej[:], in1=tmpj[:], op=ALU.add)
            D_('tensor_scalar', out=ej[:], in0=ej[:], scalar1=float(NE - 1), scalar2=float(128 * NR2), op0=ALU.min, op1=ALU.mult)
            for j in range(NBLK):
                fw.op('dve', 'tensor_scalar', out=idxf[:, j * NR2:(j + 1) * NR2], in0=base14[:], scalar1=ej[:, j:j + 1], scalar2=None,
                      op0=ALU.add, reads=[r_t, r_i], writes=[r_t])
            D_('tensor_copy', out=idxw[:], in_=idxf[:])
            fw.end()

        def phase_scatter():
            fw.begin()
            r_z = fw.res()
            fw.op('pool', 'memset', ap=hT[:], constant=0.0, writes=[r_z])
            rows_pp = NBLK * BS // 128
            hs_v = HS.rearrange("(p n) d -> p n d", p=128)
            zrows = (KD * S) // D
            hT_v = hT[:].rearrange("p k s -> p (k s)").rearrange("p (a d) -> p a d", d=D)
            n0 = 0
            qi = 0
            r_HS = fw.res()
            while n0 < rows_pp:
                n1 = min(rows_pp, n0 + zrows)
                fw.dma(('sp', 'act')[qi % 2], hs_v[:, n0:n1, :], hT_v[:, 0:n1 - n0, :], reads=[r_z], writes=[r_HS])
                qi += 1
                n0 = n1
            hb = [fw.sb("hb%d" % i, [128, D], BF16) for i in range(8)]
            r_hb = [fw.res() for _ in range(8)]
            for tt in range(NTT):
                b = tt % 8
                fw.dma('sp', hb[b][:], HTK[tt * 128:(tt + 1) * 128, :], writes=[r_hb[b]])
                for sI in (sAi, sBi):
                    fw.dma('pool', HS[:, :], hb[b][:], reads=[r_hb[b], r_HS], writes=[fw.res()], _meth='indirect_dma_start',
                           out_offset=IOA(ap=sI[:, tt:tt + 1], axis=0), in_offset=None)
            fw.end()

        def phase_moe_sparse():
            fw.begin()
            NWB = 3
            wg = [fw.sb("wg%d" % i, [128, KD, 512], BF16) for i in range(NWB)]
            wu = [fw.sb("wu%d" % i, [128, KD, 512], BF16) for i in range(NWB)]
            wd = [fw.sb("wd%d" % i, [128, 4, D], BF16) for i in range(NWB)]
            r_wgt = [fw.res() for _ in range(NWB)]
            psG = [fw.ps("fpG%d" % i, [128, 512]) for i in range(2)]
            psU = [fw.ps("fpU%d" % i, [128, 512]) for i in range(2)]
            psY = [fw.ps("fpY%d" % i, [128, 512]) for i in range(3)]
            pT = fw.ps("spT", [128, KD, 128], BF16)
            r_psG = [fw.res(True) for _ in range(2)]
            r_psU = [fw.res(True) for _ in range(2)]
            r_psY = [fw.res(True) for _ in range(3)]
            r_pT = fw.res(True)
            sl = [fw.sb("sl%d" % i, [128, 512], F32) for i in range(2)]
            r_sl = [fw.res() for _ in range(2)]
            act = [fw.sb("act%d" % i, [128, 4, 512], BF16) for i in range(2)]
            r_act = [fw.res() for _ in range(2)]
            hsb = [fw.sb("hsb%d" % i, [128, D], BF16) for i in range(SB)]
            r_hsb = [fw.res() for _ in range(SB)]
            r_hTs = [fw.res() for _ in range(2)]
            r_ys = [[fw.res() for _ in range(SB)] for _ in range(2)]
            nw = 0
            nc_ = 0
            nb_ = 0
            ny = 0
            nh = 0
            NTB = BS // TBW
            TPB = TBW // 128

            def prep_load(j):
                for st in range(SB):
                    fw.dma(('sp', 'act')[st % 2], hsb[st][:], HS[j * BS + st * 128:j * BS + (st + 1) * 128, :], writes=[r_hsb[st]])

            def prep_tr(j):
                c0_ = (j % 2) * BS
                for st in range(SB):
                    for k in range(KD):
                        fw.op('pe', 'transpose', out=pT[:, k, :], in_=hsb[st][:, k * 128:(k + 1) * 128], identity=ident[:],
                              reads=[r_hsb[st]], writes=[r_pT])
                    if st % 2 == 0:
                        fw.op('dve', 'tensor_copy', out=hT[:, :, c0_ + st * 128:c0_ + (st + 1) * 128], in_=pT[:],
                              reads=[r_pT], writes=[r_hTs[j % 2]])
                    else:
                        fw.op('act', 'activation', out=hT[:, :, c0_ + st * 128:c0_ + (st + 1) * 128], in_=pT[:], func=AF.Copy,
                              reads=[r_pT], writes=[r_hTs[j % 2]])

            prep_load(0)
            prep_tr(0)
            for j in range(NBLK):
                jb = j % 2
                c0 = jb * BS
                for r in range(NR):
                    if j + 1 < NBLK:
                        if r == max(NR - 2, 0):
                            prep_load(j + 1)
                        if r == NR - 1:
                            prep_tr(j + 1)
                    w = nw % NWB
                    nw += 1
                    for h in range(2):
                        ic = (j * NR + r) * 2 + h
                        io = IOA(ap=idxw[:, ic:ic + 1], axis=0)
                        hsl = slice(h * 2048, (h + 1) * 2048)
                        fw.dma('pool', wg[w][:].rearrange("p k f -> p (k f)")[:, hsl], exp_w_gate[:, :], writes=[r_wgt[w]],
                               _meth='indirect_dma_start', out_offset=None, in_offset=io)
                        fw.dma('pool', wu[w][:].rearrange("p k f -> p (k f)")[:, hsl], exp_w_up[:, :], writes=[r_wgt[w]],
                               _meth='indirect_dma_start', out_offset=None, in_offset=io)
                        fw.dma('pool', wd[w][:].rearrange("p c d -> p (c d)")[:, hsl], exp_w_down[:, :], writes=[r_wgt[w]],
                               _meth='indirect_dma_start', out_offset=None, in_offset=io)
                    for tb in range(NTB):
                        ab = nb_ % 2
                        nb_ += 1
                        tokb = slice(c0 + tb * TBW, c0 + (tb + 1) * TBW)
                        for c in range(4):
                            pb = nc_ % 2
                            nc_ += 1
                            cs = slice(c * 128, (c + 1) * 128)
                            for k in range(KD):
                                fw.op('pe', 'matmul', out=psG[pb][:, 0:TBW], lhsT=wg[w][:, k, cs], rhs=hT[:, k, tokb], start=(k == 0), stop=(k == KD - 1),
                                      reads=[r_wgt[w], r_hTs[jb]], writes=[r_psG[pb]])
                            for k in range(KD):
                                fw.op('pe', 'matmul', out=psU[pb][:, 0:TBW], lhsT=wu[w][:, k, cs], rhs=hT[:, k, tokb], start=(k == 0), stop=(k == KD - 1),
                                      reads=[r_wgt[w], r_hTs[jb]], writes=[r_psU[pb]])
                            fw.op('act', 'activation', out=sl[pb][:, 0:TBW], in_=psG[pb][:, 0:TBW], func=AF.Silu, reads=[r_psG[pb]], writes=[r_sl[pb]])
                            fw.op('dve', 'tensor_tensor', out=act[ab][:, c, 0:TBW], in0=psU[pb][:, 0:TBW], in1=sl[pb][:, 0:TBW], op=ALU.mult,
                                  reads=[r_psU[pb], r_sl[pb]], writes=[r_act[ab]])
                        for tt in range(TPB):
                            st = tb * TPB + tt
                            for half in range(2):
                                yb = ny % 3
                                ny += 1
                                hs = slice(half * 512, (half + 1) * 512)
                                for c in range(4):
                                    fw.op('pe', 'matmul', out=psY[yb][:], lhsT=act[ab][:, c, tt * 128:(tt + 1) * 128], rhs=wd[w][:, c, hs],
                                          start=(c == 0), stop=(c == 3), reads=[r_act[ab], r_wgt[w]], writes=[r_psY[yb]])
                                dst = xs[:, jb * SB + st, hs]
                                if r == 0:
                                    fw.op('act', 'activation', out=dst, in_=psY[yb][:], func=AF.Copy, reads=[r_psY[yb]], writes=[r_ys[jb][st]])
                                else:
                                    fw.op('dve', 'tensor_tensor', out=dst, in0=psY[yb][:], in1=dst, op=ALU.add,
                                          reads=[r_psY[yb], r_ys[jb][st]], writes=[r_ys[jb][st]])
                fw.dma('sp', YS[j * BS:(j + 1) * BS, :].rearrange("(st p) d -> p st d", p=128), xs[:, jb * SB:(jb + 1) * SB, :],
                       reads=r_ys[jb], writes=[fw.res()])
            fw.end()

        def phase_final2(seq):
            fw.begin()
            gfb = fw.sb("gfb", [128, D], F32)
            r_g = fw.res()
            fw.dma('sp', gfb[:], norm_final_g[0:1, :].partition_broadcast(128), writes=[r_g])
            ss = fw.sb("ss", [128, NT], F32)
            junk = [fw.sb("junk%d" % i, [128, D], BF16) for i in range(2)]
            yo = [fw.sb("yo%d" % i, [128, D], F32) for i in range(2)]
            NG = 6
            xt = [fw.sb("xt%d" % i, [128, D], F32) for i in range(NG)]
            ya = [fw.sb("ya%d" % i, [128, D], F32) for i in range(NG)]
            yb_ = [fw.sb("yb%d" % i, [128, D], F32) for i in range(NG)]
            r_j = [fw.res() for _ in range(2)]
            r_y = [fw.res() for _ in range(2)]
            r_xt = [fw.res() for _ in range(NG)]
            r_ya = [fw.res() for _ in range(NG)]
            r_yb = [fw.res() for _ in range(NG)]
            r_o = fw.res()
            def loads(t):
                g_ = t % NG
                tt = seq * NT + t
                fw.dma(('sp', 'act')[t % 2], xt[g_][:], XS[seq, t * 128:(t + 1) * 128, :], writes=[r_xt[g_]])
                fw.dma('pool', ya[g_][:], YS[:, :], writes=[r_ya[g_]], _meth='indirect_dma_start', out_offset=None,
                       in_offset=IOA(ap=sAi[:, tt:tt + 1], axis=0))
                fw.dma('pool', yb_[g_][:], YS[:, :], writes=[r_yb[g_]], _meth='indirect_dma_start', out_offset=None,
                       in_offset=IOA(ap=sBi[:, tt:tt + 1], axis=0))

            for t in range(min(NG - 1, NT)):
                loads(t)
            for t in range(NT):
                if t + NG - 1 < NT:
                    loads(t + NG - 1)
                b = t % 2
                g_ = t % NG
                tt = seq * NT + t
                r_s = fw.res()
                fw.op('dve', 'scalar_tensor_tensor', out=xt[g_][:], in0=ya[g_][:], scalar=gA[:, tt:tt + 1], in1=xt[g_][:],
                      op0=ALU.mult, op1=ALU.add, reads=[r_ya[g_], r_xt[g_]], writes=[r_xt[g_]])
                fw.op('dve', 'scalar_tensor_tensor', out=xt[g_][:], in0=yb_[g_][:], scalar=gB[:, tt:tt + 1], in1=xt[g_][:],
                      op0=ALU.mult, op1=ALU.add, reads=[r_yb[g_], r_xt[g_]], writes=[r_xt[g_]])
                fw.op('act', 'activation', out=junk[b][:], in_=xt[g_][:], func=AF.Square, accum_out=ss[:, t:t + 1],
                      reads=[r_xt[g_]], writes=[r_j[b], r_s])
                fw.op('act', 'activation', out=ss[:, t:t + 1], in_=ss[:, t:t + 1], func=AF.Ln, scale=1.0 / D, bias=epsc[:, 0:1],
                      reads=[r_s], writes=[r_s])
                fw.op('act', 'activation', out=ss[:, t:t + 1], in_=ss[:, t:t + 1], func=AF.Exp, scale=-0.5, reads=[r_s], writes=[r_s])
                fw.op('act', 'activation', out=yo[b][:], in_=xt[g_][:], func=AF.Copy, scale=ss[:, t:t + 1], reads=[r_s, r_xt[g_]], writes=[r_y[b]])
                fw.op('dve', 'tensor_tensor', out=yo[b][:], in0=yo[b][:], in1=gfb[:], op=ALU.mult, reads=[r_y[b], r_g], writes=[r_y[b]])
                fw.dma('sp', out_d[seq, t * 128:(t + 1) * 128, :], yo[b][:], reads=[r_y[b]], writes=[r_o])
            fw.end()

        def phase_load_x(seq):
            fw.begin()
            for t in range(NT):
                fw.dma('sp', xs[:, t, :], x_d[seq, t * 128:(t + 1) * 128, :], writes=[fw.res()])
            fw.end()

        def phase_final(seq):
            fw.begin()
            gfb = fw.sb("gfb", [128, D], F32)
            r_g = fw.res()
            fw.dma('sp', gfb[:], norm_final_g[0:1, :].partition_broadcast(128), writes=[r_g])
            ss = fw.sb("ss", [128, NT], F32)
            junk = [fw.sb("junk%d" % i, [128, D], BF16) for i in range(2)]
            yo = [fw.sb("yo%d" % i, [128, D], F32) for i in range(2)]
            r_j = [fw.res() for _ in range(2)]
            r_y = [fw.res() for _ in range(2)]
            r_o = fw.res()
            for t in range(NT):
                b = t % 2
                r_s = fw.res()
                fw.op('act', 'activation', out=junk[b][:], in_=xs[:, t, :], func=AF.Square, accum_out=ss[:, t:t + 1], writes=[r_j[b], r_s])
                fw.op('act', 'activation', out=ss[:, t:t + 1], in_=ss[:, t:t + 1], func=AF.Ln, scale=1.0 / D, bias=epsc[:, 0:1],
                      reads=[r_s], writes=[r_s])
                fw.op('act', 'activation', out=ss[:, t:t + 1], in_=ss[:, t:t + 1], func=AF.Exp, scale=-0.5, reads=[r_s], writes=[r_s])
                fw.op('act', 'activation', out=yo[b][:], in_=xs[:, t, :], func=AF.Copy, scale=ss[:, t:t + 1], reads=[r_s], writes=[r_y[b]])
                fw.op('dve', 'tensor_tensor', out=yo[b][:], in0=yo[b][:], in1=gfb[:], op=ALU.mult, reads=[r_y[b], r_g], writes=[r_y[b]])
                fw.dma('sp', out_d[seq, t * 128:(t + 1) * 128, :], yo[b][:], reads=[r_y[b]], writes=[r_o])
            fw.end()

        for seq in range(NSEQ):
            r_xs = [Res() for _ in range(NT)]
            phase_load_x(seq)
            phase_rope_tables(seq)
            for l in range(DEPTH):
                for r_ in r_xs:
                    r_.w = None
                    r_.rd = []
                phase_norm(norm_mix_g[l])
                phase_gmlp(l)
                phase_conv(l)
                phase_swa(l, 0)
                phase_swa(l, 1)
                phase_fox_cum(l)
                phase_fox(l, 0)
                phase_fox(l, 1)
                phase_gates(l)
                for r_ in r_xs:
                    r_.w = None
                    r_.rd = []
                phase_merge(l)
                for r_ in r_xs:
                    r_.w = None
                    r_.rd = []
                j = l // 2
                if l % 2 == 0:
                    phase_norm(norm_ffn_g[l])
                    for r_ in r_xs:
                        r_.w = None
                        r_.rd = []
                    phase_ffn(ffn_w_gate[j:j + 1], ffn_w_up[j:j + 1], ffn_w_down[j:j + 1], 1, False)
                elif sparse:
                    phase_norm(norm_ffn_g[l], moe_router=router_w[j], seq=seq, g_row2=norm_ffn_g[l:l + 1, :])
                    phase_stash(seq)
                else:
                    phase_norm(norm_ffn_g[l], moe_router=router_w[j])
                    for r_ in r_xs:
                        r_.w = None
                        r_.rd = []
                    phase_ffn(exp_w_gate[j], exp_w_up[j], exp_w_down[j], NE, True)
            for r_ in r_xs:
                r_.w = None
                r_.rd = []
            if not sparse:
                phase_final(seq)
        if sparse:
            LV = 9
            if LV >= 1:
                phase_route()
            if LV >= 2:
                phase_scatter()
            if LV >= 3:
                phase_moe_sparse()
            if LV >= 4:
                for seq in range(NSEQ):
                    phase_final2(seq)
        print("built: n_inst", fw.n_inst, "n_wait", fw.n_wait)
    return nc


_NC_CACHE = {}

_PARAMS = ['norm_mix_g', 'w_in', 'gmlp_ln_g', 'gmlp_ln_b', 'gmlp_ws', 'gmlp_bs', 'swa_sink', 'fox_bf', 'conv_w', 'conv_b',
           'conv_ln_g', 'conv_ln_b', 'w_branch', 'w_out', 'norm_ffn_g', 'ffn_w_gate', 'ffn_w_up', 'ffn_w_down', 'router_w',
           'exp_w_gate', 'exp_w_up', 'exp_w_down']


def relayout_experts(wg, wu, wd):
    NE, D_, DFF = wg.shape
    NR = DFF // 512

    def gu(w):
        a = np.asarray(w, dtype=np.float32).reshape(NE, 8, 128, NR, 512).transpose(0, 2, 3, 1, 4)
        return np.ascontiguousarray(a).reshape(NE * 128 * NR * 2, 2048)

    b = np.asarray(wd, dtype=np.float32).reshape(NE, NR, 4, 128, D_).transpose(0, 3, 1, 2, 4)
    return gu(wg), gu(wu), np.ascontiguousarray(b).reshape(NE * 128 * NR * 2, 2048)


def kernel(**inputs):
    n = 8
    x = np.ascontiguousarray(np.asarray(inputs['x'], dtype=np.float32))
    pos = np.ascontiguousarray(np.asarray(inputs['positions'], dtype=np.int32))
    B, S, _ = x.shape
    per = B // n
    if 'nc' not in _NC_CACHE:
        _NC_CACHE['nc'] = build(NSEQ=per, S=S)
    nc = _NC_CACHE['nc']
    shared = {k: np.ascontiguousarray(np.asarray(inputs[k], dtype=np.float32)) for k in _PARAMS}
    shared['norm_final_g'] = np.ascontiguousarray(np.asarray(inputs['norm_final_g'], dtype=np.float32)).reshape(1, -1)
    shared['exp_w_gate'], shared['exp_w_up'], shared['exp_w_down'] = relayout_experts(
        shared['exp_w_gate'][0], shared['exp_w_up'][0], shared['exp_w_down'][0])
    in_maps = []
    for c in range(n):
        m = dict(shared)
        m['x'] = x[c * per:(c + 1) * per]
        m['positions'] = pos[c * per:(c + 1) * per]
        in_maps.append(m)
    res = run_bass_kernel_spmd(nc, in_maps, core_ids=list(range(n)))
    return np.concatenate([r['out'] for r in res.results], axis=0).astype(np.float32)
```

```python
import math
import numpy as np
from contextlib import ExitStack
import concourse.bass as bass
import concourse.mybir as mybir
from concourse.bass_utils import run_bass_kernel_spmd

F32 = mybir.dt.float32
BF16 = mybir.dt.bfloat16
I32 = mybir.dt.int32
AF = mybir.ActivationFunctionType
ALU = mybir.AluOpType

CE = ('pe', 'act', 'dve', 'pool')
NSLOT = 8


class Res:
    __slots__ = ('w', 'rd', 'psum', 'extra')

    def __init__(self, psum=False):
        self.w = None
        self.rd = []
        self.psum = psum
        self.extra = []


class Tok:
    __slots__ = ('key', 'val', 'clock')

    def __init__(self, key, val, clock):
        self.key = key
        self.val = val
        self.clock = clock


class FW:
    def __init__(self, nc, es):
        self.nc = nc
        self.eng = ('pe', 'act', 'dve', 'pool', 'sp')
        self.sem = {}
        self.cnt = {}
        for e in CE:
            self.sem[e] = es.enter_context(nc.semaphore('c_' + e))
            self.cnt[e] = 0
        self.dq = ('sp', 'act', 'pool')
        self.slot_i = {q: 0 for q in self.dq}
        self.nslot = {'sp': NSLOT, 'act': NSLOT, 'pool': 2 * NSLOT}
        for q in self.dq:
            for s in range(self.nslot[q]):
                k = 'd_%s_%d' % (q, s)
                self.sem[k] = es.enter_context(nc.semaphore(k))
                self.cnt[k] = 0
        self.seen = {e: {} for e in self.eng}
        self.prog = {e: [] for e in self.eng}
        self.all_res = []
        self.pes = None
        self.n_inst = 0
        self.n_wait = 0
        self.uid = 0
        self.cond = None

    def res(self, psum=False):
        r = Res(psum)
        self.all_res.append(r)
        return r

    def begin(self):
        self.pes = ExitStack()
        self.all_res = []

    def sb(self, name, shape, dt):
        self.uid += 1
        return self.pes.enter_context(self.nc.sbuf_tensor("%s_%d" % (name, self.uid), list(shape), dt))

    def ps(self, name, shape, dt=F32):
        self.uid += 1
        return self.pes.enter_context(self.nc.psum_tensor("%s_%d" % (name, self.uid), list(shape), dt))

    def _deps(self, e, reads, writes):
        deps = []
        for r in reads:
            if r.w is not None:
                deps.append(r.w)
            if r.psum:
                deps.extend(t for t in r.rd if t.key != e)
            deps.extend(r.extra)
        for w in writes:
            if w.w is not None:
                deps.append(w.w)
            deps.extend(w.rd)
            deps.extend(w.extra)
        return deps

    def _wait(self, e, deps, skip_self=False):
        seen = self.seen[e]
        for t in sorted(deps, key=lambda t: -t.val):
            if skip_self and t.key == e:
                continue
            if seen.get(t.key, 0) >= t.val:
                continue
            self.prog[e].append(('w', self.sem[t.key], t.val))
            self.n_wait += 1
            seen[t.key] = t.val
            for k, v in t.clock.items():
                if seen.get(k, 0) < v:
                    seen[k] = v

    def _commit(self, tok, reads, writes):
        for r in reads:
            r.rd.append(tok)
            if len(r.rd) > 48:
                best = {}
                for t in r.rd:
                    if t.key not in best or best[t.key].val < t.val:
                        best[t.key] = t
                r.rd = list(best.values())
        for w in writes:
            if self.cond is not None:
                ex = w.extra + ([w.w] if w.w is not None else []) + w.rd
                best = {}
                for t in ex:
                    if t.key not in best or best[t.key].val < t.val:
                        best[t.key] = t
                w.extra = list(best.values())
            else:
                w.extra = []
            w.w = tok
            w.rd = []

    def op(self, e, name, reads=(), writes=(), **kw):
        deps = self._deps(e, reads, writes)
        self._wait(e, deps, skip_self=(e == 'pe'))
        self.cnt[e] += 1
        self.prog[e].append(('o', name, kw, self.sem[e], 1))
        self.n_inst += 1
        base = self.cond[e][0] if self.cond is not None else self.seen[e]
        clock = {k: v for k, v in base.items() if k in CE}
        tok = Tok(e, self.cnt[e], clock)
        self._commit(tok, reads, writes)
        return tok

    def dma(self, q, out, in_, reads=(), writes=(), _meth='dma_start', **kw):
        deps = self._deps(q, reads, writes)
        i = self.slot_i[q]
        self.slot_i[q] = (i + 1) % self.nslot[q]
        k = 'd_%s_%d' % (q, i)
        if self.cnt[k] > 0:
            deps.append(Tok(k, self.cnt[k], {}))
        self._wait(q, deps)
        self.cnt[k] += 16
        kw = dict(kw)
        kw['out'] = out
        kw['in_'] = in_
        self.prog[q].append(('o', _meth, kw, self.sem[k], 16))
        self.n_inst += 1
        base = self.cond[q][0] if self.cond is not None else self.seen[q]
        clock = {kk: v for kk, v in base.items() if kk in CE}
        tok = Tok(k, self.cnt[k], clock)
        self._commit(tok, reads, writes)
        return tok

    def cond_begin(self, flag_ap, r_flag):
        assert self.cond is None
        self.cond = {}
        for e in self.eng:
            deps = [r_flag.w] if r_flag.w is not None else []
            self._wait(e, deps)
            self.prog[e].append(('cb', flag_ap))
            self.cond[e] = (dict(self.seen[e]), dict(self.cnt))

    def cond_end(self):
        for e in self.eng:
            seen0, cnt0 = self.cond[e]
            fix = []
            if e in CE and self.cnt[e] != cnt0[e]:
                fix.append((self.sem[e], self.cnt[e] - cnt0[e]))
            if e in self.dq:
                for s in range(self.nslot[e]):
                    k = 'd_%s_%d' % (e, s)
                    if self.cnt[k] != cnt0[k]:
                        fix.append((self.sem[k], self.cnt[k] - cnt0[k]))
            self.prog[e].append(('ce', fix))
            self.seen[e] = seen0
        self.cond = None

    def end(self):
        deps = []
        for q in self.dq:
            for s in range(self.nslot[q]):
                k = 'd_%s_%d' % (q, s)
                if self.cnt[k] > 0:
                    deps.append(Tok(k, self.cnt[k], {}))
        self._wait('sp', deps)
        nc = self.nc
        prog = self.prog
        E_of = {'pe': nc.tensor, 'act': nc.scalar, 'dve': nc.vector, 'pool': nc.gpsimd, 'sp': nc.sync}

        def run(E, items):
            i = 0
            n = len(items)
            while i < n:
                it = items[i]
                if it[0] == 'w':
                    E.wait_ge(it[1], it[2])
                elif it[0] == 'o':
                    getattr(E, it[1])(**it[2]).then_inc(it[3], it[4])
                elif it[0] == 'cb':
                    j = i + 1
                    while items[j][0] != 'ce':
                        j += 1
                    inner = items[i + 1:j]
                    fix = items[j][1]
                    if any(x[0] == 'o' for x in inner):
                        val = E.value_load(it[1], min_val=0, max_val=1)
                        with E.If(val > 0):
                            run(E, inner)
                        with E.Else():
                            for (sem, amt) in fix:
                                E.drain().then_inc(sem, amt)
                    i = j
                i += 1

        with nc.Block() as block:
            @block.tensor
            def _(E):
                run(E, prog['pe'])

            @block.scalar
            def _(E):
                run(E, prog['act'])

            @block.vector
            def _(E):
                run(E, prog['dve'])

            @block.gpsimd
            def _(E):
                run(E, prog['pool'])

            @block.sync
            def _(E):
                run(E, prog['sp'])
        self.prog = {e: [] for e in self.eng}
        full = dict(self.cnt)
        for e in self.eng:
            self.seen[e] = dict(full)
        self.pes.close()
        self.pes = None
        self.all_res = []


D = 1024
KD = 8
HD = 64
OFF_ZU, OFF_ZV, OFF_SQ, OFF_SK, OFF_SV, OFF_FQ, OFF_FK, OFF_FV, OFF_FF, OFF_CA, OFF_CG, OFF_G = (
    0, 512, 1024, 1536, 1664, 1792, 2304, 2816, 3328, 3336, 3848, 4360)
N_IN = 8456
EPS = 1e-6
TAPS = 31
TWO_PI = 2.0 * math.pi


def build(NSEQ=2, S=2048, DFF=3584, NE=8, DEPTH=2, BS=None):
    NT = S // 128
    NTT = NSEQ * NT
    NTOK = NSEQ * S
    if BS is None:
        BS = min(512, S // 2)
    SB = BS // 128
    NBLK = (2 * NTOK + NE * (BS - 1)) // BS
    JMAX = (NTOK + BS - 1) // BS
    TBW = 384 if BS == 768 else min(512, BS)
    IOA = bass.IndirectOffsetOnAxis
    NB = S // 512
    NR = DFF // 512
    n_dense = (DEPTH + 1) // 2
    n_moe = DEPTH // 2
    sparse = (n_moe == 1 and DEPTH % 2 == 0)
    NR2 = NR
    nc = bass.Bass("TRN2", target_bir_lowering=False)
    dt = lambda name, shape, d=F32: nc.dram_tensor(name, list(shape), d, kind="ExternalInput").ap()
    x_d = dt("x", [NSEQ, S, D])
    pos_d = dt("positions", [NSEQ, S], I32)
    norm_mix_g = dt("norm_mix_g", [DEPTH, D])
    w_in = dt("w_in", [DEPTH, D, N_IN])
    gmlp_ln_g = dt("gmlp_ln_g", [DEPTH, 512])
    gmlp_ln_b = dt("gmlp_ln_b", [DEPTH, 512])
    gmlp_ws = dt("gmlp_ws", [DEPTH, 8, 128, 128])
    gmlp_bs = dt("gmlp_bs", [DEPTH, 8, 128])
    swa_sink = dt("swa_sink", [DEPTH, 8])
    fox_bf = dt("fox_bf", [DEPTH, 8])
    conv_w = dt("conv_w", [DEPTH, TAPS, 512])
    conv_b = dt("conv_b", [DEPTH, 512])
    conv_ln_g = dt("conv_ln_g", [DEPTH, 512])
    conv_ln_b = dt("conv_ln_b", [DEPTH, 512])
    w_branch = dt("w_branch", [DEPTH, 4, 512, D])
    w_out = dt("w_out", [DEPTH, D, D])
    norm_ffn_g = dt("norm_ffn_g", [DEPTH, D])
    ffn_w_gate = dt("ffn_w_gate", [n_dense, D, DFF])
    ffn_w_up = dt("ffn_w_up", [n_dense, D, DFF])
    ffn_w_down = dt("ffn_w_down", [n_dense, DFF, D])
    router_w = dt("router_w", [max(n_moe, 1), D, NE])
    if sparse:
        exp_w_gate = dt("exp_w_gate", [NE * 128 * NR2, 4096])
        exp_w_up = dt("exp_w_up", [NE * 128 * NR2, 4096])
        exp_w_down = dt("exp_w_down", [NE * 128 * NR2, 4096])
    else:
        exp_w_gate = dt("exp_w_gate", [max(n_moe, 1), NE, D, DFF])
        exp_w_up = dt("exp_w_up", [max(n_moe, 1), NE, D, DFF])
        exp_w_down = dt("exp_w_down", [max(n_moe, 1), NE, DFF, D])
    norm_final_g = dt("norm_final_g", [1, D])
    out_d = nc.dram_tensor("out", [NSEQ, S, D], F32, kind="ExternalOutput").ap()
    OT = nc.dram_tensor("ot_scr", [4, 128, 4, S], BF16, kind="Internal").ap()
    SG = nc.dram_tensor("sg_scr", [S, 4096], BF16, kind="Internal").ap()
    CS = nc.dram_tensor("cs_scr", [2, 64, S], F32, kind="Internal").ap()
    HTK = nc.dram_tensor("htk_scr", [NTOK, D], BF16, kind="Internal").ap()
    HS = nc.dram_tensor("hs_scr", [NBLK * BS, D], BF16, kind="Internal").ap()
    YS = nc.dram_tensor("ys_scr", [NBLK * BS, D], F32, kind="Internal").ap()
    XS = nc.dram_tensor("xs_scr", [NSEQ, S, D], F32, kind="Internal").ap()

    with ExitStack() as es:
        fw = FW(nc, es)
        sbp = lambda name, shape, d: es.enter_context(nc.sbuf_tensor(name, list(shape), d))
        xs = sbp("xs", [128, NT, D], F32)
        hT = sbp("hT", [128, KD, S], BF16)
        ident = sbp("ident", [128, 128], BF16)
        identf = sbp("identf", [128, 128], F32)
        onesf = sbp("onesf", [128, 128], F32)
        triu = sbp("triu", [128, 128], F32)
        mdiag = sbp("mdiag", [128, 128], BF16)
        mprev = sbp("mprev", [128, 128], BF16)
        mask2 = sbp("mask2", [128, 256], BF16)
        epsc = sbp("epsc", [128, 1], F32)
        rec_dummy = sbp("rec_dummy", [128, 1], F32)
        negpi = sbp("negpi", [128, 1], F32)
        Gt = sbp("Gt", [128, NTT * 8], F32)
        Mf = sbp("Mf", [128, NTT * 8], F32)
        sAi = sbp("sAi", [128, NTT], I32)
        sBi = sbp("sBi", [128, NTT], I32)
        gA = sbp("gA", [128, NTT], F32)
        gB = sbp("gB", [128, NTT], F32)
        idxw = sbp("idxw", [128, NBLK * NR2], I32)
        ncum = sbp("ncum", [128, NT, 8], F32)
        tot = sbp("tot", [128, NT, 8], F32)
        negtot = sbp("negtot", [128, NT, 8], F32)
        KML = sbp("KML", [128, NT, 8, 6], BF16)
        QML = sbp("QML", [128, NT, 8, 6], BF16)
        mbias = sbp("mbias", [128, 128], F32)

        fw.begin()
        r = fw.res()
        fw.op('pool', 'memset', ap=ident[:], constant=0.0, writes=[r])
        fw.op('pool', 'affine_select', out=ident[:], in_=ident[:], pattern=[[-1, 128]], compare_op=ALU.not_equal,
              fill=1.0, base=0, channel_multiplier=1, reads=[r], writes=[r])
        r = fw.res()
        fw.op('pool', 'memset', ap=identf[:], constant=0.0, writes=[r])
        fw.op('pool', 'affine_select', out=identf[:], in_=identf[:], pattern=[[-1, 128]], compare_op=ALU.not_equal,
              fill=1.0, base=0, channel_multiplier=1, reads=[r], writes=[r])
        fw.op('dve', 'memset', ap=onesf[:], constant=1.0, writes=[fw.res()])
        r = fw.res()
        fw.op('pool', 'memset', ap=triu[:], constant=1.0, writes=[r])
        fw.op('pool', 'affine_select', out=triu[:], in_=triu[:], pattern=[[1, 128]], compare_op=ALU.is_ge,
              fill=0.0, base=0, channel_multiplier=-1, reads=[r], writes=[r])
        r = fw.res()
        fw.op('pool', 'memset', ap=mdiag[:], constant=1.0, writes=[r])
        fw.op('pool', 'affine_select', out=mdiag[:], in_=mdiag[:], pattern=[[1, 128]], compare_op=ALU.is_ge,
              fill=0.0, base=0, channel_multiplier=-1, reads=[r], writes=[r])
        r_md = r
        r = fw.res()
        fw.op('pool', 'memset', ap=mprev[:], constant=1.0, writes=[r])
        fw.op('pool', 'affine_select', out=mprev[:], in_=mprev[:], pattern=[[-1, 128]], compare_op=ALU.is_gt,
              fill=0.0, base=0, channel_multiplier=1, reads=[r], writes=[r])
        r_mp = r
        r = fw.res()
        fw.op('pool', 'memset', ap=mbias[:], constant=0.0, writes=[r])
        fw.op('pool', 'affine_select', out=mbias[:], in_=mbias[:], pattern=[[1, 128]], compare_op=ALU.is_ge,
              fill=-30000.0, base=0, channel_multiplier=-1, reads=[r], writes=[r])
        r_m2 = fw.res()
        fw.op('pool', 'tensor_copy', out=mask2[:, 128:256], in_=mdiag[:], reads=[r_md], writes=[r_m2])
        fw.op('pool', 'tensor_copy', out=mask2[:, 0:128], in_=mprev[:], reads=[r_mp], writes=[r_m2])
        fw.op('dve', 'memset', ap=epsc[:], constant=EPS, writes=[fw.res()])
        fw.op('dve', 'memset', ap=negpi[:], constant=-math.pi, writes=[fw.res()])
        fw.end()

        def load_w(dst, src, res, q='pool'):
            fw.dma(q, dst, src.rearrange("(k p) n -> p k n", p=128), writes=[res])

        def phase_norm(g_row, moe_router=None, seq=0, g_row2=None):
            fw.begin()
            sp_ = sparse and moe_router is not None
            gT = fw.sb("gT", [128, KD], F32)
            r_g = fw.res()
            fw.dma('sp', gT[:], g_row.rearrange("(k p) -> p k", p=128), writes=[r_g], allow_slow_non_contiguous=True)
            ss = fw.sb("ss", [128, NT], F32)
            rstd = fw.sb("rstd", [128, NT], F32)
            junk = [fw.sb("junk%d" % i, [128, D], BF16) for i in range(2)]
            r_junk = [fw.res() for _ in range(2)]
            xn = [fw.sb("xn%d" % i, [128, D], BF16) for i in range(2)]
            r_xn = [fw.res() for _ in range(2)]
            pT = [fw.ps("pT%d" % i, [128, KD, 128], BF16) for i in range(2)]
            r_pT = [fw.res(True) for _ in range(2)]
            r_ss = [fw.res() for _ in range(NT)]
            r_hT = [fw.res() for _ in range(NT)]
            if moe_router is not None:
                wr = fw.sb("wr", [128, KD, NE], F32)
                r_wr = fw.res()
                fw.dma('sp', wr[:], moe_router.rearrange("(k p) e -> p k e", p=128), writes=[r_wr])
                for k in range(KD):
                    fw.op('dve', 'tensor_scalar', out=wr[:, k, :], in0=wr[:, k, :], scalar1=gT[:, k:k + 1], scalar2=None,
                          op0=ALU.mult, reads=[r_wr, r_g], writes=[r_wr])
                xf = [fw.sb("xf%d" % i, [128, D], F32) for i in range(2)]
                r_xf = [fw.res() for _ in range(2)]
                xfT = [fw.sb("xfT%d" % i, [128, KD, 128], F32) for i in range(2)]
                r_xfT = [fw.res() for _ in range(2)]
                pTf = [fw.ps("pTf%d" % i, [128, 4, 128], F32) for i in range(2)]
                r_pTf = [fw.res(True) for _ in range(2)]
                pL = fw.ps("pL", [128, NE], F32)
                r_pL = fw.res(True)
                lg = fw.sb("lg", [128, 8], F32)
                top = fw.sb("top", [128, 8], F32)
                nm1 = fw.sb("nm1", [128, 1], F32)
                ex = fw.sb("ex", [128, 8], F32)
                dd = fw.sb("dd", [128, 1], F32)
                msk = fw.sb("msk", [128, 8], F32)
                r_s = fw.res()
            if sp_:
                gfb_ = fw.sb("gfb_", [128, D], F32)
                fw.dma('sp', gfb_[:], g_row2.partition_broadcast(128), writes=[r_g])
                hrow = [fw.sb("hrow%d" % i, [128, D], BF16) for i in range(2)]
                r_hrow = [fw.res() for _ in range(2)]
            for t in range(NT):
                b = t % 2
                r_x = r_xs[t]
                fw.op('act', 'activation', out=junk[b][:], in_=xs[:, t, :], func=AF.Square, accum_out=ss[:, t:t + 1],
                      reads=[r_x], writes=[r_junk[b], r_ss[t]])
                fw.op('act', 'activation', out=rstd[:, t:t + 1], in_=ss[:, t:t + 1], func=AF.Ln, scale=1.0 / D,
                      bias=epsc[:, 0:1], reads=[r_ss[t]], writes=[r_ss[t]])
                fw.op('act', 'activation', out=rstd[:, t:t + 1], in_=rstd[:, t:t + 1], func=AF.Exp, scale=-0.5,
                      reads=[r_ss[t]], writes=[r_ss[t]])
                fw.op('act', 'activation', out=xn[b][:], in_=xs[:, t, :], func=AF.Copy, scale=rstd[:, t:t + 1],
                      reads=[r_x, r_ss[t]], writes=[r_xn[b]])
                for k in range(0 if sp_ else KD):
                    fw.op('pe', 'transpose', out=pT[b][:, k, :], in_=xn[b][:, k * 128:(k + 1) * 128], identity=ident[:],
                          reads=[r_xn[b]], writes=[r_pT[b]])
                for k in range(0 if sp_ else KD):
                    fw.op('dve', 'tensor_scalar', out=hT[:, k, t * 128:(t + 1) * 128], in0=pT[b][:, k, :],
                          scalar1=gT[:, k:k + 1], scalar2=None, op0=ALU.mult, reads=[r_pT[b], r_g], writes=[r_hT[t]])
                if sp_:
                    fw.op('pool', 'tensor_tensor', out=hrow[b][:], in0=xn[b][:], in1=gfb_[:], op=ALU.mult,
                          reads=[r_xn[b], r_g], writes=[r_hrow[b]])
                    fw.dma('sp', HTK[seq * S + t * 128:seq * S + (t + 1) * 128, :], hrow[b][:], reads=[r_hrow[b]], writes=[fw.res()])
                if moe_router is not None:
                    fw.op('act', 'activation', out=xf[b][:], in_=xs[:, t, :], func=AF.Copy, scale=rstd[:, t:t + 1],
                          reads=[r_x, r_ss[t]], writes=[r_xf[b]])
                    for hh in range(2):
                        for k4 in range(4):
                            k = hh * 4 + k4
                            fw.op('pe', 'matmul', out=pTf[hh][:, k4, :], lhsT=xf[b][:, k * 128:(k + 1) * 128], rhs=identf[:],
                                  start=True, stop=True, reads=[r_xf[b]], writes=[r_pTf[hh]])
                        fw.op('act', 'activation', out=xfT[b][:, hh * 4:(hh + 1) * 4, :], in_=pTf[hh][:], func=AF.Copy,
                              reads=[r_pTf[hh]], writes=[r_xfT[b]])
                    for k in range(KD):
                        fw.op('pe', 'matmul', out=pL[:], lhsT=xfT[b][:, k, :], rhs=wr[:, k, :], start=(k == 0), stop=(k == KD - 1),
                              reads=[r_xfT[b], r_wr], writes=[r_pL])
                    fw.op('dve', 'tensor_copy', out=lg[:, 0:NE], in_=pL[:], reads=[r_pL], writes=[r_s])
                    fw.op('dve', 'max', out=top[:], in_=lg[:, 0:NE], reads=[r_s], writes=[r_s])
                    fw.op('dve', 'tensor_scalar', out=nm1[:], in0=top[:, 0:1], scalar1=-1.0, scalar2=None, op0=ALU.mult,
                          reads=[r_s], writes=[r_s])
                    fw.op('act', 'activation', out=ex[:, 0:NE], in_=lg[:, 0:NE], func=AF.Exp, bias=nm1[:, 0:1], reads=[r_s], writes=[r_s])
                    fw.op('act', 'activation', out=dd[:], in_=top[:, 1:2], func=AF.Exp, bias=nm1[:, 0:1], reads=[r_s], writes=[r_s])
                    fw.op('dve', 'tensor_scalar', out=dd[:], in0=dd[:], scalar1=1.0, scalar2=None, op0=ALU.add, reads=[r_s], writes=[r_s])
                    fw.op('dve', 'reciprocal', out=dd[:], in_=dd[:], reads=[r_s], writes=[r_s])
                    fw.op('dve', 'tensor_scalar', out=msk[:, 0:NE], in0=lg[:, 0:NE], scalar1=top[:, 1:2], scalar2=None, op0=ALU.is_ge,
                          reads=[r_s], writes=[r_s])
                    tt_ = seq * NT + t if sparse else t
                    fw.op('dve', 'scalar_tensor_tensor', out=Gt[:, tt_ * 8:tt_ * 8 + NE], in0=ex[:, 0:NE], scalar=dd[:, 0:1], in1=msk[:, 0:NE],
                          op0=ALU.mult, op1=ALU.mult, reads=[r_s], writes=[r_s])
                    fw.op('dve', 'tensor_copy', out=Mf[:, tt_ * 8:tt_ * 8 + NE], in_=msk[:, 0:NE], reads=[r_s], writes=[r_s])
            fw.end()

        def phase_gmlp(l):
            fw.begin()
            wA = fw.sb("wA", [128, KD, 1024], BF16)
            r_wA = fw.res()
            load_w(wA[:, :, 0:512], w_in[l, :, OFF_ZU:OFF_ZU + 512], r_wA)
            load_w(wA[:, :, 512:1024], w_in[l, :, OFF_ZV:OFF_ZV + 512], r_wA)
            wsb = fw.sb("wsb", [128, 8, 128], BF16)
            r_ws = fw.res()
            fw.dma('pool', wsb[:], gmlp_ws[l].rearrange("g t s -> t g s"), writes=[r_ws])
            pW = fw.ps("pW", [128, 8, 128], BF16)
            r_pW = fw.res(True)
            for g in range(8):
                fw.op('pe', 'transpose', out=pW[:, g, :], in_=wsb[:, g, :], identity=ident[:], reads=[r_ws], writes=[r_pW])
            wsT = fw.sb("wsT", [128, 8, 128], BF16)
            r_wsT = fw.res()
            fw.op('dve', 'tensor_copy', out=wsT[:], in_=pW[:], reads=[r_pW], writes=[r_wsT])
            fw.op('pool', 'affine_select', out=wsT[:], in_=wsT[:], pattern=[[0, 8], [1, 128]], compare_op=ALU.is_ge, fill=0.0,
                  base=0, channel_multiplier=-1, reads=[r_wsT], writes=[r_wsT])
            bsT = fw.sb("bsT", [128, 8], F32)
            r_c = fw.res()
            fw.dma('sp', bsT[:], gmlp_bs[l].rearrange("g t -> t g"), writes=[r_c], allow_slow_non_contiguous=True)
            lng = fw.sb("lng", [128, 512], F32)
            lnb = fw.sb("lnb", [128, 512], F32)
            fw.dma('sp', lng[:], gmlp_ln_g[l:l + 1, :].partition_broadcast(128), writes=[r_c])
            fw.dma('sp', lnb[:], gmlp_ln_b[l:l + 1, :].partition_broadcast(128), writes=[r_c])
            psUV = [fw.ps("psUV%d" % i, [128, 1024]) for i in range(2)]
            psU = [p[:, 0:512] for p in psUV]
            psV = [p[:, 512:1024] for p in psUV]
            psM = fw.ps("psM", [128, 512])
            pT2 = pW[:, 0:4, :]
            r_psU = [fw.res(True) for _ in range(2)]
            r_psV = r_psU
            r_psM = fw.res(True)
            r_pT2 = r_pW
            u = [fw.sb("u%d" % i, [128, 512], F32) for i in range(3)]
            gv = [fw.sb("gv%d" % i, [128, 512], F32) for i in range(3)]
            sq = [fw.sb("sq%d" % i, [128, 512], BF16) for i in range(3)]
            vn = [fw.sb("vn%d" % i, [128, 512], F32) for i in range(3)]
            vb = [fw.sb("vb%d" % i, [128, 512], BF16) for i in range(3)]
            oa = [fw.sb("oa%d" % i, [128, 512], BF16) for i in range(3)]
            st = [fw.sb("st%d" % i, [128, 8], F32) for i in range(3)]
            r_u = [fw.res() for _ in range(3)]
            r_gv = [fw.res() for _ in range(3)]
            r_sq = [fw.res() for _ in range(3)]
            r_vn = [fw.res() for _ in range(3)]
            r_vb = [fw.res() for _ in range(3)]
            r_oa = [fw.res() for _ in range(3)]
            r_st = [fw.res() for _ in range(3)]
            oT = [fw.sb("oT%d" % i, [128, 4, 512], BF16) for i in range(2)]
            r_oT = [fw.res() for _ in range(2)]
            r_OT = fw.res()
            gx = [fw.sb("gx%d" % i, [128, 1024], F32) for i in range(2)]
            gt = [fw.sb("gt%d" % i, [128, 1024], F32) for i in range(2)]
            r_gx = [fw.res() for _ in range(2)]
            r_gt = [fw.res() for _ in range(2)]
            gcnt = [0]

            def gelu2(b, bb, acc_ap, r_acc):
                i = gcnt[0] % 2
                gcnt[0] += 1
                fw.op('act', 'activation', out=gx[i][:, 0:512], in_=psU[b], func=AF.Copy, reads=[r_psU[b]], writes=[r_gx[i]])
                fw.op('act', 'activation', out=gx[i][:, 512:1024], in_=psV[b], func=AF.Copy, reads=[r_psU[b]], writes=[r_gx[i]])
                fw.op('act', 'activation', out=gt[i][:], in_=gx[i][:], func=AF.Square, reads=[r_gx[i]], writes=[r_gt[i]])
                fw.op('dve', 'tensor_scalar', out=gt[i][:], in0=gt[i][:], scalar1=0.044715, scalar2=1.0, op0=ALU.mult, op1=ALU.add,
                      reads=[r_gt[i]], writes=[r_gt[i]])
                fw.op('dve', 'tensor_tensor', out=gt[i][:], in0=gt[i][:], in1=gx[i][:], op=ALU.mult, reads=[r_gt[i], r_gx[i]], writes=[r_gt[i]])
                fw.op('act', 'activation', out=gt[i][:], in_=gt[i][:], func=AF.Exp, scale=-1.5957691216057308, reads=[r_gt[i]], writes=[r_gt[i]])
                fw.op('dve', 'tensor_scalar', out=gt[i][:], in0=gt[i][:], scalar1=1.0, scalar2=None, op0=ALU.add, reads=[r_gt[i]], writes=[r_gt[i]])
                fw.op('dve', 'reciprocal', out=gt[i][:], in_=gt[i][:], reads=[r_gt[i]], writes=[r_gt[i]])
                fw.op('dve', 'tensor_tensor', out=u[bb][:], in0=gx[i][:, 0:512], in1=gt[i][:, 0:512], op=ALU.mult,
                      reads=[r_gx[i], r_gt[i]], writes=[r_u[bb]])
                fw.op('dve', 'scalar_tensor_tensor', out=gv[bb][:], in0=gx[i][:, 512:1024], scalar=1.0, in1=gt[i][:, 512:1024], op0=ALU.mult,
                      op1=ALU.mult, accum_out=acc_ap, reads=[r_gx[i], r_gt[i]], writes=[r_gv[bb], r_acc])

            def part1(t):
                b = t % 2
                bb = t % 3
                tok = slice(t * 128, (t + 1) * 128)
                for k in range(KD):
                    fw.op('pe', 'matmul', out=psU[b][:], lhsT=hT[:, k, tok], rhs=wA[:, k, 0:512], start=(k == 0), stop=(k == KD - 1),
                          reads=[r_wA], writes=[r_psU[b]])
                for k in range(KD):
                    fw.op('pe', 'matmul', out=psV[b][:], lhsT=hT[:, k, tok], rhs=wA[:, k, 512:1024], start=(k == 0), stop=(k == KD - 1),
                          reads=[r_wA], writes=[r_psV[b]])
                s = st[bb]
                gelu2(b, bb, s[:, 0:1], r_st[bb])
                fw.op('act', 'activation', out=sq[bb][:], in_=gv[bb][:], func=AF.Square, accum_out=s[:, 1:2],
                      reads=[r_gv[bb]], writes=[r_sq[bb], r_st[bb]])
                rs = [r_st[bb]]
                fw.op('dve', 'tensor_scalar', out=s[:, 2:3], in0=s[:, 0:1], scalar1=1.0 / 512, scalar2=None, op0=ALU.mult, reads=rs, writes=rs)
                fw.op('dve', 'tensor_tensor', out=s[:, 3:4], in0=s[:, 2:3], in1=s[:, 2:3], op=ALU.mult, reads=rs, writes=rs)
                fw.op('dve', 'scalar_tensor_tensor', out=s[:, 4:5], in0=s[:, 1:2], scalar=1.0 / 512, in1=s[:, 3:4], op0=ALU.mult,
                      op1=ALU.subtract, reads=rs, writes=rs)
                fw.op('act', 'activation', out=s[:, 5:6], in_=s[:, 4:5], func=AF.Ln, bias=epsc[:, 0:1], reads=rs, writes=rs)
                fw.op('act', 'activation', out=s[:, 5:6], in_=s[:, 5:6], func=AF.Exp, scale=-0.5, reads=rs, writes=rs)
                fw.op('dve', 'tensor_scalar', out=vn[bb][:], in0=gv[bb][:], scalar1=s[:, 2:3], scalar2=s[:, 5:6], op0=ALU.subtract,
                      op1=ALU.mult, reads=[r_gv[bb], r_st[bb]], writes=[r_vn[bb]])
                fw.op('pool', 'tensor_tensor', out=vn[bb][:], in0=vn[bb][:], in1=lng[:], op=ALU.mult, reads=[r_vn[bb], r_c], writes=[r_vn[bb]])
                fw.op('pool', 'tensor_tensor', out=vb[bb][:], in0=vn[bb][:], in1=lnb[:], op=ALU.add, reads=[r_vn[bb], r_c], writes=[r_vb[bb]])
            def part2(t):
                bb = t % 3
                for g in range(8):
                    gs = slice(g * 64, (g + 1) * 64)
                    fw.op('pe', 'matmul', out=psM[:, gs], lhsT=wsT[:, g, :], rhs=vb[bb][:, gs], start=True, stop=True,
                          reads=[r_wsT, r_vb[bb]], writes=[r_psM])
                for g in range(8):
                    gs = slice(g * 64, (g + 1) * 64)
                    fw.op('dve', 'scalar_tensor_tensor', out=oa[bb][:, gs], in0=psM[:, gs], scalar=bsT[:, g:g + 1], in1=u[bb][:, gs],
                          op0=ALU.add, op1=ALU.mult, reads=[r_psM, r_c, r_u[bb]], writes=[r_oa[bb]])
                for c in range(4):
                    fw.op('pe', 'transpose', out=pT2[:, c, :], in_=oa[bb][:, c * 128:(c + 1) * 128], identity=ident[:],
                          reads=[r_oa[bb]], writes=[r_pT2])
                blk = t // 4
                ob = blk % 2
                fw.op('act', 'activation', out=oT[ob][:, :, (t % 4) * 128:(t % 4 + 1) * 128], in_=pT2[:], func=AF.Copy,
                      reads=[r_pT2], writes=[r_oT[ob]])
                if t % 4 == 3:
                    fw.dma('sp', OT[0, :, :, blk * 512:(blk + 1) * 512], oT[ob][:], reads=[r_oT[ob]], writes=[r_OT])
            gu = iter(())
            per = 0
            for t in range(NT):
                part1(t)
                for _ in range(per):
                    next(gu, None)
                if t > 1:
                    part2(t - 2)
            part2(NT - 2)
            part2(NT - 1)
            for _ in gu:
                pass
            fw.end()

        def phase_conv(l):
            fw.begin()
            wD = fw.sb("wD", [128, KD, 1024], BF16)
            r_wD = fw.res()
            load_w(wD[:, :, 0:512], w_in[l, :, OFF_CA:OFF_CA + 512], r_wD)
            load_w(wD[:, :, 512:1024], w_in[l, :, OFF_CG:OFF_CG + 512], r_wD)
            cw = fw.sb("cw", [128, 4, TAPS], F32)
            cb = fw.sb("cb", [128, 4], F32)
            cg_ = fw.sb("cg_", [128, 4], F32)
            cbe = fw.sb("cbe", [128, 4], F32)
            r_c = fw.res()
            for c in range(4):
                fw.dma('sp', cw[:, c, :], conv_w[l, :, c * 128:(c + 1) * 128].rearrange("j p -> p j"), writes=[r_c],
                       allow_slow_non_contiguous=True)
            fw.dma('sp', cb[:], conv_b[l].rearrange("(k p) -> p k", p=128), writes=[r_c], allow_slow_non_contiguous=True)
            fw.dma('sp', cg_[:], conv_ln_g[l].rearrange("(k p) -> p k", p=128), writes=[r_c], allow_slow_non_contiguous=True)
            fw.dma('sp', cbe[:], conv_ln_b[l].rearrange("(k p) -> p k", p=128), writes=[r_c], allow_slow_non_contiguous=True)
            PAD = TAPS - 1
            yT = [[fw.sb("yT%d_%d" % (c, i), [128, PAD + 512], BF16) for i in range(2)] for c in range(4)]
            dg = fw.sb("dg", [128, 4, TAPS, 128], BF16)
            r_dg = fw.res()
            for c in range(4):
                for j in range(TAPS):
                    fw.op('dve', 'tensor_scalar', out=dg[:, c, j, :], in0=ident[:], scalar1=cw[:, c, j:j + 1], scalar2=None, op0=ALU.mult,
                          reads=[r_c], writes=[r_dg])
            psK = [fw.ps("psK%d" % i, [128, 512]) for i in range(2)]
            r_psK = [fw.res(True) for _ in range(2)]
            r_yT = [[fw.res() for i in range(2)] for c in range(4)]
            acc = [fw.sb("acc%d" % c, [128, 512], F32) for c in range(4)]
            r_acc = [fw.res() for _ in range(4)]
            psA = [fw.ps("psA%d" % i, [128, 512]) for i in range(2)]
            psG = [fw.ps("psG%d" % i, [128, 512]) for i in range(2)]
            r_psA = [fw.res(True) for _ in range(2)]
            r_psG = [fw.res(True) for _ in range(2)]
            ps1 = fw.ps("ps1", [128, 512])
            ps2 = fw.ps("ps2", [128, 512])
            r_ps1 = fw.res(True)
            r_ps2 = fw.res(True)
            sig = [fw.sb("sig%d" % i, [128, 512], F32) for i in range(2)]
            r_sig = [fw.res() for _ in range(2)]
            sqb = [fw.sb("sqb%d" % i, [128, 512], F32) for i in range(2)]
            r_sqb = [fw.res() for _ in range(2)]
            mean = fw.sb("mean", [128, 512], F32)
            msq = fw.sb("msq", [128, 512], F32)
            rsd = fw.sb("rsd", [128, 512], F32)
            r_m = fw.res()
            tmp = [fw.sb("tmp%d" % i, [128, 512], F32) for i in range(2)]
            r_tmp = [fw.res() for _ in range(2)]
            od = [fw.sb("od%d" % i, [128, 4, 512], BF16) for i in range(2)]
            r_od = [fw.res() for _ in range(2)]
            r_OT = fw.res()
            def unit_proj(n):
                bk, c = divmod(n, 4)
                yb = bk % 2
                b = n % 2
                tok = slice(bk * 512, (bk + 1) * 512)
                y = yT[c][yb]
                ry = r_yT[c][yb]
                if bk == 0:
                    fw.op('pool', 'memset', ap=y[:, 0:PAD], constant=0.0, writes=[ry])
                else:
                    fw.op('pool', 'tensor_copy', out=y[:, 0:PAD], in_=yT[c][1 - yb][:, 512:512 + PAD], reads=[r_yT[c][1 - yb]], writes=[ry])
                for k in range(KD):
                    fw.op('pe', 'matmul', out=psA[b][:], lhsT=wD[:, k, c * 128:(c + 1) * 128], rhs=hT[:, k, tok],
                          start=(k == 0), stop=(k == KD - 1), reads=[r_wD], writes=[r_psA[b]])
                for k in range(KD):
                    fw.op('pe', 'matmul', out=psG[b][:], lhsT=wD[:, k, 512 + c * 128:512 + (c + 1) * 128], rhs=hT[:, k, tok],
                          start=(k == 0), stop=(k == KD - 1), reads=[r_wD], writes=[r_psG[b]])
                fw.op('act', 'activation', out=sig[b][:], in_=psG[b][:], func=AF.Sigmoid, reads=[r_psG[b]], writes=[r_sig[b]])
                fw.op('dve', 'tensor_tensor', out=y[:, PAD:PAD + 512], in0=psA[b][:], in1=sig[b][:],
                      op=ALU.mult, reads=[r_psA[b], r_sig[b]], writes=[ry])

            def unit_taps(n):
                bk, c = divmod(n, 4)
                yb = bk % 2
                b = n % 2
                y = yT[c][yb]
                ry = r_yT[c][yb]
                for j in range(TAPS):
                    fw.op('pe', 'matmul', out=psK[b][:], lhsT=dg[:, c, j, :], rhs=y[:, j:j + 512], start=(j == 0), stop=(j == TAPS - 1),
                          reads=[ry, r_dg], writes=[r_psK[b]])
                fw.op('dve', 'tensor_scalar', out=acc[c][:], in0=psK[b][:], scalar1=cb[:, c:c + 1], scalar2=None, op0=ALU.add,
                      reads=[r_psK[b], r_c], writes=[r_acc[c]])

            def block_ln(bk):
                tok = slice(bk * 512, (bk + 1) * 512)
                for c in range(4):
                    fw.op('pe', 'matmul', out=ps1[:], lhsT=onesf[:], rhs=acc[c][:], start=(c == 0), stop=(c == 3),
                          reads=[r_acc[c]], writes=[r_ps1])
                for c in range(4):
                    sb_ = c % 2
                    fw.op('act', 'activation', out=sqb[sb_][:], in_=acc[c][:], func=AF.Square, reads=[r_acc[c]], writes=[r_sqb[sb_]])
                    fw.op('pe', 'matmul', out=ps2[:], lhsT=onesf[:], rhs=sqb[sb_][:], start=(c == 0), stop=(c == 3),
                          reads=[r_sqb[sb_]], writes=[r_ps2])
                fw.op('dve', 'tensor_scalar', out=mean[:], in0=ps1[:], scalar1=1.0 / 512, scalar2=None, op0=ALU.mult,
                      reads=[r_ps1], writes=[r_m])
                fw.op('dve', 'tensor_tensor', out=msq[:], in0=mean[:], in1=mean[:], op=ALU.mult, reads=[r_m], writes=[r_m])
                fw.op('dve', 'scalar_tensor_tensor', out=rsd[:], in0=ps2[:], scalar=1.0 / 512, in1=msq[:], op0=ALU.mult, op1=ALU.subtract,
                      reads=[r_ps2, r_m], writes=[r_m])
                fw.op('act', 'activation', out=rsd[:], in_=rsd[:], func=AF.Sqrt, bias=epsc[:, 0:1], reads=[r_m], writes=[r_m])
                fw.op('dve', 'reciprocal', out=rsd[:], in_=rsd[:], reads=[r_m], writes=[r_m])
                ob = bk % 2
                for c in range(4):
                    b = c % 2
                    fw.op('dve', 'tensor_tensor', out=tmp[b][:], in0=acc[c][:], in1=mean[:], op=ALU.subtract,
                          reads=[r_acc[c], r_m], writes=[r_tmp[b]])
                    fw.op('dve', 'tensor_tensor', out=tmp[b][:], in0=tmp[b][:], in1=rsd[:], op=ALU.mult,
                          reads=[r_tmp[b], r_m], writes=[r_tmp[b]])
                    fw.op('dve', 'tensor_scalar', out=tmp[b][:], in0=tmp[b][:], scalar1=cg_[:, c:c + 1], scalar2=cbe[:, c:c + 1],
                          op0=ALU.mult, op1=ALU.add, reads=[r_tmp[b], r_c], writes=[r_tmp[b]])
                    fw.op('act', 'activation', out=od[ob][:, c, :], in_=tmp[b][:], func=AF.Silu, reads=[r_tmp[b]], writes=[r_od[ob]])
                fw.dma('sp', OT[3, :, :, tok], od[ob][:], reads=[r_od[ob]], writes=[r_OT])

            NU = NB * 4
            for n in range(NU + 1):
                if n < NU:
                    unit_proj(n)
                if n > 0:
                    unit_taps(n - 1)
                    if (n - 1) % 4 == 3:
                        block_ln((n - 1) // 4)
            fw.end()

        def attention(banks, r_bk, nheads, kd, qT, kT, kv_of, vx, r_q, r_k, r_v, bias_of, r_bias, prev_only, add_sink, dst_idx):
            LA = 2
            psS = [banks[4 + i] for i in range(3)]
            r_psS = [r_bk[4 + i] for i in range(3)]
            psO = [banks[7][:, 0:128], banks[3][:, 0:128]]
            r_psO = [r_bk[7], r_bk[3]]
            pt = [fw.sb("pt%d" % i, [128, 512], BF16) for i in range(4)]
            r_pt = [fw.res() for _ in range(4)]
            rec = [fw.sb("rec%d" % i, [128, 128], F32) for i in range(2)]
            r_rec = [fw.res() for _ in range(2)]
            oTt = fw.sb("oTt", [128, S], BF16)
            r_oTt = fw.res()
            r_OT = fw.res()
            groups = []
            for h in range(nheads):
                for i in range(NT):
                    js = [j for j in ((i - 1, i) if prev_only else range(i + 1)) if j >= 0]
                    chunks = [js[c:c + 4] for c in range(0, len(js), 4)]
                    for ci, ch in enumerate(chunks):
                        groups.append((h, i, ch, ci == 0, ci == len(chunks) - 1))
            pending = []

            def flush_one():
                gi, (h, i, ch, first, last) = pending.pop(0)
                hk = kv_of(h)
                pb = gi % 4
                ob = (h * NT + i) % 2
                half = h % 2
                qs = slice(i * 128, (i + 1) * 128)
                for jj, j in enumerate(ch):
                    fw.op('pe', 'matmul', out=psO[ob], lhsT=vx[:, j, hk, :], rhs=pt[pb][:, jj * 128:(jj + 1) * 128],
                          start=(first and jj == 0), stop=(last and jj == len(ch) - 1), reads=[r_v, r_pt[pb]], writes=[r_psO[ob]])
                if last:
                    if add_sink is not None:
                        fw.op('dve', 'tensor_scalar', out=rec[ob][64:128, :], in0=psO[ob][64:128, :],
                              scalar1=add_sink[0][64:128, add_sink[1] + h:add_sink[1] + h + 1],
                              scalar2=None, op0=ALU.add, reads=[r_psO[ob], r_bias], writes=[r_rec[ob]])
                        fw.op('dve', 'reciprocal', out=rec[ob][64:128, :], in_=rec[ob][64:128, :], reads=[r_rec[ob]], writes=[r_rec[ob]])
                    else:
                        fw.op('dve', 'reciprocal', out=rec[ob][64:128, :], in_=psO[ob][64:128, :], reads=[r_psO[ob]], writes=[r_rec[ob]])
                    fw.op('dve', 'tensor_tensor', out=oTt[half * 64:(half + 1) * 64, qs], in0=psO[ob][0:64, :], in1=rec[ob][64:128, :],
                          op=ALU.mult, reads=[r_psO[ob], r_rec[ob]], writes=[r_oTt])
                    if half == 1 and i == NT - 1:
                        hc = dst_idx[1] + h // 2
                        fw.dma('sp', OT[dst_idx[0], :, hc, :], oTt[:], reads=[r_oTt], writes=[r_OT])

            for gi, g in enumerate(groups):
                h, i, ch, first, last = g
                hk = kv_of(h)
                sb_ = gi % 3
                pb = gi % 4
                qs = slice(i * 128, (i + 1) * 128)
                n = len(ch)
                for jj, j in enumerate(ch):
                    ks = slice(j * 128, (j + 1) * 128)
                    fw.op('pe', 'matmul', out=psS[sb_][:, jj * 128:(jj + 1) * 128], lhsT=kT[0:kd, hk, ks], rhs=qT[0:kd, h, qs],
                          start=True, stop=True, reads=[r_q, r_k], writes=[r_psS[sb_]])
                kw = {}
                rds = [r_psS[sb_]]
                bias = bias_of(i, h)
                if bias is not None:
                    kw['bias'] = bias
                    rds.append(r_bias)
                fw.op('act', 'activation', out=pt[pb][:, 0:n * 128], in_=psS[sb_][:, 0:n * 128], func=AF.Exp, scale=0.125,
                      reads=rds, writes=[r_pt[pb]], **kw)
                if prev_only and n == 2:
                    fw.op('pool', 'tensor_tensor', out=pt[pb][:, 0:256], in0=pt[pb][:, 0:256], in1=mask2[:], op=ALU.mult,
                          reads=[r_pt[pb]], writes=[r_pt[pb]])
                else:
                    for jj, j in enumerate(ch):
                        if j == i:
                            fw.op('pool', 'tensor_tensor', out=pt[pb][:, jj * 128:(jj + 1) * 128], in0=pt[pb][:, jj * 128:(jj + 1) * 128],
                                  in1=mdiag[:], op=ALU.mult, reads=[r_pt[pb]], writes=[r_pt[pb]])
                        elif prev_only:
                            fw.op('pool', 'tensor_tensor', out=pt[pb][:, jj * 128:(jj + 1) * 128], in0=pt[pb][:, jj * 128:(jj + 1) * 128],
                                  in1=mprev[:], op=ALU.mult, reads=[r_pt[pb]], writes=[r_pt[pb]])
                pending.append((gi, g))
                if len(pending) > LA:
                    flush_one()
            while pending:
                flush_one()

        def attention_blk(banks, r_bk, nheads, kd, qT, kT, vx, r_q, r_k, r_v, dst_idx):
            LA = 2
            psS = [banks[4 + i] for i in range(3)]
            r_psS = [r_bk[4 + i] for i in range(3)]
            psO = [banks[7], banks[3]]
            r_psO = [r_bk[7], r_bk[3]]
            pt = [fw.sb("pt%d" % i, [128, 512], BF16) for i in range(4)]
            r_pt = [fw.res() for _ in range(4)]
            rec = [fw.sb("rec%d" % i, [128, 512], F32) for i in range(2)]
            r_rec = [fw.res() for _ in range(2)]
            oTt = fw.sb("oTt", [128, S], BF16)
            r_oTt = fw.res()
            r_OT = fw.res()
            groups = []
            for h in range(nheads):
                for B in range(NB):
                    nj = 4 * B + 4
                    for j in range(nj):
                        groups.append((h, B, j, j == 0, j == nj - 1))
            pending = []

            def geom(B, j):
                if j < 4 * B:
                    return 0, 512
                jr = j - 4 * B
                return jr * 128, (4 - jr) * 128

            def flush_one():
                gi, (h, B, j, first, last) = pending.pop(0)
                pb = gi % 4
                ob = (h * NB + B) % 2
                half = h % 2
                c0, ncols = geom(B, j)
                fw.op('pe', 'matmul', out=psO[ob][:, c0:c0 + ncols], lhsT=vx[:, j, h, :], rhs=pt[pb][:, 0:ncols],
                      start=first, stop=last, reads=[r_v, r_pt[pb]], writes=[r_psO[ob]])
                if last:
                    qb = slice(B * 512, (B + 1) * 512)
                    fw.op('dve', 'reciprocal', out=rec[ob][64:128, :], in_=psO[ob][64:128, :], reads=[r_psO[ob]], writes=[r_rec[ob]])
                    fw.op('dve', 'tensor_tensor', out=oTt[half * 64:(half + 1) * 64, qb], in0=psO[ob][0:64, :], in1=rec[ob][64:128, :],
                          op=ALU.mult, reads=[r_psO[ob], r_rec[ob]], writes=[r_oTt])
                    if half == 1 and B == NB - 1:
                        hc = dst_idx[1] + h // 2
                        fw.dma('sp', OT[dst_idx[0], :, hc, :], oTt[:], reads=[r_oTt], writes=[r_OT])

            for gi, g in enumerate(groups):
                h, B, j, first, last = g
                sb_ = gi % 3
                pb = gi % 4
                c0, ncols = geom(B, j)
                q0 = B * 512 + c0
                ks = slice(j * 128, (j + 1) * 128)
                fw.op('pe', 'matmul', out=psS[sb_][:, 0:ncols], lhsT=kT[0:kd, h, ks], rhs=qT[0:kd, h, q0:q0 + ncols],
                      start=True, stop=True, reads=[r_q, r_k], writes=[r_psS[sb_]])
                if j >= 4 * B:
                    fw.op('dve', 'tensor_tensor', out=psS[sb_][:, 0:128], in0=psS[sb_][:, 0:128], in1=mbias[:], op=ALU.add,
                          reads=[r_psS[sb_]], writes=[r_psS[sb_]])
                fw.op('act', 'activation', out=pt[pb][:, 0:ncols], in_=psS[sb_][:, 0:ncols], func=AF.Exp, scale=0.125,
                      reads=[r_psS[sb_]], writes=[r_pt[pb]])
                pending.append((gi, g))
                if len(pending) > LA:
                    flush_one()
            while pending:
                flush_one()

        def proj_heads(banks, r_bk, dst, w, nh, r_w, r_dst, rope=None, w_rot=None):
            psA = [banks[i][0:64, :] for i in range(2)]
            r_psA = [r_bk[i] for i in range(2)]
            if rope is not None:
                psB = [banks[2][0:64, :], banks[3][0:64, :]]
                r_psB = [r_bk[2], r_bk[3]]
                t1 = [fw.sb("rt1_%d" % i, [64, 512], F32) for i in range(2)]
                t2 = [fw.sb("rt2_%d" % i, [64, 512], F32) for i in range(2)]
                r_t1 = [fw.res() for _ in range(2)]
                r_t2 = [fw.res() for _ in range(2)]
                cos2, sin2, r_cs = rope
            n = 0
            for h in range(nh):
                for bk in range(NB):
                    b = n % 2
                    n += 1
                    tok = slice(bk * 512, (bk + 1) * 512)
                    for k in range(KD):
                        fw.op('pe', 'matmul', out=psA[b], lhsT=w[:, k, h * 64:(h + 1) * 64], rhs=hT[:, k, tok], start=(k == 0),
                              stop=(k == KD - 1), reads=[r_w], writes=[r_psA[b]])
                    if rope is None:
                        fw.op('act', 'activation', out=dst[:, h, tok], in_=psA[b], func=AF.Copy, reads=[r_psA[b]], writes=[r_dst])
                    else:
                        for k in range(KD):
                            fw.op('pe', 'matmul', out=psB[b], lhsT=w_rot[:, k, h * 64:(h + 1) * 64], rhs=hT[:, k, tok], start=(k == 0),
                                  stop=(k == KD - 1), reads=[r_w], writes=[r_psB[b]])
                        fw.op('dve', 'tensor_tensor', out=t1[b][:], in0=psA[b], in1=cos2[:, tok], op=ALU.mult,
                              reads=[r_psA[b], r_cs], writes=[r_t1[b]])
                        fw.op('dve', 'tensor_tensor', out=t2[b][:], in0=psB[b], in1=sin2[:, tok], op=ALU.mult,
                              reads=[r_psB[b], r_cs], writes=[r_t2[b]])
                        fw.op('pool', 'tensor_tensor', out=dst[:, h, tok], in0=t1[b][:], in1=t2[b][:], op=ALU.add,
                              reads=[r_t1[b], r_t2[b]], writes=[r_dst])

        def proj_v(banks, r_bk, vx, w, nh, r_w, r_vx):
            psV = [banks[i][:, 0:nh * 64] for i in range(2)]
            r_psV = [r_bk[i] for i in range(2)]
            fw.op('pool', 'memset', ap=vx[:], constant=1.0, writes=[r_vx])
            for t in range(NT):
                b = t % 2
                tok = slice(t * 128, (t + 1) * 128)
                for k in range(KD):
                    fw.op('pe', 'matmul', out=psV[b], lhsT=hT[:, k, tok], rhs=w[:, k, 0:nh * 64], start=(k == 0), stop=(k == KD - 1),
                          reads=[r_w], writes=[r_psV[b]])
                for h in range(nh):
                    fw.op('act', 'activation', out=vx[:, t, h, 0:64], in_=psV[b][:, h * 64:(h + 1) * 64], func=AF.Copy,
                          reads=[r_psV[b]], writes=[r_vx])

        def load_rot(dst, src_cols, nh, res):
            sv = src_cols.rearrange("(k p) (h two f) -> p k h two f", p=128, two=2, f=32)
            dv = dst.rearrange("p k (h two f) -> p k h two f", two=2, f=32)
            for k in range(KD):
                fw.dma('pool', dv[:, k, :, 0, :], sv[:, k, :, 1, :], writes=[res])
                fw.dma('pool', dv[:, k, :, 1, :], sv[:, k, :, 0, :], writes=[res])

        def phase_rope_tables(seq):
            fw.begin()
            posi = fw.sb("posi", [64, S], I32)
            ang = fw.sb("ang", [64, S], F32)
            ang2 = fw.sb("ang2", [64, S], F32)
            ni = fw.sb("ni", [64, S], I32)
            nf = fw.sb("nf", [64, S], F32)
            invf = fw.sb("invf", [64, 1], F32)
            sgn = fw.sb("sgn", [64, 1], F32)
            r_cs = fw.res()
            fw.dma('sp', posi[:], pos_d[seq:seq + 1, :].partition_broadcast(64), writes=[r_cs])
            fw.op('pool', 'iota', out=invf[0:32, :], pattern=[[0, 1]], base=0, channel_multiplier=1,
                  allow_small_or_imprecise_dtypes=True, writes=[r_cs])
            fw.op('pool', 'iota', out=invf[32:64, :], pattern=[[0, 1]], base=0, channel_multiplier=1,
                  allow_small_or_imprecise_dtypes=True, reads=[r_cs], writes=[r_cs])
            fw.op('act', 'activation', out=invf[:], in_=invf[:], func=AF.Exp, scale=-math.log(10000.0) / 32.0, reads=[r_cs], writes=[r_cs])
            fw.op('dve', 'memset', ap=sgn[0:32, :], constant=-1.0, reads=[r_cs], writes=[r_cs])
            fw.op('dve', 'memset', ap=sgn[32:64, :], constant=1.0, reads=[r_cs], writes=[r_cs])
            fw.op('dve', 'tensor_copy', out=ang[:], in_=posi[:], reads=[r_cs], writes=[r_cs])
            fw.op('dve', 'tensor_scalar', out=ang[:], in0=ang[:], scalar1=invf[:, 0:1], scalar2=None, op0=ALU.mult, reads=[r_cs], writes=[r_cs])

            def sin_of(dst, shift, sign_ap):
                fw.op('dve', 'tensor_scalar', out=ang2[:], in0=ang[:], scalar1=shift, scalar2=None, op0=ALU.add, reads=[r_cs], writes=[r_cs])
                fw.op('dve', 'tensor_scalar', out=nf[:], in0=ang2[:], scalar1=1.0 / TWO_PI, scalar2=None, op0=ALU.mult, reads=[r_cs], writes=[r_cs])
                fw.op('dve', 'tensor_copy', out=ni[:], in_=nf[:], reads=[r_cs], writes=[r_cs])
                fw.op('dve', 'tensor_copy', out=nf[:], in_=ni[:], reads=[r_cs], writes=[r_cs])
                fw.op('dve', 'scalar_tensor_tensor', out=ang2[:], in0=nf[:], scalar=-TWO_PI, in1=ang2[:], op0=ALU.mult, op1=ALU.add,
                      reads=[r_cs], writes=[r_cs])
                fw.op('dve', 'tensor_scalar', out=nf[:], in0=ang2[:], scalar1=math.pi, scalar2=-TWO_PI, op0=ALU.is_gt, op1=ALU.mult,
                      reads=[r_cs], writes=[r_cs])
                fw.op('dve', 'tensor_tensor', out=ang2[:], in0=ang2[:], in1=nf[:], op=ALU.add, reads=[r_cs], writes=[r_cs])
                fw.op('dve', 'tensor_scalar', out=nf[:], in0=ang2[:], scalar1=-math.pi, scalar2=TWO_PI, op0=ALU.is_lt, op1=ALU.mult,
                      reads=[r_cs], writes=[r_cs])
                fw.op('dve', 'tensor_tensor', out=ang2[:], in0=ang2[:], in1=nf[:], op=ALU.add, reads=[r_cs], writes=[r_cs])
                fw.op('act', 'activation', out=dst[:], in_=ang2[:], func=AF.Sin, reads=[r_cs], writes=[r_cs])
                if sign_ap is not None:
                    fw.op('dve', 'tensor_scalar', out=dst[:], in0=dst[:], scalar1=sign_ap, scalar2=None, op0=ALU.mult, reads=[r_cs], writes=[r_cs])

            cos2 = fw.sb("cos2", [64, S], F32)
            sin2 = fw.sb("sin2", [64, S], F32)
            sin_of(cos2, math.pi / 2.0, None)
            sin_of(sin2, 0.0, sgn[:, 0:1])
            r_CS = fw.res()
            fw.dma('sp', CS[0], cos2[:], reads=[r_cs], writes=[r_CS])
            fw.dma('sp', CS[1], sin2[:], reads=[r_cs], writes=[r_CS])
            fw.end()

        def rot_copy(dst, src_t, r_w):
            sv = src_t.rearrange("p k (h two f) -> p (k h) two f", two=2, f=32)
            dv = dst.rearrange("p k (h two f) -> p (k h) two f", two=2, f=32)
            fw.op('pool', 'tensor_copy', out=dv[:, :, 0, :], in_=sv[:, :, 1, :], reads=[r_w], writes=[r_w])
            fw.op('pool', 'tensor_copy', out=dv[:, :, 1, :], in_=sv[:, :, 0, :], reads=[r_w], writes=[r_w])

        def phase_swa(l, grp):
            fw.begin()
            banks = [fw.ps("bk%d" % i, [128, 512]) for i in range(8)]
            r_bk = [fw.res(True) for _ in range(8)]
            wq = fw.sb("wq", [128, KD, 256], BF16)
            wqr = fw.sb("wqr", [128, KD, 256], BF16)
            wk = fw.sb("wk", [128, KD, 64], BF16)
            wkr = fw.sb("wkr", [128, KD, 64], BF16)
            wv = fw.sb("wv", [128, KD, 64], BF16)
            r_wq = fw.res()
            r_wk = fw.res()
            r_wv = fw.res()
            q0 = OFF_SQ + grp * 256
            k0 = OFF_SK + grp * 64
            v0 = OFF_SV + grp * 64
            load_w(wq[:], w_in[l, :, q0:q0 + 256], r_wq)
            load_w(wk[:], w_in[l, :, k0:k0 + 64], r_wk)
            load_w(wv[:], w_in[l, :, v0:v0 + 64], r_wv)
            rot_copy(wqr[:], wq[:], r_wq)
            rot_copy(wkr[:], wk[:], r_wk)
            cos2 = fw.sb("cos2", [64, S], F32)
            sin2 = fw.sb("sin2", [64, S], F32)
            esk = fw.sb("esk", [128, 8], F32)
            r_cs = fw.res()
            r_es = fw.res()
            fw.dma('sp', cos2[:], CS[0], writes=[r_cs])
            fw.dma('sp', sin2[:], CS[1], writes=[r_cs])
            fw.dma('sp', esk[:], swa_sink[l:l + 1, :].partition_broadcast(128), writes=[r_es])
            fw.op('act', 'activation', out=esk[:], in_=esk[:], func=AF.Exp, reads=[r_es], writes=[r_es])
            qT = fw.sb("qT", [64, 4, S], BF16)
            kT = fw.sb("kT", [64, 1, S], BF16)
            vx = fw.sb("vx", [128, NT, 1, 128], BF16)
            r_q = fw.res()
            r_k = fw.res()
            r_v = fw.res()
            proj_heads(banks, r_bk, kT, wk, 1, r_wk, r_k, rope=(cos2, sin2, r_cs), w_rot=wkr)
            proj_v(banks, r_bk, vx, wv, 1, r_wv, r_v)
            proj_heads(banks, r_bk, qT, wq, 4, r_wq, r_q, rope=(cos2, sin2, r_cs), w_rot=wqr)
            attention(banks, r_bk, 4, 64, qT, kT, lambda h: 0, vx, r_q, r_k, r_v, lambda i, h: None, r_es, True, (esk, grp * 4), (1, grp * 2))
            fw.end()

        def phase_fox_cum(l):
            fw.begin()
            wff = fw.sb("wff", [128, KD, 8], BF16)
            r_w = fw.res()
            load_w(wff[:], w_in[l, :, OFF_FF:OFF_FF + 8], r_w)
            bfb = fw.sb("bfb", [128, 8], F32)
            r_c = fw.res()
            fw.dma('sp', bfb[:], fox_bf[l:l + 1, :].partition_broadcast(128), writes=[r_c])
            psF = [fw.ps("psF%d" % i, [128, 8]) for i in range(2)]
            r_psF = [fw.res(True) for _ in range(2)]
            psC = [fw.ps("psC%d" % i, [128, 8]) for i in range(2)]
            r_psC = [fw.res(True) for _ in range(2)]
            psT = [fw.ps("psT%d" % i, [128, 8]) for i in range(2)]
            r_psT = [fw.res(True) for _ in range(2)]
            nls = fw.sb("nls", [128, NT, 8], F32)
            r_nls = [fw.res() for _ in range(NT)]
            r_cum = [fw.res() for _ in range(NT)]
            for t in range(NT):
                b = t % 2
                tok = slice(t * 128, (t + 1) * 128)
                for k in range(KD):
                    fw.op('pe', 'matmul', out=psF[b][:], lhsT=hT[:, k, tok], rhs=wff[:, k, :], start=(k == 0), stop=(k == KD - 1),
                          reads=[r_w], writes=[r_psF[b]])
                fw.op('dve', 'tensor_tensor', out=nls[:, t, :], in0=psF[b][:], in1=bfb[:], op=ALU.add, reads=[r_psF[b], r_c], writes=[r_nls[t]])
                fw.op('act', 'activation', out=nls[:, t, :], in_=nls[:, t, :], func=AF.Exp, scale=-1.0, reads=[r_nls[t]], writes=[r_nls[t]])
                fw.op('dve', 'tensor_scalar', out=nls[:, t, :], in0=nls[:, t, :], scalar1=1.0, scalar2=None, op0=ALU.add,
                      reads=[r_nls[t]], writes=[r_nls[t]])
                fw.op('act', 'activation', out=nls[:, t, :], in_=nls[:, t, :], func=AF.Ln, reads=[r_nls[t]], writes=[r_nls[t]])
                fw.op('pe', 'matmul', out=psC[b][:], lhsT=triu[:], rhs=nls[:, t, :], start=True, stop=True, reads=[r_nls[t]], writes=[r_psC[b]])
                fw.op('pe', 'matmul', out=psT[b][:], lhsT=onesf[:], rhs=nls[:, t, :], start=True, stop=True, reads=[r_nls[t]], writes=[r_psT[b]])
                if t == 0:
                    fw.op('dve', 'tensor_copy', out=ncum[:, t, :], in_=psC[b][:], reads=[r_psC[b]], writes=[r_cum[t]])
                    fw.op('dve', 'tensor_copy', out=tot[:, t, :], in_=psT[b][:], reads=[r_psT[b]], writes=[r_cum[t]])
                else:
                    fw.op('dve', 'tensor_tensor', out=ncum[:, t, :], in0=psC[b][:], in1=tot[:, t - 1, :], op=ALU.add,
                          reads=[r_psC[b], r_cum[t - 1]], writes=[r_cum[t]])
                    fw.op('dve', 'tensor_tensor', out=tot[:, t, :], in0=psT[b][:], in1=tot[:, t - 1, :], op=ALU.add,
                          reads=[r_psT[b], r_cum[t - 1]], writes=[r_cum[t]])
            r1 = fw.sb("r1", [128, NT, 8], F32)
            n8 = fw.sb("n8", [128, NT, 8], F32)
            r_h = fw.res()
            rr = [r_cum[NT - 1], r_h]
            fw.op('dve', 'memset', ap=KML[:, :, :, 3:6], constant=1.0, writes=[r_h])
            fw.op('dve', 'memset', ap=QML[:, :, :, 0:3], constant=8.0, reads=[r_h], writes=[r_h])
            fw.op('dve', 'tensor_copy', out=KML[:, :, :, 0], in_=ncum[:], reads=rr, writes=[r_h])
            fw.op('dve', 'tensor_tensor', out=r1[:], in0=ncum[:], in1=KML[:, :, :, 0], op=ALU.subtract, reads=rr, writes=[r_h])
            fw.op('dve', 'tensor_copy', out=KML[:, :, :, 1], in_=r1[:], reads=rr, writes=[r_h])
            fw.op('dve', 'tensor_tensor', out=r1[:], in0=r1[:], in1=KML[:, :, :, 1], op=ALU.subtract, reads=rr, writes=[r_h])
            fw.op('dve', 'tensor_copy', out=KML[:, :, :, 2], in_=r1[:], reads=rr, writes=[r_h])
            fw.op('dve', 'tensor_scalar', out=n8[:], in0=ncum[:], scalar1=-8.0, scalar2=None, op0=ALU.mult, reads=rr, writes=[r_h])
            fw.op('dve', 'tensor_copy', out=QML[:, :, :, 3], in_=n8[:], reads=rr, writes=[r_h])
            fw.op('dve', 'tensor_tensor', out=r1[:], in0=n8[:], in1=QML[:, :, :, 3], op=ALU.subtract, reads=rr, writes=[r_h])
            fw.op('dve', 'tensor_copy', out=QML[:, :, :, 4], in_=r1[:], reads=rr, writes=[r_h])
            fw.op('dve', 'tensor_tensor', out=r1[:], in0=r1[:], in1=QML[:, :, :, 4], op=ALU.subtract, reads=rr, writes=[r_h])
            fw.op('dve', 'tensor_copy', out=QML[:, :, :, 5], in_=r1[:], reads=rr, writes=[r_h])
            fw.end()

        def phase_fox(l, grp):
            h0 = grp * 4
            fw.begin()
            banks = [fw.ps("bk%d" % i, [128, 512]) for i in range(8)]
            r_bk = [fw.res(True) for _ in range(8)]
            wfq = fw.sb("wfq", [128, KD, 256], BF16)
            wfk = fw.sb("wfk", [128, KD, 256], BF16)
            wfv = fw.sb("wfv", [128, KD, 256], BF16)
            r_wq = fw.res()
            r_wk = fw.res()
            r_wv = fw.res()
            load_w(wfk[:], w_in[l, :, OFF_FK + h0 * 64:OFF_FK + h0 * 64 + 256], r_wk)
            load_w(wfv[:], w_in[l, :, OFF_FV + h0 * 64:OFF_FV + h0 * 64 + 256], r_wv)
            load_w(wfq[:], w_in[l, :, OFF_FQ + h0 * 64:OFF_FQ + h0 * 64 + 256], r_wq)
            qT = fw.sb("qT", [128, 4, S], BF16)
            kT = fw.sb("kT", [128, 4, S], BF16)
            vx = fw.sb("vx", [128, NT, 4, 128], BF16)
            r_q = fw.res()
            r_k = fw.res()
            r_ka = fw.res()
            r_v = fw.res()
            r_qa = fw.res()
            n = 0
            for (ML, dstT, r_a) in ((KML, kT, r_ka), (QML, qT, r_qa)):
                for h in range(4):
                    for tb in range(NB):
                        bb = 4 + n % 3
                        n += 1
                        for tt in range(4):
                            t = tb * 4 + tt
                            fw.op('pe', 'matmul', out=banks[bb][0:6, tt * 128:(tt + 1) * 128], lhsT=ML[:, t, h0 + h, :], rhs=ident[:],
                                  start=True, stop=True, writes=[r_bk[bb]])
                        fw.op('dve', 'tensor_copy', out=dstT[64:70, h, tb * 512:(tb + 1) * 512], in_=banks[bb][0:6, :],
                              reads=[r_bk[bb]], writes=[r_a])
            proj_heads(banks, r_bk, kT[0:64], wfk, 4, r_wk, r_k)
            proj_v(banks, r_bk, vx, wfv, 4, r_wv, r_v)
            proj_heads(banks, r_bk, qT[0:64], wfq, 4, r_wq, r_q)

            r_kk = fw.res()
            r_qq = fw.res()
            fw.op('pool', 'memset', ap=rec_dummy[:], constant=0.0, reads=[r_k, r_ka, r_q, r_qa], writes=[r_kk, r_qq])
            attention_blk(banks, r_bk, 4, 70, qT, kT, vx, r_qq, r_kk, r_v, (2, grp * 2))
            fw.end()

        def phase_gates(l):
            fw.begin()
            wG = [fw.sb("wG%d" % i, [128, KD, 512], BF16) for i in range(2)]
            r_wG = [fw.res() for _ in range(2)]
            psZ = [fw.ps("psZ%d" % i, [128, 512]) for i in range(4)]
            r_psZ = [fw.res(True) for _ in range(4)]
            sg = [fw.sb("sg%d" % i, [128, 512], BF16) for i in range(4)]
            r_sg = [fw.res() for _ in range(4)]
            r_SG = fw.res()
            n = 0
            for c in range(8):
                wb = c % 2
                load_w(wG[wb][:], w_in[l, :, OFF_G + c * 512:OFF_G + (c + 1) * 512], r_wG[wb])
                for t in range(NT):
                    b = n % 4
                    n += 1
                    tok = slice(t * 128, (t + 1) * 128)
                    for k in range(KD):
                        fw.op('pe', 'matmul', out=psZ[b][:], lhsT=hT[:, k, tok], rhs=wG[wb][:, k, :], start=(k == 0), stop=(k == KD - 1),
                              reads=[r_wG[wb]], writes=[r_psZ[b]])
                    fw.op('act', 'activation', out=sg[b][:], in_=psZ[b][:], func=AF.Sigmoid, reads=[r_psZ[b]], writes=[r_sg[b]])
                    fw.dma('sp', SG[tok, c * 512:(c + 1) * 512], sg[b][:], reads=[r_sg[b]], writes=[r_SG])
            fw.end()

        def phase_merge(l):
            fw.begin()
            wbr = fw.sb("wbr", [128, 16, D], BF16)
            wo = fw.sb("wo", [128, KD, D], BF16)
            r_w = fw.res()
            for n_ in range(4):
                fw.dma('pool', wbr[:, n_ * 4:(n_ + 1) * 4, :], w_branch[l, n_].rearrange("(k p) d -> p k d", p=128), writes=[r_w])
            load_w(wo[:], w_out[l], r_w)
            sgt = [fw.sb("sgt%d" % i, [128, 4096], BF16) for i in range(2)]
            r_sgt = [fw.res() for _ in range(2)]
            ot = [fw.sb("ot%d" % i, [128, 16, 128], BF16) for i in range(2)]
            r_ot = [fw.res() for _ in range(2)]
            psP = [fw.ps("psP%d" % i, [128, 512]) for i in range(3)]
            r_psP = [fw.res(True) for _ in range(3)]
            pT = fw.ps("pTm", [128, KD, 128], BF16)
            r_pT = fw.res(True)
            psY = [fw.ps("psY%d" % i, [128, 512]) for i in range(2)]
            r_psY = [fw.res(True) for _ in range(2)]
            mg = [fw.sb("mg%d" % i, [128, D], F32) for i in range(2)]
            r_mg = [fw.res() for _ in range(2)]
            tm = [fw.sb("tm%d" % i, [128, 512], F32) for i in range(2)]
            r_tm = [fw.res() for _ in range(2)]
            mb = [fw.sb("mb%d" % i, [128, D], BF16) for i in range(2)]
            r_mb = [fw.res() for _ in range(2)]
            mT = [fw.sb("mT%d" % i, [128, KD, 128], BF16) for i in range(2)]
            r_mT = [fw.res() for _ in range(2)]
            cnt = {'np': 0}

            def stageA(t):
                np_ = cnt['np']
                b = t % 2
                tok = slice(t * 128, (t + 1) * 128)
                fw.dma('sp', sgt[b][:], SG[tok, :], writes=[r_sgt[b]])
                for n_ in range(4):
                    fw.dma('sp', ot[b][:, n_ * 4:(n_ + 1) * 4, :], OT[n_, :, :, tok], writes=[r_ot[b]])
                for half in range(2):
                    hs = slice(half * 512, (half + 1) * 512)
                    for n_ in range(4):
                        pb = np_ % 3
                        np_ += 1
                        for k in range(4):
                            fw.op('pe', 'matmul', out=psP[pb][:], lhsT=ot[b][:, n_ * 4 + k, :], rhs=wbr[:, n_ * 4 + k, hs], start=(k == 0),
                                  stop=(k == 3), reads=[r_ot[b], r_w], writes=[r_psP[pb]])
                        gsl = slice(n_ * 1024 + half * 512, n_ * 1024 + (half + 1) * 512)
                        if n_ == 0:
                            fw.op('dve', 'tensor_tensor', out=mg[b][:, hs], in0=psP[pb][:], in1=sgt[b][:, gsl], op=ALU.mult,
                                  reads=[r_psP[pb], r_sgt[b]], writes=[r_mg[b]])
                        else:
                            tb = np_ % 2
                            fw.op('dve', 'tensor_tensor', out=tm[tb][:], in0=psP[pb][:], in1=sgt[b][:, gsl], op=ALU.mult,
                                  reads=[r_psP[pb], r_sgt[b]], writes=[r_tm[tb]])
                            if n_ < 3:
                                fw.op('dve', 'tensor_tensor', out=mg[b][:, hs], in0=mg[b][:, hs], in1=tm[tb][:], op=ALU.add,
                                      reads=[r_tm[tb], r_mg[b]], writes=[r_mg[b]])
                            else:
                                fw.op('dve', 'tensor_tensor', out=mb[b][:, hs], in0=mg[b][:, hs], in1=tm[tb][:], op=ALU.add,
                                      reads=[r_tm[tb], r_mg[b]], writes=[r_mb[b]])
                cnt['np'] = np_

            def stageB(t):
                b = t % 2
                for k in range(KD):
                    fw.op('pe', 'transpose', out=pT[:, k, :], in_=mb[b][:, k * 128:(k + 1) * 128], identity=ident[:],
                          reads=[r_mb[b]], writes=[r_pT])
                fw.op('act', 'activation', out=mT[b][:], in_=pT[:], func=AF.Copy, reads=[r_pT], writes=[r_mT[b]])
                for half in range(2):
                    hs = slice(half * 512, (half + 1) * 512)
                    for k in range(KD):
                        fw.op('pe', 'matmul', out=psY[half][:], lhsT=mT[b][:, k, :], rhs=wo[:, k, hs], start=(k == 0), stop=(k == KD - 1),
                              reads=[r_mT[b], r_w], writes=[r_psY[half]])
                    fw.op('dve', 'tensor_tensor', out=xs[:, t, hs], in0=psY[half][:], in1=xs[:, t, hs], op=ALU.add,
                          reads=[r_psY[half], r_xs[t]], writes=[r_xs[t]])
            stageA(0)
            for t in range(1, NT):
                stageA(t)
                stageB(t - 1)
            stageB(NT - 1)
            fw.end()

        def phase_ffn(wg_d, wu_d, wd_d, ne, gated):
            fw.begin()
            wg = [fw.sb("wg%d" % i, [128, KD, 512], BF16) for i in range(2)]
            wu = [fw.sb("wu%d" % i, [128, KD, 512], BF16) for i in range(2)]
            wd = [fw.sb("wd%d" % i, [128, 4, D], BF16) for i in range(2)]
            r_wgt = [fw.res() for _ in range(2)]
            psG = [fw.ps("fpG%d" % i, [128, 512]) for i in range(2)]
            psU = [fw.ps("fpU%d" % i, [128, 512]) for i in range(2)]
            psY = [fw.ps("fpY%d" % i, [128, 512]) for i in range(3)]
            r_psG = [fw.res(True) for _ in range(2)]
            r_psU = [fw.res(True) for _ in range(2)]
            r_psY = [fw.res(True) for _ in range(3)]
            sl = [fw.sb("sl%d" % i, [128, 512], F32) for i in range(2)]
            r_sl = [fw.res() for _ in range(2)]
            act = [fw.sb("act%d" % i, [128, 4, 512], BF16) for i in range(2)]
            r_act = [fw.res() for _ in range(2)]
            nw = 0
            nc_ = 0
            nb_ = 0
            ny = 0
            for e in range(ne):
                for r in range(NR):
                    w = nw % 2
                    nw += 1
                    fs = slice(r * 512, (r + 1) * 512)
                    load_w(wg[w][:], wg_d[e, :, fs], r_wgt[w])
                    load_w(wu[w][:], wu_d[e, :, fs], r_wgt[w])
                    fw.dma('pool', wd[w][:], wd_d[e, fs, :].rearrange("(k p) d -> p k d", p=128), writes=[r_wgt[w]])
                    for bk in range(NB):
                        ab = nb_ % 2
                        nb_ += 1
                        tokb = slice(bk * 512, (bk + 1) * 512)
                        for c in range(4):
                            pb = nc_ % 2
                            nc_ += 1
                            cs = slice(c * 128, (c + 1) * 128)
                            for k in range(KD):
                                fw.op('pe', 'matmul', out=psG[pb][:], lhsT=wg[w][:, k, cs], rhs=hT[:, k, tokb], start=(k == 0), stop=(k == KD - 1),
                                      reads=[r_wgt[w]], writes=[r_psG[pb]])
                            for k in range(KD):
                                fw.op('pe', 'matmul', out=psU[pb][:], lhsT=wu[w][:, k, cs], rhs=hT[:, k, tokb], start=(k == 0), stop=(k == KD - 1),
                                      reads=[r_wgt[w]], writes=[r_psU[pb]])
                            fw.op('act', 'activation', out=sl[pb][:], in_=psG[pb][:], func=AF.Silu, reads=[r_psG[pb]], writes=[r_sl[pb]])
                            fw.op('dve', 'tensor_tensor', out=act[ab][:, c, :], in0=psU[pb][:], in1=sl[pb][:], op=ALU.mult,
                                  reads=[r_psU[pb], r_sl[pb]], writes=[r_act[ab]])
                        for tt in range(4):
                            t = bk * 4 + tt
                            for half in range(2):
                                yb = ny % 3
                                ny += 1
                                hs = slice(half * 512, (half + 1) * 512)
                                for c in range(4):
                                    fw.op('pe', 'matmul', out=psY[yb][:], lhsT=act[ab][:, c, tt * 128:(tt + 1) * 128], rhs=wd[w][:, c, hs],
                                          start=(c == 0), stop=(c == 3), reads=[r_act[ab], r_wgt[w]], writes=[r_psY[yb]])
                                if gated:
                                    fw.op('dve', 'scalar_tensor_tensor', out=xs[:, t, hs], in0=psY[yb][:], scalar=Gt[:, t * 8 + e:t * 8 + e + 1], in1=xs[:, t, hs],
                                          op0=ALU.mult, op1=ALU.add, reads=[r_psY[yb], r_xs[t]], writes=[r_xs[t]])
                                else:
                                    fw.op('dve', 'tensor_tensor', out=xs[:, t, hs], in0=psY[yb][:], in1=xs[:, t, hs], op=ALU.add,
                                          reads=[r_psY[yb], r_xs[t]], writes=[r_xs[t]])
            fw.end()

        def phase_stash(seq):
            fw.begin()
            for t in range(NT):
                fw.dma(('sp', 'act')[t % 2], XS[seq, t * 128:(t + 1) * 128, :], xs[:, t, :], writes=[fw.res()])
            fw.end()

        def phase_route():
            fw.begin()
            X_ = mybir.AxisListType.X
            W8 = NTT * 8
            v3 = lambda ap: ap.rearrange("p (t e) -> p t e", e=8)
            Mb = fw.sb("Mb", [128, W8], BF16)
            onesb = fw.sb("onesb", [128, 128], BF16)
            r_c = fw.res()
            fw.op('pool', 'memset', ap=onesb[:], constant=1.0, writes=[r_c])
            r_M = fw.res()
            fw.op('dve', 'tensor_copy', out=Mb[:], in_=Mf[:], writes=[r_M])
            ps_rank = fw.ps("ps_rank", [128, W8])
            ps_tot = fw.ps("ps_tot", [128, W8])
            r_pr = fw.res(True)
            r_pt = fw.res(True)
            fw.op('pe', 'matmul', out=ps_rank[:], lhsT=mdiag[:], rhs=Mb[:], start=True, stop=True, reads=[r_M], writes=[r_pr])
            fw.op('pe', 'matmul', out=ps_tot[:], lhsT=onesb[:], rhs=Mb[:], start=True, stop=True, reads=[r_M, r_c], writes=[r_pt])
            tot_sb = fw.sb("tot_sb", [128, W8], F32)
            tp = fw.sb("tp", [128, W8 + 8], F32)
            r_t = fw.res()
            D_ = lambda name, **kw: fw.op('dve', name, reads=[r_t], writes=[r_t], **kw)
            fw.op('dve', 'tensor_copy', out=tot_sb[:], in_=ps_tot[:], reads=[r_pt], writes=[r_t])
            D_('memset', ap=tp[:, 0:8], constant=0.0)
            for tt in range(NTT):
                D_('tensor_tensor', out=tp[:, (tt + 1) * 8:(tt + 2) * 8], in0=tp[:, tt * 8:(tt + 1) * 8],
                   in1=tot_sb[:, tt * 8:(tt + 1) * 8], op=ALU.add)
            cnt = tp[:, W8:W8 + 8]
            nb = fw.sb("nb", [128, 8], F32)
            tmp8 = fw.sb("tmp8", [128, 8], F32)
            D_('memset', ap=nb[:], constant=0.0)
            for j in range(JMAX):
                D_('tensor_scalar', out=tmp8[:], in0=cnt, scalar1=float(j * BS), scalar2=None, op0=ALU.is_gt)
                D_('tensor_tensor', out=nb[:], in0=nb[:], in1=tmp8[:], op=ALU.add)
            bend = fw.sb("bend", [128, 8], F32)
            off = fw.sb("off", [128, 8], F32)
            D_('tensor_copy', out=bend[:, 0:1], in_=nb[:, 0:1])
            for e in range(1, 8):
                D_('tensor_tensor', out=bend[:, e:e + 1], in0=bend[:, e - 1:e], in1=nb[:, e:e + 1], op=ALU.add)
            D_('tensor_tensor', out=off[:], in0=bend[:], in1=nb[:], op=ALU.subtract)
            D_('tensor_scalar', out=off[:], in0=off[:], scalar1=float(BS), scalar2=None, op0=ALU.mult)
            slot = fw.sb("slot", [128, W8], F32)
            slotM = fw.sb("slotM", [128, W8], F32)
            valA = fw.sb("valA", [128, W8], F32)
            fw.op('dve', 'tensor_tensor', out=slot[:], in0=ps_rank[:], in1=Mf[:], op=ALU.subtract, reads=[r_pr, r_t], writes=[r_t])
            D_('tensor_tensor', out=slot[:], in0=slot[:], in1=tp[:, 0:W8], op=ALU.add)
            for tt in range(NTT):
                D_('tensor_tensor', out=slot[:, tt * 8:(tt + 1) * 8], in0=slot[:, tt * 8:(tt + 1) * 8], in1=off[:], op=ALU.add)
            D_('tensor_tensor', out=slotM[:], in0=slot[:], in1=Mf[:], op=ALU.mult)
            D_('tensor_scalar', out=valA[:], in0=Mf[:], scalar1=-1.0e6, scalar2=1.0e6, op0=ALU.mult, op1=ALU.add)
            D_('tensor_tensor', out=valA[:], in0=valA[:], in1=slot[:], op=ALU.add)
            sAf = fw.sb("sAf", [128, NTT], F32)
            sBf = fw.sb("sBf", [128, NTT], F32)
            eq = fw.sb("eq", [128, NTT], F32)
            D_('tensor_reduce', out=sAf[:], in_=v3(valA[:]), axis=X_, op=ALU.min)
            D_('tensor_reduce', out=sBf[:], in_=v3(slotM[:]), axis=X_, op=ALU.max)
            D_('tensor_copy', out=sAi[:], in_=sAf[:])
            D_('tensor_copy', out=sBi[:], in_=sBf[:])
            D_('tensor_reduce', out=gA[:], in_=v3(Gt[:]), axis=X_, op=ALU.add)
            D_('memset', ap=gB[:], constant=0.0)
            for e in range(NE):
                D_('tensor_tensor', out=eq[:], in0=v3(slotM[:])[:, :, e], in1=sBf[:], op=ALU.is_equal)
                D_('tensor_tensor', out=eq[:], in0=eq[:], in1=v3(Gt[:])[:, :, e], op=ALU.mult)
                D_('tensor_tensor', out=gB[:], in0=gB[:], in1=eq[:], op=ALU.add)
            D_('tensor_tensor', out=gA[:], in0=gA[:], in1=gB[:], op=ALU.subtract)
            jidx = fw.sb("jidx", [128, NBLK], F32)
            ej = fw.sb("ej", [128, NBLK], F32)
            tmpj = fw.sb("tmpj", [128, NBLK], F32)
            base14 = fw.sb("base14", [128, NR2], F32)
            idxf = fw.sb("idxf", [128, NBLK * NR2], F32)
            r_i = fw.res()
            fw.op('pool', 'iota', out=jidx[:], pattern=[[1, NBLK]], base=0, channel_multiplier=0,
                  allow_small_or_imprecise_dtypes=True, writes=[r_i])
            fw.op('pool', 'iota', out=base14[:], pattern=[[1, NR2]], base=0, channel_multiplier=NR2,
                  allow_small_or_imprecise_dtypes=True, writes=[r_i])
            D_('memset', ap=ej[:], constant=0.0)
            for e in range(NE):
                fw.op('dve', 'tensor_scalar', out=tmpj[:], in0=jidx[:], scalar1=bend[:, e:e + 1], scalar2=None, op0=ALU.is_ge,
                      reads=[r_t, r_i], writes=[r_t])
                D_('tensor_tensor', out=ej[:], in0=ej[:], in1=tmpj[:], op=ALU.add)
            D_('tensor_scalar', out=ej[:], in0=ej[:], scalar1=float(NE - 1), scalar2=float(128 * NR2), op0=ALU.min, op1=ALU.mult)
            for j in range(NBLK):
                fw.op('dve', 'tensor_scalar', out=idxf[:, j * NR2:(j + 1) * NR2], in0=base14[:], scalar1=ej[:, j:j + 1], scalar2=None,
                      op0=ALU.add, reads=[r_t, r_i], writes=[r_t])
            D_('tensor_copy', out=idxw[:], in_=idxf[:])
            fw.end()

        def phase_scatter():
            fw.begin()
            r_z = fw.res()
            fw.op('pool', 'memset', ap=hT[:], constant=0.0, writes=[r_z])
            rows_pp = NBLK * BS // 128
            hs_v = HS.rearrange("(p n) d -> p n d", p=128)
            zrows = (KD * S) // D
            hT_v = hT[:].rearrange("p k s -> p (k s)").rearrange("p (a d) -> p a d", d=D)
            n0 = 0
            qi = 0
            r_HS = fw.res()
            while n0 < rows_pp:
                n1 = min(rows_pp, n0 + zrows)
                fw.dma(('sp', 'act')[qi % 2], hs_v[:, n0:n1, :], hT_v[:, 0:n1 - n0, :], reads=[r_z], writes=[r_HS])
                qi += 1
                n0 = n1
            hb = [fw.sb("hb%d" % i, [128, D], BF16) for i in range(8)]
            r_hb = [fw.res() for _ in range(8)]
            for tt in range(NTT):
                b = tt % 8
                fw.dma('sp', hb[b][:], HTK[tt * 128:(tt + 1) * 128, :], writes=[r_hb[b]])
                for sI in (sAi, sBi):
                    fw.dma('pool', HS[:, :], hb[b][:], reads=[r_hb[b], r_HS], writes=[fw.res()], _meth='indirect_dma_start',
                           out_offset=IOA(ap=sI[:, tt:tt + 1], axis=0), in_offset=None)
            fw.end()

        def phase_moe_sparse():
            fw.begin()
            NWB = 3
            wg = [fw.sb("wg%d" % i, [128, KD, 512], BF16) for i in range(NWB)]
            wu = [fw.sb("wu%d" % i, [128, KD, 512], BF16) for i in range(NWB)]
            wd = [fw.sb("wd%d" % i, [128, 4, D], BF16) for i in range(NWB)]
            r_wgt = [fw.res() for _ in range(NWB)]
            psG = [fw.ps("fpG%d" % i, [128, 512]) for i in range(2)]
            psU = [fw.ps("fpU%d" % i, [128, 512]) for i in range(2)]
            psY = [fw.ps("fpY%d" % i, [128, 512]) for i in range(3)]
            pT = fw.ps("spT", [128, KD, 128], BF16)
            r_psG = [fw.res(True) for _ in range(2)]
            r_psU = [fw.res(True) for _ in range(2)]
            r_psY = [fw.res(True) for _ in range(3)]
            r_pT = fw.res(True)
            sl = [fw.sb("sl%d" % i, [128, 512], F32) for i in range(2)]
            r_sl = [fw.res() for _ in range(2)]
            act = [fw.sb("act%d" % i, [128, 4, 512], BF16) for i in range(2)]
            r_act = [fw.res() for _ in range(2)]
            hsb = [fw.sb("hsb%d" % i, [128, D], BF16) for i in range(SB)]
            r_hsb = [fw.res() for _ in range(SB)]
            r_hTs = [fw.res() for _ in range(2)]
            r_ys = [[fw.res() for _ in range(SB)] for _ in range(2)]
            nw = 0
            nc_ = 0
            nb_ = 0
            ny = 0
            nh = 0
            NTB = BS // TBW
            TPB = TBW // 128

            def prep_load(j):
                for st in range(SB):
                    fw.dma(('sp', 'act')[st % 2], hsb[st][:], HS[j * BS + st * 128:j * BS + (st + 1) * 128, :], writes=[r_hsb[st]])

            def prep_tr(j):
                c0_ = (j % 2) * BS
                for st in range(SB):
                    for k in range(KD):
                        fw.op('pe', 'transpose', out=pT[:, k, :], in_=hsb[st][:, k * 128:(k + 1) * 128], identity=ident[:],
                              reads=[r_hsb[st]], writes=[r_pT])
                    if st % 2 == 0:
                        fw.op('dve', 'tensor_copy', out=hT[:, :, c0_ + st * 128:c0_ + (st + 1) * 128], in_=pT[:],
                              reads=[r_pT], writes=[r_hTs[j % 2]])
                    else:
                        fw.op('act', 'activation', out=hT[:, :, c0_ + st * 128:c0_ + (st + 1) * 128], in_=pT[:], func=AF.Copy,
                              reads=[r_pT], writes=[r_hTs[j % 2]])

            prep_load(0)
            prep_tr(0)
            for j in range(NBLK):
                jb = j % 2
                c0 = jb * BS
                for r in range(NR):
                    if j + 1 < NBLK:
                        if r == max(NR - 2, 0):
                            prep_load(j + 1)
                        if r == NR - 1:
                            prep_tr(j + 1)
                    w = nw % NWB
                    nw += 1
                    ic = j * NR + r
                    io = IOA(ap=idxw[:, ic:ic + 1], axis=0)
                    fw.dma('pool', wg[w][:].rearrange("p k f -> p (k f)"), exp_w_gate[:, :], writes=[r_wgt[w]],
                           _meth='indirect_dma_start', out_offset=None, in_offset=io)
                    fw.dma('pool', wu[w][:].rearrange("p k f -> p (k f)"), exp_w_up[:, :], writes=[r_wgt[w]],
                           _meth='indirect_dma_start', out_offset=None, in_offset=io)
                    fw.dma('pool', wd[w][:].rearrange("p c d -> p (c d)"), exp_w_down[:, :], writes=[r_wgt[w]],
                           _meth='indirect_dma_start', out_offset=None, in_offset=io)
                    for tb in range(NTB):
                        ab = nb_ % 2
                        nb_ += 1
                        tokb = slice(c0 + tb * TBW, c0 + (tb + 1) * TBW)
                        for c in range(4):
                            pb = nc_ % 2
                            nc_ += 1
                            cs = slice(c * 128, (c + 1) * 128)
                            for k in range(KD):
                                fw.op('pe', 'matmul', out=psG[pb][:, 0:TBW], lhsT=wg[w][:, k, cs], rhs=hT[:, k, tokb], start=(k == 0), stop=(k == KD - 1),
                                      reads=[r_wgt[w], r_hTs[jb]], writes=[r_psG[pb]])
                            for k in range(KD):
                                fw.op('pe', 'matmul', out=psU[pb][:, 0:TBW], lhsT=wu[w][:, k, cs], rhs=hT[:, k, tokb], start=(k == 0), stop=(k == KD - 1),
                                      reads=[r_wgt[w], r_hTs[jb]], writes=[r_psU[pb]])
                            fw.op('act', 'activation', out=sl[pb][:, 0:TBW], in_=psG[pb][:, 0:TBW], func=AF.Silu, reads=[r_psG[pb]], writes=[r_sl[pb]])
                            fw.op('dve', 'tensor_tensor', out=act[ab][:, c, 0:TBW], in0=psU[pb][:, 0:TBW], in1=sl[pb][:, 0:TBW], op=ALU.mult,
                                  reads=[r_psU[pb], r_sl[pb]], writes=[r_act[ab]])
                        for tt in range(TPB):
                            st = tb * TPB + tt
                            for half in range(2):
                                yb = ny % 3
                                ny += 1
                                hs = slice(half * 512, (half + 1) * 512)
                                for c in range(4):
                                    fw.op('pe', 'matmul', out=psY[yb][:], lhsT=act[ab][:, c, tt * 128:(tt + 1) * 128], rhs=wd[w][:, c, hs],
                                          start=(c == 0), stop=(c == 3), reads=[r_act[ab], r_wgt[w]], writes=[r_psY[yb]])
                                dst = xs[:, jb * SB + st, hs]
                                if r == 0:
                                    fw.op('act', 'activation', out=dst, in_=psY[yb][:], func=AF.Copy, reads=[r_psY[yb]], writes=[r_ys[jb][st]])
                                else:
                                    fw.op('dve', 'tensor_tensor', out=dst, in0=psY[yb][:], in1=dst, op=ALU.add,
                                          reads=[r_psY[yb], r_ys[jb][st]], writes=[r_ys[jb][st]])
                fw.dma('sp', YS[j * BS:(j + 1) * BS, :].rearrange("(st p) d -> p st d", p=128), xs[:, jb * SB:(jb + 1) * SB, :],
                       reads=r_ys[jb], writes=[fw.res()])
            fw.end()

        def phase_final2(seq):
            fw.begin()
            gfb = fw.sb("gfb", [128, D], F32)
            r_g = fw.res()
            fw.dma('sp', gfb[:], norm_final_g[0:1, :].partition_broadcast(128), writes=[r_g])
            ss = fw.sb("ss", [128, NT], F32)
            junk = [fw.sb("junk%d" % i, [128, D], BF16) for i in range(2)]
            yo = [fw.sb("yo%d" % i, [128, D], F32) for i in range(2)]
            NG = 6
            xt = [fw.sb("xt%d" % i, [128, D], F32) for i in range(NG)]
            ya = [fw.sb("ya%d" % i, [128, D], F32) for i in range(NG)]
            yb_ = [fw.sb("yb%d" % i, [128, D], F32) for i in range(NG)]
            r_j = [fw.res() for _ in range(2)]
            r_y = [fw.res() for _ in range(2)]
            r_xt = [fw.res() for _ in range(NG)]
            r_ya = [fw.res() for _ in range(NG)]
            r_yb = [fw.res() for _ in range(NG)]
            r_o = fw.res()
            def loads(t):
                g_ = t % NG
                tt = seq * NT + t
                fw.dma(('sp', 'act')[t % 2], xt[g_][:], XS[seq, t * 128:(t + 1) * 128, :], writes=[r_xt[g_]])
                fw.dma('pool', ya[g_][:], YS[:, :], writes=[r_ya[g_]], _meth='indirect_dma_start', out_offset=None,
                       in_offset=IOA(ap=sAi[:, tt:tt + 1], axis=0))
                fw.dma('pool', yb_[g_][:], YS[:, :], writes=[r_yb[g_]], _meth='indirect_dma_start', out_offset=None,
                       in_offset=IOA(ap=sBi[:, tt:tt + 1], axis=0))

            for t in range(min(NG - 1, NT)):
                loads(t)
            for t in range(NT):
                if t + NG - 1 < NT:
                    loads(t + NG - 1)
                b = t % 2
                g_ = t % NG
                tt = seq * NT + t
                r_s = fw.res()
                fw.op('dve', 'scalar_tensor_tensor', out=xt[g_][:], in0=ya[g_][:], scalar=gA[:, tt:tt + 1], in1=xt[g_][:],
                      op0=ALU.mult, op1=ALU.add, reads=[r_ya[g_], r_xt[g_]], writes=[r_xt[g_]])
                fw.op('dve', 'scalar_tensor_tensor', out=xt[g_][:], in0=yb_[g_][:], scalar=gB[:, tt:tt + 1], in1=xt[g_][:],
                      op0=ALU.mult, op1=ALU.add, reads=[r_yb[g_], r_xt[g_]], writes=[r_xt[g_]])
                fw.op('act', 'activation', out=junk[b][:], in_=xt[g_][:], func=AF.Square, accum_out=ss[:, t:t + 1],
                      reads=[r_xt[g_]], writes=[r_j[b], r_s])
                fw.op('act', 'activation', out=ss[:, t:t + 1], in_=ss[:, t:t + 1], func=AF.Ln, scale=1.0 / D, bias=epsc[:, 0:1],
                      reads=[r_s], writes=[r_s])
                fw.op('act', 'activation', out=ss[:, t:t + 1], in_=ss[:, t:t + 1], func=AF.Exp, scale=-0.5, reads=[r_s], writes=[r_s])
                fw.op('act', 'activation', out=yo[b][:], in_=xt[g_][:], func=AF.Copy, scale=ss[:, t:t + 1], reads=[r_s, r_xt[g_]], writes=[r_y[b]])
                fw.op('dve', 'tensor_tensor', out=yo[b][:], in0=yo[b][:], in1=gfb[:], op=ALU.mult, reads=[r_y[b], r_g], writes=[r_y[b]])
                fw.dma('sp', out_d[seq, t * 128:(t + 1) * 128, :], yo[b][:], reads=[r_y[b]], writes=[r_o])
            fw.end()

        def phase_load_x(seq):
            fw.begin()
            for t in range(NT):
                fw.dma('sp', xs[:, t, :], x_d[seq, t * 128:(t + 1) * 128, :], writes=[fw.res()])
            fw.end()

        def phase_final(seq):
            fw.begin()
            gfb = fw.sb("gfb", [128, D], F32)
            r_g = fw.res()
            fw.dma('sp', gfb[:], norm_final_g[0:1, :].partition_broadcast(128), writes=[r_g])
            ss = fw.sb("ss", [128, NT], F32)
            junk = [fw.sb("junk%d" % i, [128, D], BF16) for i in range(2)]
            yo = [fw.sb("yo%d" % i, [128, D], F32) for i in range(2)]
            r_j = [fw.res() for _ in range(2)]
            r_y = [fw.res() for _ in range(2)]
            r_o = fw.res()
            for t in range(NT):
                b = t % 2
                r_s = fw.res()
                fw.op('act', 'activation', out=junk[b][:], in_=xs[:, t, :], func=AF.Square, accum_out=ss[:, t:t + 1], writes=[r_j[b], r_s])
                fw.op('act', 'activation', out=ss[:, t:t + 1], in_=ss[:, t:t + 1], func=AF.Ln, scale=1.0 / D, bias=epsc[:, 0:1],
                      reads=[r_s], writes=[r_s])
                fw.op('act', 'activation', out=ss[:, t:t + 1], in_=ss[:, t:t + 1], func=AF.Exp, scale=-0.5, reads=[r_s], writes=[r_s])
                fw.op('act', 'activation', out=yo[b][:], in_=xs[:, t, :], func=AF.Copy, scale=ss[:, t:t + 1], reads=[r_s], writes=[r_y[b]])
                fw.op('dve', 'tensor_tensor', out=yo[b][:], in0=yo[b][:], in1=gfb[:], op=ALU.mult, reads=[r_y[b], r_g], writes=[r_y[b]])
                fw.dma('sp', out_d[seq, t * 128:(t + 1) * 128, :], yo[b][:], reads=[r_y[b]], writes=[r_o])
            fw.end()

        for seq in range(NSEQ):
            r_xs = [Res() for _ in range(NT)]
            phase_load_x(seq)
            phase_rope_tables(seq)
            for l in range(DEPTH):
                for r_ in r_xs:
                    r_.w = None
                    r_.rd = []
                phase_norm(norm_mix_g[l])
                phase_gmlp(l)
                phase_conv(l)
                phase_swa(l, 0)
                phase_swa(l, 1)
                phase_fox_cum(l)
                phase_fox(l, 0)
                phase_fox(l, 1)
                phase_gates(l)
                for r_ in r_xs:
                    r_.w = None
                    r_.rd = []
                phase_merge(l)
                for r_ in r_xs:
                    r_.w = None
                    r_.rd = []
                j = l // 2
                if l % 2 == 0:
                    phase_norm(norm_ffn_g[l])
                    for r_ in r_xs:
                        r_.w = None
                        r_.rd = []
                    phase_ffn(ffn_w_gate[j:j + 1], ffn_w_up[j:j + 1], ffn_w_down[j:j + 1], 1, False)
                elif sparse:
                    phase_norm(norm_ffn_g[l], moe_router=router_w[j], seq=seq, g_row2=norm_ffn_g[l:l + 1, :])
                    phase_stash(seq)
                else:
                    phase_norm(norm_ffn_g[l], moe_router=router_w[j])
                    for r_ in r_xs:
                        r_.w = None
                        r_.rd = []
                    phase_ffn(exp_w_gate[j], exp_w_up[j], exp_w_down[j], NE, True)
            for r_ in r_xs:
                r_.w = None
                r_.rd = []
            if not sparse:
                phase_final(seq)
        if sparse:
            LV = 9
            if LV >= 1:
                phase_route()
            if LV >= 2:
                phase_scatter()
            if LV >= 3:
                phase_moe_sparse()
            if LV >= 4:
                for seq in range(NSEQ):
                    phase_final2(seq)
        print("built: n_inst", fw.n_inst, "n_wait", fw.n_wait)
    return nc


_NC_CACHE = {}

_PARAMS = ['norm_mix_g', 'w_in', 'gmlp_ln_g', 'gmlp_ln_b', 'gmlp_ws', 'gmlp_bs', 'swa_sink', 'fox_bf', 'conv_w', 'conv_b',
           'conv_ln_g', 'conv_ln_b', 'w_branch', 'w_out', 'norm_ffn_g', 'ffn_w_gate', 'ffn_w_up', 'ffn_w_down', 'router_w',
           'exp_w_gate', 'exp_w_up', 'exp_w_down']


def relayout_experts(wg, wu, wd):
    NE, D_, DFF = wg.shape
    NR = DFF // 512

    def gu(w):
        a = np.asarray(w, dtype=np.float32).reshape(NE, 8, 128, NR, 512).transpose(0, 2, 3, 1, 4)
        return np.ascontiguousarray(a).reshape(NE * 128 * NR, 4096)

    b = np.asarray(wd, dtype=np.float32).reshape(NE, NR, 4, 128, D_).transpose(0, 3, 1, 2, 4)
    return gu(wg), gu(wu), np.ascontiguousarray(b).reshape(NE * 128 * NR, 4096)


def kernel(**inputs):
    n = 8
    x = np.ascontiguousarray(np.asarray(inputs['x'], dtype=np.float32))
    pos = np.ascontiguousarray(np.asarray(inputs['positions'], dtype=np.int32))
    B, S, _ = x.shape
    per = B // n
    if 'nc' not in _NC_CACHE:
        _NC_CACHE['nc'] = build(NSEQ=per, S=S)
    nc = _NC_CACHE['nc']
    shared = {k: np.ascontiguousarray(np.asarray(inputs[k], dtype=np.float32)) for k in _PARAMS}
    shared['norm_final_g'] = np.ascontiguousarray(np.asarray(inputs['norm_final_g'], dtype=np.float32)).reshape(1, -1)
    shared['exp_w_gate'], shared['exp_w_up'], shared['exp_w_down'] = relayout_experts(
        shared['exp_w_gate'][0], shared['exp_w_up'][0], shared['exp_w_down'][0])
    in_maps = []
    for c in range(n):
        m = dict(shared)
        m['x'] = x[c * per:(c + 1) * per]
        m['positions'] = pos[c * per:(c + 1) * per]
        in_maps.append(m)
    res = run_bass_kernel_spmd(nc, in_maps, core_ids=list(range(n)))
    return np.concatenate([r['out'] for r in res.results], axis=0).astype(np.float32)
```

```python
import math
import numpy as np
from contextlib import ExitStack
import concourse.bass as bass
import concourse.mybir as mybir
from concourse.bass_utils import run_bass_kernel_spmd

F32 = mybir.dt.float32
BF16 = mybir.dt.bfloat16
I32 = mybir.dt.int32
AF = mybir.ActivationFunctionType
ALU = mybir.AluOpType

CE = ('pe', 'act', 'dve', 'pool')
NSLOT = 8


class Res:
    __slots__ = ('w', 'rd', 'psum', 'extra')

    def __init__(self, psum=False):
        self.w = None
        self.rd = []
        self.psum = psum
        self.extra = []


class Tok:
    __slots__ = ('key', 'val', 'clock')

    def __init__(self, key, val, clock):
        self.key = key
        self.val = val
        self.clock = clock


class FW:
    def __init__(self, nc, es):
        self.nc = nc
        self.eng = ('pe', 'act', 'dve', 'pool', 'sp')
        self.sem = {}
        self.cnt = {}
        for e in CE:
            self.sem[e] = es.enter_context(nc.semaphore('c_' + e))
            self.cnt[e] = 0
        self.dq = ('sp', 'act', 'pool')
        self.slot_i = {q: 0 for q in self.dq}
        self.nslot = {'sp': NSLOT, 'act': NSLOT, 'pool': 2 * NSLOT}
        for q in self.dq:
            for s in range(self.nslot[q]):
                k = 'd_%s_%d' % (q, s)
                self.sem[k] = es.enter_context(nc.semaphore(k))
                self.cnt[k] = 0
        self.seen = {e: {} for e in self.eng}
        self.prog = {e: [] for e in self.eng}
        self.all_res = []
        self.pes = None
        self.n_inst = 0
        self.n_wait = 0
        self.uid = 0
        self.cond = None

    def res(self, psum=False):
        r = Res(psum)
        self.all_res.append(r)
        return r

    def begin(self):
        self.pes = ExitStack()
        self.all_res = []

    def sb(self, name, shape, dt):
        self.uid += 1
        return self.pes.enter_context(self.nc.sbuf_tensor("%s_%d" % (name, self.uid), list(shape), dt))

    def ps(self, name, shape, dt=F32):
        self.uid += 1
        return self.pes.enter_context(self.nc.psum_tensor("%s_%d" % (name, self.uid), list(shape), dt))

    def _deps(self, e, reads, writes):
        deps = []
        for r in reads:
            if r.w is not None:
                deps.append(r.w)
            if r.psum:
                deps.extend(t for t in r.rd if t.key != e)
            deps.extend(r.extra)
        for w in writes:
            if w.w is not None:
                deps.append(w.w)
            deps.extend(w.rd)
            deps.extend(w.extra)
        return deps

    def _wait(self, e, deps, skip_self=False):
        seen = self.seen[e]
        for t in sorted(deps, key=lambda t: -t.val):
            if skip_self and t.key == e:
                continue
            if seen.get(t.key, 0) >= t.val:
                continue
            self.prog[e].append(('w', self.sem[t.key], t.val))
            self.n_wait += 1
            seen[t.key] = t.val
            for k, v in t.clock.items():
                if seen.get(k, 0) < v:
                    seen[k] = v

    def _commit(self, tok, reads, writes):
        for r in reads:
            r.rd.append(tok)
            if len(r.rd) > 48:
                best = {}
                for t in r.rd:
                    if t.key not in best or best[t.key].val < t.val:
                        best[t.key] = t
                r.rd = list(best.values())
        for w in writes:
            if self.cond is not None:
                ex = w.extra + ([w.w] if w.w is not None else []) + w.rd
                best = {}
                for t in ex:
                    if t.key not in best or best[t.key].val < t.val:
                        best[t.key] = t
                w.extra = list(best.values())
            else:
                w.extra = []
            w.w = tok
            w.rd = []

    def op(self, e, name, reads=(), writes=(), **kw):
        deps = self._deps(e, reads, writes)
        self._wait(e, deps, skip_self=(e == 'pe'))
        self.cnt[e] += 1
        self.prog[e].append(('o', name, kw, self.sem[e], 1))
        self.n_inst += 1
        base = self.cond[e][0] if self.cond is not None else self.seen[e]
        clock = {k: v for k, v in base.items() if k in CE}
        tok = Tok(e, self.cnt[e], clock)
        self._commit(tok, reads, writes)
        return tok

    def dma(self, q, out, in_, reads=(), writes=(), _meth='dma_start', **kw):
        deps = self._deps(q, reads, writes)
        i = self.slot_i[q]
        self.slot_i[q] = (i + 1) % self.nslot[q]
        k = 'd_%s_%d' % (q, i)
        if self.cnt[k] > 0:
            deps.append(Tok(k, self.cnt[k], {}))
        self._wait(q, deps)
        self.cnt[k] += 16
        kw = dict(kw)
        kw['out'] = out
        kw['in_'] = in_
        self.prog[q].append(('o', _meth, kw, self.sem[k], 16))
        self.n_inst += 1
        base = self.cond[q][0] if self.cond is not None else self.seen[q]
        clock = {kk: v for kk, v in base.items() if kk in CE}
        tok = Tok(k, self.cnt[k], clock)
        self._commit(tok, reads, writes)
        return tok

    def cond_begin(self, flag_ap, r_flag):
        assert self.cond is None
        self.cond = {}
        for e in self.eng:
            deps = [r_flag.w] if r_flag.w is not None else []
            self._wait(e, deps)
            self.prog[e].append(('cb', flag_ap))
            self.cond[e] = (dict(self.seen[e]), dict(self.cnt))

    def cond_end(self):
        for e in self.eng:
            seen0, cnt0 = self.cond[e]
            fix = []
            if e in CE and self.cnt[e] != cnt0[e]:
                fix.append((self.sem[e], self.cnt[e] - cnt0[e]))
            if e in self.dq:
                for s in range(self.nslot[e]):
                    k = 'd_%s_%d' % (e, s)
                    if self.cnt[k] != cnt0[k]:
                        fix.append((self.sem[k], self.cnt[k] - cnt0[k]))
            self.prog[e].append(('ce', fix))
            self.seen[e] = seen0
        self.cond = None

    def end(self):
        deps = []
        for q in self.dq:
            for s in range(self.nslot[q]):
                k = 'd_%s_%d' % (q, s)
                if self.cnt[k] > 0:
                    deps.append(Tok(k, self.cnt[k], {}))
        self._wait('sp', deps)
        nc = self.nc
        prog = self.prog
        E_of = {'pe': nc.tensor, 'act': nc.scalar, 'dve': nc.vector, 'pool': nc.gpsimd, 'sp': nc.sync}

        def run(E, items):
            i = 0
            n = len(items)
            while i < n:
                it = items[i]
                if it[0] == 'w':
                    E.wait_ge(it[1], it[2])
                elif it[0] == 'o':
                    getattr(E, it[1])(**it[2]).then_inc(it[3], it[4])
                elif it[0] == 'cb':
                    j = i + 1
                    while items[j][0] != 'ce':
                        j += 1
                    inner = items[i + 1:j]
                    fix = items[j][1]
                    if any(x[0] == 'o' for x in inner):
                        val = E.value_load(it[1], min_val=0, max_val=1)
                        with E.If(val > 0):
                            run(E, inner)
                        with E.Else():
                            for (sem, amt) in fix:
                                E.drain().then_inc(sem, amt)
                    i = j
                i += 1

        with nc.Block() as block:
            @block.tensor
            def _(E):
                run(E, prog['pe'])

            @block.scalar
            def _(E):
                run(E, prog['act'])

            @block.vector
            def _(E):
                run(E, prog['dve'])

            @block.gpsimd
            def _(E):
                run(E, prog['pool'])

            @block.sync
            def _(E):
                run(E, prog['sp'])
        self.prog = {e: [] for e in self.eng}
        full = dict(self.cnt)
        for e in self.eng:
            self.seen[e] = dict(full)
        self.pes.close()
        self.pes = None
        self.all_res = []


D = 1024
KD = 8
HD = 64
OFF_ZU, OFF_ZV, OFF_SQ, OFF_SK, OFF_SV, OFF_FQ, OFF_FK, OFF_FV, OFF_FF, OFF_CA, OFF_CG, OFF_G = (
    0, 512, 1024, 1536, 1664, 1792, 2304, 2816, 3328, 3336, 3848, 4360)
N_IN = 8456
EPS = 1e-6
TAPS = 31
TWO_PI = 2.0 * math.pi


def build(NSEQ=2, S=2048, DFF=3584, NE=8, DEPTH=2, BS=None):
    NT = S // 128
    NTT = NSEQ * NT
    NTOK = NSEQ * S
    if BS is None:
        BS = min(512, S // 2)
    SB = BS // 128
    NBLK = (2 * NTOK + NE * (BS - 1)) // BS
    JMAX = (NTOK + BS - 1) // BS
    TBW = 384 if BS == 768 else min(512, BS)
    IOA = bass.IndirectOffsetOnAxis
    NB = S // 512
    NR = DFF // 512
    n_dense = (DEPTH + 1) // 2
    n_moe = DEPTH // 2
    sparse = (n_moe == 1 and DEPTH % 2 == 0)
    NR2 = NR
    nc = bass.Bass("TRN2", target_bir_lowering=False)
    dt = lambda name, shape, d=F32: nc.dram_tensor(name, list(shape), d, kind="ExternalInput").ap()
    x_d = dt("x", [NSEQ, S, D])
    pos_d = dt("positions", [NSEQ, S], I32)
    norm_mix_g = dt("norm_mix_g", [DEPTH, D])
    w_in = dt("w_in", [DEPTH, D, N_IN])
    gmlp_ln_g = dt("gmlp_ln_g", [DEPTH, 512])
    gmlp_ln_b = dt("gmlp_ln_b", [DEPTH, 512])
    gmlp_ws = dt("gmlp_ws", [DEPTH, 8, 128, 128])
    gmlp_bs = dt("gmlp_bs", [DEPTH, 8, 128])
    swa_sink = dt("swa_sink", [DEPTH, 8])
    fox_bf = dt("fox_bf", [DEPTH, 8])
    conv_w = dt("conv_w", [DEPTH, TAPS, 512])
    conv_b = dt("conv_b", [DEPTH, 512])
    conv_ln_g = dt("conv_ln_g", [DEPTH, 512])
    conv_ln_b = dt("conv_ln_b", [DEPTH, 512])
    w_branch = dt("w_branch", [DEPTH, 4, 512, D])
    w_out = dt("w_out", [DEPTH, D, D])
    norm_ffn_g = dt("norm_ffn_g", [DEPTH, D])
    ffn_w_gate = dt("ffn_w_gate", [n_dense, D, DFF])
    ffn_w_up = dt("ffn_w_up", [n_dense, D, DFF])
    ffn_w_down = dt("ffn_w_down", [n_dense, DFF, D])
    router_w = dt("router_w", [max(n_moe, 1), D, NE])
    if sparse:
        exp_w_gate = dt("exp_w_gate", [NE * 128 * NR2, 4096])
        exp_w_up = dt("exp_w_up", [NE * 128 * NR2, 4096])
        exp_w_down = dt("exp_w_down", [NE * 128 * NR2, 4096])
    else:
        exp_w_gate = dt("exp_w_gate", [max(n_moe, 1), NE, D, DFF])
        exp_w_up = dt("exp_w_up", [max(n_moe, 1), NE, D, DFF])
        exp_w_down = dt("exp_w_down", [max(n_moe, 1), NE, DFF, D])
    norm_final_g = dt("norm_final_g", [1, D])
    out_d = nc.dram_tensor("out", [NSEQ, S, D], F32, kind="ExternalOutput").ap()
    OT = nc.dram_tensor("ot_scr", [4, 128, 4, S], BF16, kind="Internal").ap()
    SG = nc.dram_tensor("sg_scr", [S, 4096], BF16, kind="Internal").ap()
    CS = nc.dram_tensor("cs_scr", [2, 64, S], F32, kind="Internal").ap()
    HTK = nc.dram_tensor("htk_scr", [NTOK, D], BF16, kind="Internal").ap()
    HS = nc.dram_tensor("hs_scr", [NBLK * BS, D], BF16, kind="Internal").ap()
    YS = nc.dram_tensor("ys_scr", [NBLK * BS, D], F32, kind="Internal").ap()
    XS = nc.dram_tensor("xs_scr", [NSEQ, S, D], F32, kind="Internal").ap()

    with ExitStack() as es:
        fw = FW(nc, es)
        sbp = lambda name, shape, d: es.enter_context(nc.sbuf_tensor(name, list(shape), d))
        xs = sbp("xs", [128, NT, D], F32)
        hT = sbp("hT", [128, KD, S], BF16)
        ident = sbp("ident", [128, 128], BF16)
        identf = sbp("identf", [128, 128], F32)
        onesf = sbp("onesf", [128, 128], F32)
        triu = sbp("triu", [128, 128], F32)
        mdiag = sbp("mdiag", [128, 128], BF16)
        mprev = sbp("mprev", [128, 128], BF16)
        mask2 = sbp("mask2", [128, 256], BF16)
        epsc = sbp("epsc", [128, 1], F32)
        rec_dummy = sbp("rec_dummy", [128, 1], F32)
        negpi = sbp("negpi", [128, 1], F32)
        Gt = sbp("Gt", [128, NTT * 8], F32)
        Mf = sbp("Mf", [128, NTT * 8], F32)
        sAi = sbp("sAi", [128, NTT], I32)
        sBi = sbp("sBi", [128, NTT], I32)
        gA = sbp("gA", [128, NTT], F32)
        gB = sbp("gB", [128, NTT], F32)
        idxw = sbp("idxw", [128, NBLK * NR2], I32)
        ncum = sbp("ncum", [128, NT, 8], F32)
        tot = sbp("tot", [128, NT, 8], F32)
        negtot = sbp("negtot", [128, NT, 8], F32)
        KML = sbp("KML", [128, NT, 8, 6], BF16)
        QML = sbp("QML", [128, NT, 8, 6], BF16)
        mbias = sbp("mbias", [128, 128], F32)

        fw.begin()
        r = fw.res()
        fw.op('pool', 'memset', ap=ident[:], constant=0.0, writes=[r])
        fw.op('pool', 'affine_select', out=ident[:], in_=ident[:], pattern=[[-1, 128]], compare_op=ALU.not_equal,
              fill=1.0, base=0, channel_multiplier=1, reads=[r], writes=[r])
        r = fw.res()
        fw.op('pool', 'memset', ap=identf[:], constant=0.0, writes=[r])
        fw.op('pool', 'affine_select', out=identf[:], in_=identf[:], pattern=[[-1, 128]], compare_op=ALU.not_equal,
              fill=1.0, base=0, channel_multiplier=1, reads=[r], writes=[r])
        fw.op('dve', 'memset', ap=onesf[:], constant=1.0, writes=[fw.res()])
        r = fw.res()
        fw.op('pool', 'memset', ap=triu[:], constant=1.0, writes=[r])
        fw.op('pool', 'affine_select', out=triu[:], in_=triu[:], pattern=[[1, 128]], compare_op=ALU.is_ge,
              fill=0.0, base=0, channel_multiplier=-1, reads=[r], writes=[r])
        r = fw.res()
        fw.op('pool', 'memset', ap=mdiag[:], constant=1.0, writes=[r])
        fw.op('pool', 'affine_select', out=mdiag[:], in_=mdiag[:], pattern=[[1, 128]], compare_op=ALU.is_ge,
              fill=0.0, base=0, channel_multiplier=-1, reads=[r], writes=[r])
        r_md = r
        r = fw.res()
        fw.op('pool', 'memset', ap=mprev[:], constant=1.0, writes=[r])
        fw.op('pool', 'affine_select', out=mprev[:], in_=mprev[:], pattern=[[-1, 128]], compare_op=ALU.is_gt,
              fill=0.0, base=0, channel_multiplier=1, reads=[r], writes=[r])
        r_mp = r
        r = fw.res()
        fw.op('pool', 'memset', ap=mbias[:], constant=0.0, writes=[r])
        fw.op('pool', 'affine_select', out=mbias[:], in_=mbias[:], pattern=[[1, 128]], compare_op=ALU.is_ge,
              fill=-30000.0, base=0, channel_multiplier=-1, reads=[r], writes=[r])
        r_m2 = fw.res()
        fw.op('pool', 'tensor_copy', out=mask2[:, 128:256], in_=mdiag[:], reads=[r_md], writes=[r_m2])
        fw.op('pool', 'tensor_copy', out=mask2[:, 0:128], in_=mprev[:], reads=[r_mp], writes=[r_m2])
        fw.op('dve', 'memset', ap=epsc[:], constant=EPS, writes=[fw.res()])
        fw.op('dve', 'memset', ap=negpi[:], constant=-math.pi, writes=[fw.res()])
        fw.end()

        def load_w(dst, src, res, q='pool'):
            fw.dma(q, dst, src.rearrange("(k p) n -> p k n", p=128), writes=[res])

        def phase_norm(g_row, moe_router=None, seq=0, g_row2=None):
            fw.begin()
            sp_ = sparse and moe_router is not None
            gT = fw.sb("gT", [128, KD], F32)
            r_g = fw.res()
            fw.dma('sp', gT[:], g_row.rearrange("(k p) -> p k", p=128), writes=[r_g], allow_slow_non_contiguous=True)
            ss = fw.sb("ss", [128, NT], F32)
            rstd = fw.sb("rstd", [128, NT], F32)
            junk = [fw.sb("junk%d" % i, [128, D], BF16) for i in range(2)]
            r_junk = [fw.res() for _ in range(2)]
            xn = [fw.sb("xn%d" % i, [128, D], BF16) for i in range(2)]
            r_xn = [fw.res() for _ in range(2)]
            pT = [fw.ps("pT%d" % i, [128, KD, 128], BF16) for i in range(2)]
            r_pT = [fw.res(True) for _ in range(2)]
            r_ss = [fw.res() for _ in range(NT)]
            r_hT = [fw.res() for _ in range(NT)]
            if moe_router is not None:
                wr = fw.sb("wr", [128, KD, NE], F32)
                r_wr = fw.res()
                fw.dma('sp', wr[:], moe_router.rearrange("(k p) e -> p k e", p=128), writes=[r_wr])
                for k in range(KD):
                    fw.op('dve', 'tensor_scalar', out=wr[:, k, :], in0=wr[:, k, :], scalar1=gT[:, k:k + 1], scalar2=None,
                          op0=ALU.mult, reads=[r_wr, r_g], writes=[r_wr])
                xf = [fw.sb("xf%d" % i, [128, D], F32) for i in range(2)]
                r_xf = [fw.res() for _ in range(2)]
                xfT = [fw.sb("xfT%d" % i, [128, KD, 128], F32) for i in range(2)]
                r_xfT = [fw.res() for _ in range(2)]
                pTf = [fw.ps("pTf%d" % i, [128, 4, 128], F32) for i in range(2)]
                r_pTf = [fw.res(True) for _ in range(2)]
                pL = fw.ps("pL", [128, NE], F32)
                r_pL = fw.res(True)
                lg = fw.sb("lg", [128, 8], F32)
                top = fw.sb("top", [128, 8], F32)
                nm1 = fw.sb("nm1", [128, 1], F32)
                ex = fw.sb("ex", [128, 8], F32)
                dd = fw.sb("dd", [128, 1], F32)
                msk = fw.sb("msk", [128, 8], F32)
                r_s = fw.res()
            if sp_:
                gfb_ = fw.sb("gfb_", [128, D], F32)
                fw.dma('sp', gfb_[:], g_row2.partition_broadcast(128), writes=[r_g])
                hrow = [fw.sb("hrow%d" % i, [128, D], BF16) for i in range(2)]
                r_hrow = [fw.res() for _ in range(2)]
            for t in range(NT):
                b = t % 2
                r_x = r_xs[t]
                fw.op('act', 'activation', out=junk[b][:], in_=xs[:, t, :], func=AF.Square, accum_out=ss[:, t:t + 1],
                      reads=[r_x], writes=[r_junk[b], r_ss[t]])
                fw.op('act', 'activation', out=rstd[:, t:t + 1], in_=ss[:, t:t + 1], func=AF.Ln, scale=1.0 / D,
                      bias=epsc[:, 0:1], reads=[r_ss[t]], writes=[r_ss[t]])
                fw.op('act', 'activation', out=rstd[:, t:t + 1], in_=rstd[:, t:t + 1], func=AF.Exp, scale=-0.5,
                      reads=[r_ss[t]], writes=[r_ss[t]])
                fw.op('act', 'activation', out=xn[b][:], in_=xs[:, t, :], func=AF.Copy, scale=rstd[:, t:t + 1],
                      reads=[r_x, r_ss[t]], writes=[r_xn[b]])
                for k in range(0 if sp_ else KD):
                    fw.op('pe', 'transpose', out=pT[b][:, k, :], in_=xn[b][:, k * 128:(k + 1) * 128], identity=ident[:],
                          reads=[r_xn[b]], writes=[r_pT[b]])
                for k in range(0 if sp_ else KD):
                    fw.op('dve', 'tensor_scalar', out=hT[:, k, t * 128:(t + 1) * 128], in0=pT[b][:, k, :],
                          scalar1=gT[:, k:k + 1], scalar2=None, op0=ALU.mult, reads=[r_pT[b], r_g], writes=[r_hT[t]])
                if sp_:
                    fw.op('pool', 'tensor_tensor', out=hrow[b][:], in0=xn[b][:], in1=gfb_[:], op=ALU.mult,
                          reads=[r_xn[b], r_g], writes=[r_hrow[b]])
                    fw.dma('sp', HTK[seq * S + t * 128:seq * S + (t + 1) * 128, :], hrow[b][:], reads=[r_hrow[b]], writes=[fw.res()])
                if moe_router is not None:
                    fw.op('act', 'activation', out=xf[b][:], in_=xs[:, t, :], func=AF.Copy, scale=rstd[:, t:t + 1],
                          reads=[r_x, r_ss[t]], writes=[r_xf[b]])
                    for hh in range(2):
                        for k4 in range(4):
                            k = hh * 4 + k4
                            fw.op('pe', 'matmul', out=pTf[hh][:, k4, :], lhsT=xf[b][:, k * 128:(k + 1) * 128], rhs=identf[:],
                                  start=True, stop=True, reads=[r_xf[b]], writes=[r_pTf[hh]])
                        fw.op('act', 'activation', out=xfT[b][:, hh * 4:(hh + 1) * 4, :], in_=pTf[hh][:], func=AF.Copy,
                              reads=[r_pTf[hh]], writes=[r_xfT[b]])
                    for k in range(KD):
                        fw.op('pe', 'matmul', out=pL[:], lhsT=xfT[b][:, k, :], rhs=wr[:, k, :], start=(k == 0), stop=(k == KD - 1),
                              reads=[r_xfT[b], r_wr], writes=[r_pL])
                    fw.op('dve', 'tensor_copy', out=lg[:, 0:NE], in_=pL[:], reads=[r_pL], writes=[r_s])
                    fw.op('dve', 'max', out=top[:], in_=lg[:, 0:NE], reads=[r_s], writes=[r_s])
                    fw.op('dve', 'tensor_scalar', out=nm1[:], in0=top[:, 0:1], scalar1=-1.0, scalar2=None, op0=ALU.mult,
                          reads=[r_s], writes=[r_s])
                    fw.op('act', 'activation', out=ex[:, 0:NE], in_=lg[:, 0:NE], func=AF.Exp, bias=nm1[:, 0:1], reads=[r_s], writes=[r_s])
                    fw.op('act', 'activation', out=dd[:], in_=top[:, 1:2], func=AF.Exp, bias=nm1[:, 0:1], reads=[r_s], writes=[r_s])
                    fw.op('dve', 'tensor_scalar', out=dd[:], in0=dd[:], scalar1=1.0, scalar2=None, op0=ALU.add, reads=[r_s], writes=[r_s])
                    fw.op('dve', 'reciprocal', out=dd[:], in_=dd[:], reads=[r_s], writes=[r_s])
                    fw.op('dve', 'tensor_scalar', out=msk[:, 0:NE], in0=lg[:, 0:NE], scalar1=top[:, 1:2], scalar2=None, op0=ALU.is_ge,
                          reads=[r_s], writes=[r_s])
                    tt_ = seq * NT + t if sparse else t
                    fw.op('dve', 'scalar_tensor_tensor', out=Gt[:, tt_ * 8:tt_ * 8 + NE], in0=ex[:, 0:NE], scalar=dd[:, 0:1], in1=msk[:, 0:NE],
                          op0=ALU.mult, op1=ALU.mult, reads=[r_s], writes=[r_s])
                    fw.op('dve', 'tensor_copy', out=Mf[:, tt_ * 8:tt_ * 8 + NE], in_=msk[:, 0:NE], reads=[r_s], writes=[r_s])
            fw.end()

        def phase_gmlp(l):
            fw.begin()
            wA = fw.sb("wA", [128, KD, 1024], BF16)
            r_wA = fw.res()
            load_w(wA[:, :, 0:512], w_in[l, :, OFF_ZU:OFF_ZU + 512], r_wA)
            load_w(wA[:, :, 512:1024], w_in[l, :, OFF_ZV:OFF_ZV + 512], r_wA)
            wsb = fw.sb("wsb", [128, 8, 128], BF16)
            r_ws = fw.res()
            fw.dma('pool', wsb[:], gmlp_ws[l].rearrange("g t s -> t g s"), writes=[r_ws])
            pW = fw.ps("pW", [128, 8, 128], BF16)
            r_pW = fw.res(True)
            for g in range(8):
                fw.op('pe', 'transpose', out=pW[:, g, :], in_=wsb[:, g, :], identity=ident[:], reads=[r_ws], writes=[r_pW])
            wsT = fw.sb("wsT", [128, 8, 128], BF16)
            r_wsT = fw.res()
            fw.op('dve', 'tensor_copy', out=wsT[:], in_=pW[:], reads=[r_pW], writes=[r_wsT])
            fw.op('pool', 'affine_select', out=wsT[:], in_=wsT[:], pattern=[[0, 8], [1, 128]], compare_op=ALU.is_ge, fill=0.0,
                  base=0, channel_multiplier=-1, reads=[r_wsT], writes=[r_wsT])
            bsT = fw.sb("bsT", [128, 8], F32)
            r_c = fw.res()
            fw.dma('sp', bsT[:], gmlp_bs[l].rearrange("g t -> t g"), writes=[r_c], allow_slow_non_contiguous=True)
            lng = fw.sb("lng", [128, 512], F32)
            lnb = fw.sb("lnb", [128, 512], F32)
            fw.dma('sp', lng[:], gmlp_ln_g[l:l + 1, :].partition_broadcast(128), writes=[r_c])
            fw.dma('sp', lnb[:], gmlp_ln_b[l:l + 1, :].partition_broadcast(128), writes=[r_c])
            psUV = [fw.ps("psUV%d" % i, [128, 1024]) for i in range(2)]
            psU = [p[:, 0:512] for p in psUV]
            psV = [p[:, 512:1024] for p in psUV]
            psM = fw.ps("psM", [128, 512])
            pT2 = pW[:, 0:4, :]
            r_psU = [fw.res(True) for _ in range(2)]
            r_psV = r_psU
            r_psM = fw.res(True)
            r_pT2 = r_pW
            u = [fw.sb("u%d" % i, [128, 512], F32) for i in range(3)]
            gv = [fw.sb("gv%d" % i, [128, 512], F32) for i in range(3)]
            sq = [fw.sb("sq%d" % i, [128, 512], BF16) for i in range(3)]
            vn = [fw.sb("vn%d" % i, [128, 512], F32) for i in range(3)]
            vb = [fw.sb("vb%d" % i, [128, 512], BF16) for i in range(3)]
            oa = [fw.sb("oa%d" % i, [128, 512], BF16) for i in range(3)]
            st = [fw.sb("st%d" % i, [128, 8], F32) for i in range(3)]
            r_u = [fw.res() for _ in range(3)]
            r_gv = [fw.res() for _ in range(3)]
            r_sq = [fw.res() for _ in range(3)]
            r_vn = [fw.res() for _ in range(3)]
            r_vb = [fw.res() for _ in range(3)]
            r_oa = [fw.res() for _ in range(3)]
            r_st = [fw.res() for _ in range(3)]
            oT = [fw.sb("oT%d" % i, [128, 4, 512], BF16) for i in range(2)]
            r_oT = [fw.res() for _ in range(2)]
            r_OT = fw.res()
            gx = [fw.sb("gx%d" % i, [128, 1024], F32) for i in range(2)]
            gt = [fw.sb("gt%d" % i, [128, 1024], F32) for i in range(2)]
            r_gx = [fw.res() for _ in range(2)]
            r_gt = [fw.res() for _ in range(2)]
            gcnt = [0]

            def gelu2(b, bb, acc_ap, r_acc):
                i = gcnt[0] % 2
                gcnt[0] += 1
                fw.op('act', 'activation', out=gx[i][:, 0:512], in_=psU[b], func=AF.Copy, reads=[r_psU[b]], writes=[r_gx[i]])
                fw.op('act', 'activation', out=gx[i][:, 512:1024], in_=psV[b], func=AF.Copy, reads=[r_psU[b]], writes=[r_gx[i]])
                fw.op('act', 'activation', out=gt[i][:], in_=gx[i][:], func=AF.Square, reads=[r_gx[i]], writes=[r_gt[i]])
                fw.op('dve', 'tensor_scalar', out=gt[i][:], in0=gt[i][:], scalar1=0.044715, scalar2=1.0, op0=ALU.mult, op1=ALU.add,
                      reads=[r_gt[i]], writes=[r_gt[i]])
                fw.op('dve', 'tensor_tensor', out=gt[i][:], in0=gt[i][:], in1=gx[i][:], op=ALU.mult, reads=[r_gt[i], r_gx[i]], writes=[r_gt[i]])
                fw.op('act', 'activation', out=gt[i][:], in_=gt[i][:], func=AF.Exp, scale=-1.5957691216057308, reads=[r_gt[i]], writes=[r_gt[i]])
                fw.op('dve', 'tensor_scalar', out=gt[i][:], in0=gt[i][:], scalar1=1.0, scalar2=None, op0=ALU.add, reads=[r_gt[i]], writes=[r_gt[i]])
                fw.op('dve', 'reciprocal', out=gt[i][:], in_=gt[i][:], reads=[r_gt[i]], writes=[r_gt[i]])
                fw.op('dve', 'tensor_tensor', out=u[bb][:], in0=gx[i][:, 0:512], in1=gt[i][:, 0:512], op=ALU.mult,
                      reads=[r_gx[i], r_gt[i]], writes=[r_u[bb]])
                fw.op('dve', 'scalar_tensor_tensor', out=gv[bb][:], in0=gx[i][:, 512:1024], scalar=1.0, in1=gt[i][:, 512:1024], op0=ALU.mult,
                      op1=ALU.mult, accum_out=acc_ap, reads=[r_gx[i], r_gt[i]], writes=[r_gv[bb], r_acc])

            def part1(t):
                b = t % 2
                bb = t % 3
                tok = slice(t * 128, (t + 1) * 128)
                for k in range(KD):
                    fw.op('pe', 'matmul', out=psU[b][:], lhsT=hT[:, k, tok], rhs=wA[:, k, 0:512], start=(k == 0), stop=(k == KD - 1),
                          reads=[r_wA], writes=[r_psU[b]])
                for k in range(KD):
                    fw.op('pe', 'matmul', out=psV[b][:], lhsT=hT[:, k, tok], rhs=wA[:, k, 512:1024], start=(k == 0), stop=(k == KD - 1),
                          reads=[r_wA], writes=[r_psV[b]])
                s = st[bb]
                gelu2(b, bb, s[:, 0:1], r_st[bb])
                fw.op('act', 'activation', out=sq[bb][:], in_=gv[bb][:], func=AF.Square, accum_out=s[:, 1:2],
                      reads=[r_gv[bb]], writes=[r_sq[bb], r_st[bb]])
                rs = [r_st[bb]]
                fw.op('dve', 'tensor_scalar', out=s[:, 2:3], in0=s[:, 0:1], scalar1=1.0 / 512, scalar2=None, op0=ALU.mult, reads=rs, writes=rs)
                fw.op('dve', 'tensor_tensor', out=s[:, 3:4], in0=s[:, 2:3], in1=s[:, 2:3], op=ALU.mult, reads=rs, writes=rs)
                fw.op('dve', 'scalar_tensor_tensor', out=s[:, 4:5], in0=s[:, 1:2], scalar=1.0 / 512, in1=s[:, 3:4], op0=ALU.mult,
                      op1=ALU.subtract, reads=rs, writes=rs)
                fw.op('act', 'activation', out=s[:, 5:6], in_=s[:, 4:5], func=AF.Ln, bias=epsc[:, 0:1], reads=rs, writes=rs)
                fw.op('act', 'activation', out=s[:, 5:6], in_=s[:, 5:6], func=AF.Exp, scale=-0.5, reads=rs, writes=rs)
                fw.op('dve', 'tensor_scalar', out=vn[bb][:], in0=gv[bb][:], scalar1=s[:, 2:3], scalar2=s[:, 5:6], op0=ALU.subtract,
                      op1=ALU.mult, reads=[r_gv[bb], r_st[bb]], writes=[r_vn[bb]])
                fw.op('pool', 'tensor_tensor', out=vn[bb][:], in0=vn[bb][:], in1=lng[:], op=ALU.mult, reads=[r_vn[bb], r_c], writes=[r_vn[bb]])
                fw.op('pool', 'tensor_tensor', out=vb[bb][:], in0=vn[bb][:], in1=lnb[:], op=ALU.add, reads=[r_vn[bb], r_c], writes=[r_vb[bb]])
            def part2(t):
                bb = t % 3
                for g in range(8):
                    gs = slice(g * 64, (g + 1) * 64)
                    fw.op('pe', 'matmul', out=psM[:, gs], lhsT=wsT[:, g, :], rhs=vb[bb][:, gs], start=True, stop=True,
                          reads=[r_wsT, r_vb[bb]], writes=[r_psM])
                for g in range(8):
                    gs = slice(g * 64, (g + 1) * 64)
                    fw.op('dve', 'scalar_tensor_tensor', out=oa[bb][:, gs], in0=psM[:, gs], scalar=bsT[:, g:g + 1], in1=u[bb][:, gs],
                          op0=ALU.add, op1=ALU.mult, reads=[r_psM, r_c, r_u[bb]], writes=[r_oa[bb]])
                for c in range(4):
                    fw.op('pe', 'transpose', out=pT2[:, c, :], in_=oa[bb][:, c * 128:(c + 1) * 128], identity=ident[:],
                          reads=[r_oa[bb]], writes=[r_pT2])
                blk = t // 4
                ob = blk % 2
                fw.op('act', 'activation', out=oT[ob][:, :, (t % 4) * 128:(t % 4 + 1) * 128], in_=pT2[:], func=AF.Copy,
                      reads=[r_pT2], writes=[r_oT[ob]])
                if t % 4 == 3:
                    fw.dma('sp', OT[0, :, :, blk * 512:(blk + 1) * 512], oT[ob][:], reads=[r_oT[ob]], writes=[r_OT])
            gu = iter(())
            per = 0
            for t in range(NT):
                part1(t)
                for _ in range(per):
                    next(gu, None)
                if t > 1:
                    part2(t - 2)
            part2(NT - 2)
            part2(NT - 1)
            for _ in gu:
                pass
            fw.end()

        def phase_conv(l):
            fw.begin()
            wD = fw.sb("wD", [128, KD, 1024], BF16)
            r_wD = fw.res()
            load_w(wD[:, :, 0:512], w_in[l, :, OFF_CA:OFF_CA + 512], r_wD)
            load_w(wD[:, :, 512:1024], w_in[l, :, OFF_CG:OFF_CG + 512], r_wD)
            cw = fw.sb("cw", [128, 4, TAPS], F32)
            cb = fw.sb("cb", [128, 4], F32)
            cg_ = fw.sb("cg_", [128, 4], F32)
            cbe = fw.sb("cbe", [128, 4], F32)
            r_c = fw.res()
            for c in range(4):
                fw.dma('sp', cw[:, c, :], conv_w[l, :, c * 128:(c + 1) * 128].rearrange("j p -> p j"), writes=[r_c],
                       allow_slow_non_contiguous=True)
            fw.dma('sp', cb[:], conv_b[l].rearrange("(k p) -> p k", p=128), writes=[r_c], allow_slow_non_contiguous=True)
            fw.dma('sp', cg_[:], conv_ln_g[l].rearrange("(k p) -> p k", p=128), writes=[r_c], allow_slow_non_contiguous=True)
            fw.dma('sp', cbe[:], conv_ln_b[l].rearrange("(k p) -> p k", p=128), writes=[r_c], allow_slow_non_contiguous=True)
            PAD = TAPS - 1
            yT = [[fw.sb("yT%d_%d" % (c, i), [128, PAD + 512], BF16) for i in range(2)] for c in range(4)]
            dg = fw.sb("dg", [128, 4, TAPS, 128], BF16)
            r_dg = fw.res()
            for c in range(4):
                for j in range(TAPS):
                    fw.op('dve', 'tensor_scalar', out=dg[:, c, j, :], in0=ident[:], scalar1=cw[:, c, j:j + 1], scalar2=None, op0=ALU.mult,
                          reads=[r_c], writes=[r_dg])
            psK = [fw.ps("psK%d" % i, [128, 512]) for i in range(2)]
            r_psK = [fw.res(True) for _ in range(2)]
            r_yT = [[fw.res() for i in range(2)] for c in range(4)]
            acc = [fw.sb("acc%d" % c, [128, 512], F32) for c in range(4)]
            r_acc = [fw.res() for _ in range(4)]
            psA = [fw.ps("psA%d" % i, [128, 512]) for i in range(2)]
            psG = [fw.ps("psG%d" % i, [128, 512]) for i in range(2)]
            r_psA = [fw.res(True) for _ in range(2)]
            r_psG = [fw.res(True) for _ in range(2)]
            ps1 = fw.ps("ps1", [128, 512])
            ps2 = fw.ps("ps2", [128, 512])
            r_ps1 = fw.res(True)
            r_ps2 = fw.res(True)
            sig = [fw.sb("sig%d" % i, [128, 512], F32) for i in range(2)]
            r_sig = [fw.res() for _ in range(2)]
            sqb = [fw.sb("sqb%d" % i, [128, 512], F32) for i in range(2)]
            r_sqb = [fw.res() for _ in range(2)]
            mean = fw.sb("mean", [128, 512], F32)
            msq = fw.sb("msq", [128, 512], F32)
            rsd = fw.sb("rsd", [128, 512], F32)
            r_m = fw.res()
            tmp = [fw.sb("tmp%d" % i, [128, 512], F32) for i in range(2)]
            r_tmp = [fw.res() for _ in range(2)]
            od = [fw.sb("od%d" % i, [128, 4, 512], BF16) for i in range(2)]
            r_od = [fw.res() for _ in range(2)]
            r_OT = fw.res()
            def unit_proj(n):
                bk, c = divmod(n, 4)
                yb = bk % 2
                b = n % 2
                tok = slice(bk * 512, (bk + 1) * 512)
                y = yT[c][yb]
                ry = r_yT[c][yb]
                if bk == 0:
                    fw.op('pool', 'memset', ap=y[:, 0:PAD], constant=0.0, writes=[ry])
                else:
                    fw.op('pool', 'tensor_copy', out=y[:, 0:PAD], in_=yT[c][1 - yb][:, 512:512 + PAD], reads=[r_yT[c][1 - yb]], writes=[ry])
                for k in range(KD):
                    fw.op('pe', 'matmul', out=psA[b][:], lhsT=wD[:, k, c * 128:(c + 1) * 128], rhs=hT[:, k, tok],
                          start=(k == 0), stop=(k == KD - 1), reads=[r_wD], writes=[r_psA[b]])
                for k in range(KD):
                    fw.op('pe', 'matmul', out=psG[b][:], lhsT=wD[:, k, 512 + c * 128:512 + (c + 1) * 128], rhs=hT[:, k, tok],
                          start=(k == 0), stop=(k == KD - 1), reads=[r_wD], writes=[r_psG[b]])
                fw.op('act', 'activation', out=sig[b][:], in_=psG[b][:], func=AF.Sigmoid, reads=[r_psG[b]], writes=[r_sig[b]])
                fw.op('dve', 'tensor_tensor', out=y[:, PAD:PAD + 512], in0=psA[b][:], in1=sig[b][:],
                      op=ALU.mult, reads=[r_psA[b], r_sig[b]], writes=[ry])

            def unit_taps(n):
                bk, c = divmod(n, 4)
                yb = bk % 2
                b = n % 2
                y = yT[c][yb]
                ry = r_yT[c][yb]
                for j in range(TAPS):
                    fw.op('pe', 'matmul', out=psK[b][:], lhsT=dg[:, c, j, :], rhs=y[:, j:j + 512], start=(j == 0), stop=(j == TAPS - 1),
                          reads=[ry, r_dg], writes=[r_psK[b]])
                fw.op('dve', 'tensor_scalar', out=acc[c][:], in0=psK[b][:], scalar1=cb[:, c:c + 1], scalar2=None, op0=ALU.add,
                      reads=[r_psK[b], r_c], writes=[r_acc[c]])

            def block_ln(bk):
                tok = slice(bk * 512, (bk + 1) * 512)
                for c in range(4):
                    fw.op('pe', 'matmul', out=ps1[:], lhsT=onesf[:], rhs=acc[c][:], start=(c == 0), stop=(c == 3),
                          reads=[r_acc[c]], writes=[r_ps1])
                for c in range(4):
                    sb_ = c % 2
                    fw.op('act', 'activation', out=sqb[sb_][:], in_=acc[c][:], func=AF.Square, reads=[r_acc[c]], writes=[r_sqb[sb_]])
                    fw.op('pe', 'matmul', out=ps2[:], lhsT=onesf[:], rhs=sqb[sb_][:], start=(c == 0), stop=(c == 3),
                          reads=[r_sqb[sb_]], writes=[r_ps2])
                fw.op('dve', 'tensor_scalar', out=mean[:], in0=ps1[:], scalar1=1.0 / 512, scalar2=None, op0=ALU.mult,
                      reads=[r_ps1], writes=[r_m])
                fw.op('dve', 'tensor_tensor', out=msq[:], in0=mean[:], in1=mean[:], op=ALU.mult, reads=[r_m], writes=[r_m])
                fw.op('dve', 'scalar_tensor_tensor', out=rsd[:], in0=ps2[:], scalar=1.0 / 512, in1=msq[:], op0=ALU.mult, op1=ALU.subtract,
                      reads=[r_ps2, r_m], writes=[r_m])
                fw.op('act', 'activation', out=rsd[:], in_=rsd[:], func=AF.Sqrt, bias=epsc[:, 0:1], reads=[r_m], writes=[r_m])
                fw.op('dve', 'reciprocal', out=rsd[:], in_=rsd[:], reads=[r_m], writes=[r_m])
                ob = bk % 2
                for c in range(4):
                    b = c % 2
                    fw.op('dve', 'tensor_tensor', out=tmp[b][:], in0=acc[c][:], in1=mean[:], op=ALU.subtract,
                          reads=[r_acc[c], r_m], writes=[r_tmp[b]])
                    fw.op('dve', 'tensor_tensor', out=tmp[b][:], in0=tmp[b][:], in1=rsd[:], op=ALU.mult,
                          reads=[r_tmp[b], r_m], writes=[r_tmp[b]])
                    fw.op('dve', 'tensor_scalar', out=tmp[b][:], in0=tmp[b][:], scalar1=cg_[:, c:c + 1], scalar2=cbe[:, c:c + 1],
                          op0=ALU.mult, op1=ALU.add, reads=[r_tmp[b], r_c], writes=[r_tmp[b]])
                    fw.op('act', 'activation', out=od[ob][:, c, :], in_=tmp[b][:], func=AF.Silu, reads=[r_tmp[b]], writes=[r_od[ob]])
                fw.dma('sp', OT[3, :, :, tok], od[ob][:], reads=[r_od[ob]], writes=[r_OT])

            NU = NB * 4
            for n in range(NU + 1):
                if n < NU:
                    unit_proj(n)
                if n > 0:
                    unit_taps(n - 1)
                    if (n - 1) % 4 == 3:
                        block_ln((n - 1) // 4)
            fw.end()

        def attention(banks, r_bk, nheads, kd, qT, kT, kv_of, vx, r_q, r_k, r_v, bias_of, r_bias, prev_only, add_sink, dst_idx):
            LA = 2
            psS = [banks[4 + i] for i in range(3)]
            r_psS = [r_bk[4 + i] for i in range(3)]
            psO = [banks[7][:, 0:128], banks[3][:, 0:128]]
            r_psO = [r_bk[7], r_bk[3]]
            pt = [fw.sb("pt%d" % i, [128, 512], BF16) for i in range(4)]
            r_pt = [fw.res() for _ in range(4)]
            rec = [fw.sb("rec%d" % i, [128, 128], F32) for i in range(2)]
            r_rec = [fw.res() for _ in range(2)]
            oTt = fw.sb("oTt", [128, S], BF16)
            r_oTt = fw.res()
            r_OT = fw.res()
            groups = []
            for h in range(nheads):
                for i in range(NT):
                    js = [j for j in ((i - 1, i) if prev_only else range(i + 1)) if j >= 0]
                    chunks = [js[c:c + 4] for c in range(0, len(js), 4)]
                    for ci, ch in enumerate(chunks):
                        groups.append((h, i, ch, ci == 0, ci == len(chunks) - 1))
            pending = []

            def flush_one():
                gi, (h, i, ch, first, last) = pending.pop(0)
                hk = kv_of(h)
                pb = gi % 4
                ob = (h * NT + i) % 2
                half = h % 2
                qs = slice(i * 128, (i + 1) * 128)
                for jj, j in enumerate(ch):
                    fw.op('pe', 'matmul', out=psO[ob], lhsT=vx[:, j, hk, :], rhs=pt[pb][:, jj * 128:(jj + 1) * 128],
                          start=(first and jj == 0), stop=(last and jj == len(ch) - 1), reads=[r_v, r_pt[pb]], writes=[r_psO[ob]])
                if last:
                    if add_sink is not None:
                        fw.op('dve', 'tensor_scalar', out=rec[ob][64:128, :], in0=psO[ob][64:128, :],
                              scalar1=add_sink[0][64:128, add_sink[1] + h:add_sink[1] + h + 1],
                              scalar2=None, op0=ALU.add, reads=[r_psO[ob], r_bias], writes=[r_rec[ob]])
                        fw.op('dve', 'reciprocal', out=rec[ob][64:128, :], in_=rec[ob][64:128, :], reads=[r_rec[ob]], writes=[r_rec[ob]])
                    else:
                        fw.op('dve', 'reciprocal', out=rec[ob][64:128, :], in_=psO[ob][64:128, :], reads=[r_psO[ob]], writes=[r_rec[ob]])
                    fw.op('dve', 'tensor_tensor', out=oTt[half * 64:(half + 1) * 64, qs], in0=psO[ob][0:64, :], in1=rec[ob][64:128, :],
                          op=ALU.mult, reads=[r_psO[ob], r_rec[ob]], writes=[r_oTt])
                    if half == 1 and i == NT - 1:
                        hc = dst_idx[1] + h // 2
                        fw.dma('sp', OT[dst_idx[0], :, hc, :], oTt[:], reads=[r_oTt], writes=[r_OT])

            for gi, g in enumerate(groups):
                h, i, ch, first, last = g
                hk = kv_of(h)
                sb_ = gi % 3
                pb = gi % 4
                qs = slice(i * 128, (i + 1) * 128)
                n = len(ch)
                for jj, j in enumerate(ch):
                    ks = slice(j * 128, (j + 1) * 128)
                    fw.op('pe', 'matmul', out=psS[sb_][:, jj * 128:(jj + 1) * 128], lhsT=kT[0:kd, hk, ks], rhs=qT[0:kd, h, qs],
                          start=True, stop=True, reads=[r_q, r_k], writes=[r_psS[sb_]])
                kw = {}
                rds = [r_psS[sb_]]
                bias = bias_of(i, h)
                if bias is not None:
                    kw['bias'] = bias
                    rds.append(r_bias)
                fw.op('act', 'activation', out=pt[pb][:, 0:n * 128], in_=psS[sb_][:, 0:n * 128], func=AF.Exp, scale=0.125,
                      reads=rds, writes=[r_pt[pb]], **kw)
                if prev_only and n == 2:
                    fw.op('pool', 'tensor_tensor', out=pt[pb][:, 0:256], in0=pt[pb][:, 0:256], in1=mask2[:], op=ALU.mult,
                          reads=[r_pt[pb]], writes=[r_pt[pb]])
                else:
                    for jj, j in enumerate(ch):
                        if j == i:
                            fw.op('pool', 'tensor_tensor', out=pt[pb][:, jj * 128:(jj + 1) * 128], in0=pt[pb][:, jj * 128:(jj + 1) * 128],
                                  in1=mdiag[:], op=ALU.mult, reads=[r_pt[pb]], writes=[r_pt[pb]])
                        elif prev_only:
                            fw.op('pool', 'tensor_tensor', out=pt[pb][:, jj * 128:(jj + 1) * 128], in0=pt[pb][:, jj * 128:(jj + 1) * 128],
                                  in1=mprev[:], op=ALU.mult, reads=[r_pt[pb]], writes=[r_pt[pb]])
                pending.append((gi, g))
                if len(pending) > LA:
                    flush_one()
            while pending:
                flush_one()

        def attention_blk(banks, r_bk, nheads, kd, qT, kT, vx, r_q, r_k, r_v, dst_idx):
            LA = 2
            psS = [banks[4 + i] for i in range(3)]
            r_psS = [r_bk[4 + i] for i in range(3)]
            psO = [banks[7], banks[3]]
            r_psO = [r_bk[7], r_bk[3]]
            pt = [fw.sb("pt%d" % i, [128, 512], BF16) for i in range(4)]
            r_pt = [fw.res() for _ in range(4)]
            rec = [fw.sb("rec%d" % i, [128, 512], F32) for i in range(2)]
            r_rec = [fw.res() for _ in range(2)]
            oTt = fw.sb("oTt", [128, S], BF16)
            r_oTt = fw.res()
            r_OT = fw.res()
            groups = []
            for h in range(nheads):
                for B in range(NB):
                    nj = 4 * B + 4
                    for j in range(nj):
                        groups.append((h, B, j, j == 0, j == nj - 1))
            pending = []

            def geom(B, j):
                if j < 4 * B:
                    return 0, 512
                jr = j - 4 * B
                return jr * 128, (4 - jr) * 128

            def flush_one():
                gi, (h, B, j, first, last) = pending.pop(0)
                pb = gi % 4
                ob = (h * NB + B) % 2
                half = h % 2
                c0, ncols = geom(B, j)
                fw.op('pe', 'matmul', out=psO[ob][:, c0:c0 + ncols], lhsT=vx[:, j, h, :], rhs=pt[pb][:, 0:ncols],
                      start=first, stop=last, reads=[r_v, r_pt[pb]], writes=[r_psO[ob]])
                if last:
                    qb = slice(B * 512, (B + 1) * 512)
                    fw.op('dve', 'reciprocal', out=rec[ob][64:128, :], in_=psO[ob][64:128, :], reads=[r_psO[ob]], writes=[r_rec[ob]])
                    fw.op('dve', 'tensor_tensor', out=oTt[half * 64:(half + 1) * 64, qb], in0=psO[ob][0:64, :], in1=rec[ob][64:128, :],
                          op=ALU.mult, reads=[r_psO[ob], r_rec[ob]], writes=[r_oTt])
                    if half == 1 and B == NB - 1:
                        hc = dst_idx[1] + h // 2
                        fw.dma('sp', OT[dst_idx[0], :, hc, :], oTt[:], reads=[r_oTt], writes=[r_OT])

            for gi, g in enumerate(groups):
                h, B, j, first, last = g
                sb_ = gi % 3
                pb = gi % 4
                c0, ncols = geom(B, j)
                q0 = B * 512 + c0
                ks = slice(j * 128, (j + 1) * 128)
                fw.op('pe', 'matmul', out=psS[sb_][:, 0:ncols], lhsT=kT[0:kd, h, ks], rhs=qT[0:kd, h, q0:q0 + ncols],
                      start=True, stop=True, reads=[r_q, r_k], writes=[r_psS[sb_]])
                if j >= 4 * B:
                    fw.op('dve', 'tensor_tensor', out=psS[sb_][:, 0:128], in0=psS[sb_][:, 0:128], in1=mbias[:], op=ALU.add,
                          reads=[r_psS[sb_]], writes=[r_psS[sb_]])
                fw.op('act', 'activation', out=pt[pb][:, 0:ncols], in_=psS[sb_][:, 0:ncols], func=AF.Exp, scale=0.125,
                      reads=[r_psS[sb_]], writes=[r_pt[pb]])
                pending.append((gi, g))
                if len(pending) > LA:
                    flush_one()
            while pending:
                flush_one()

        def proj_heads(banks, r_bk, dst, w, nh, r_w, r_dst, rope=None, w_rot=None):
            psA = [banks[i][0:64, :] for i in range(2)]
            r_psA = [r_bk[i] for i in range(2)]
            if rope is not None:
                psB = [banks[2][0:64, :], banks[3][0:64, :]]
                r_psB = [r_bk[2], r_bk[3]]
                t1 = [fw.sb("rt1_%d" % i, [64, 512], F32) for i in range(2)]
                t2 = [fw.sb("rt2_%d" % i, [64, 512], F32) for i in range(2)]
                r_t1 = [fw.res() for _ in range(2)]
                r_t2 = [fw.res() for _ in range(2)]
                cos2, sin2, r_cs = rope
            n = 0
            for h in range(nh):
                for bk in range(NB):
                    b = n % 2
                    n += 1
                    tok = slice(bk * 512, (bk + 1) * 512)
                    for k in range(KD):
                        fw.op('pe', 'matmul', out=psA[b], lhsT=w[:, k, h * 64:(h + 1) * 64], rhs=hT[:, k, tok], start=(k == 0),
                              stop=(k == KD - 1), reads=[r_w], writes=[r_psA[b]])
                    if rope is None:
                        fw.op('act', 'activation', out=dst[:, h, tok], in_=psA[b], func=AF.Copy, reads=[r_psA[b]], writes=[r_dst])
                    else:
                        for k in range(KD):
                            fw.op('pe', 'matmul', out=psB[b], lhsT=w_rot[:, k, h * 64:(h + 1) * 64], rhs=hT[:, k, tok], start=(k == 0),
                                  stop=(k == KD - 1), reads=[r_w], writes=[r_psB[b]])
                        fw.op('dve', 'tensor_tensor', out=t1[b][:], in0=psA[b], in1=cos2[:, tok], op=ALU.mult,
                              reads=[r_psA[b], r_cs], writes=[r_t1[b]])
                        fw.op('dve', 'tensor_tensor', out=t2[b][:], in0=psB[b], in1=sin2[:, tok], op=ALU.mult,
                              reads=[r_psB[b], r_cs], writes=[r_t2[b]])
                        fw.op('pool', 'tensor_tensor', out=dst[:, h, tok], in0=t1[b][:], in1=t2[b][:], op=ALU.add,
                              reads=[r_t1[b], r_t2[b]], writes=[r_dst])

        def proj_v(banks, r_bk, vx, w, nh, r_w, r_vx):
            psV = [banks[i][:, 0:nh * 64] for i in range(2)]
            r_psV = [r_bk[i] for i in range(2)]
            fw.op('pool', 'memset', ap=vx[:], constant=1.0, writes=[r_vx])
            for t in range(NT):
                b = t % 2
                tok = slice(t * 128, (t + 1) * 128)
                for k in range(KD):
                    fw.op('pe', 'matmul', out=psV[b], lhsT=hT[:, k, tok], rhs=w[:, k, 0:nh * 64], start=(k == 0), stop=(k == KD - 1),
                          reads=[r_w], writes=[r_psV[b]])
                for h in range(nh):
                    fw.op('act', 'activation', out=vx[:, t, h, 0:64], in_=psV[b][:, h * 64:(h + 1) * 64], func=AF.Copy,
                          reads=[r_psV[b]], writes=[r_vx])

        def load_rot(dst, src_cols, nh, res):
            sv = src_cols.rearrange("(k p) (h two f) -> p k h two f", p=128, two=2, f=32)
            dv = dst.rearrange("p k (h two f) -> p k h two f", two=2, f=32)
            for k in range(KD):
                fw.dma('pool', dv[:, k, :, 0, :], sv[:, k, :, 1, :], writes=[res])
                fw.dma('pool', dv[:, k, :, 1, :], sv[:, k, :, 0, :], writes=[res])

        def phase_rope_tables(seq):
            fw.begin()
            posi = fw.sb("posi", [64, S], I32)
            ang = fw.sb("ang", [64, S], F32)
            ang2 = fw.sb("ang2", [64, S], F32)
            ni = fw.sb("ni", [64, S], I32)
            nf = fw.sb("nf", [64, S], F32)
            invf = fw.sb("invf", [64, 1], F32)
            sgn = fw.sb("sgn", [64, 1], F32)
            r_cs = fw.res()
            fw.dma('sp', posi[:], pos_d[seq:seq + 1, :].partition_broadcast(64), writes=[r_cs])
            fw.op('pool', 'iota', out=invf[0:32, :], pattern=[[0, 1]], base=0, channel_multiplier=1,
                  allow_small_or_imprecise_dtypes=True, writes=[r_cs])
            fw.op('pool', 'iota', out=invf[32:64, :], pattern=[[0, 1]], base=0, channel_multiplier=1,
                  allow_small_or_imprecise_dtypes=True, reads=[r_cs], writes=[r_cs])
            fw.op('act', 'activation', out=invf[:], in_=invf[:], func=AF.Exp, scale=-math.log(10000.0) / 32.0, reads=[r_cs], writes=[r_cs])
            fw.op('dve', 'memset', ap=sgn[0:32, :], constant=-1.0, reads=[r_cs], writes=[r_cs])
            fw.op('dve', 'memset', ap=sgn[32:64, :], constant=1.0, reads=[r_cs], writes=[r_cs])
            fw.op('dve', 'tensor_copy', out=ang[:], in_=posi[:], reads=[r_cs], writes=[r_cs])
            fw.op('dve', 'tensor_scalar', out=ang[:], in0=ang[:], scalar1=invf[:, 0:1], scalar2=None, op0=ALU.mult, reads=[r_cs], writes=[r_cs])

            def sin_of(dst, shift, sign_ap):
                fw.op('dve', 'tensor_scalar', out=ang2[:], in0=ang[:], scalar1=shift, scalar2=None, op0=ALU.add, reads=[r_cs], writes=[r_cs])
                fw.op('dve', 'tensor_scalar', out=nf[:], in0=ang2[:], scalar1=1.0 / TWO_PI, scalar2=None, op0=ALU.mult, reads=[r_cs], writes=[r_cs])
                fw.op('dve', 'tensor_copy', out=ni[:], in_=nf[:], reads=[r_cs], writes=[r_cs])
                fw.op('dve', 'tensor_copy', out=nf[:], in_=ni[:], reads=[r_cs], writes=[r_cs])
                fw.op('dve', 'scalar_tensor_tensor', out=ang2[:], in0=nf[:], scalar=-TWO_PI, in1=ang2[:], op0=ALU.mult, op1=ALU.add,
                      reads=[r_cs], writes=[r_cs])
                fw.op('dve', 'tensor_scalar', out=nf[:], in0=ang2[:], scalar1=math.pi, scalar2=-TWO_PI, op0=ALU.is_gt, op1=ALU.mult,
                      reads=[r_cs], writes=[r_cs])
                fw.op('dve', 'tensor_tensor', out=ang2[:], in0=ang2[:], in1=nf[:], op=ALU.add, reads=[r_cs], writes=[r_cs])
                fw.op('dve', 'tensor_scalar', out=nf[:], in0=ang2[:], scalar1=-math.pi, scalar2=TWO_PI, op0=ALU.is_lt, op1=ALU.mult,
                      reads=[r_cs], writes=[r_cs])
                fw.op('dve', 'tensor_tensor', out=ang2[:], in0=ang2[:], in1=nf[:], op=ALU.add, reads=[r_cs], writes=[r_cs])
                fw.op('act', 'activation', out=dst[:], in_=ang2[:], func=AF.Sin, reads=[r_cs], writes=[r_cs])
                if sign_ap is not None:
                    fw.op('dve', 'tensor_scalar', out=dst[:], in0=dst[:], scalar1=sign_ap, scalar2=None, op0=ALU.mult, reads=[r_cs], writes=[r_cs])

            cos2 = fw.sb("cos2", [64, S], F32)
            sin2 = fw.sb("sin2", [64, S], F32)
            sin_of(cos2, math.pi / 2.0, None)
            sin_of(sin2, 0.0, sgn[:, 0:1])
            r_CS = fw.res()
            fw.dma('sp', CS[0], cos2[:], reads=[r_cs], writes=[r_CS])
            fw.dma('sp', CS[1], sin2[:], reads=[r_cs], writes=[r_CS])
            fw.end()

        def rot_copy(dst, src_t, r_w):
            sv = src_t.rearrange("p k (h two f) -> p (k h) two f", two=2, f=32)
            dv = dst.rearrange("p k (h two f) -> p (k h) two f", two=2, f=32)
            fw.op('pool', 'tensor_copy', out=dv[:, :, 0, :], in_=sv[:, :, 1, :], reads=[r_w], writes=[r_w])
            fw.op('pool', 'tensor_copy', out=dv[:, :, 1, :], in_=sv[:, :, 0, :], reads=[r_w], writes=[r_w])

        def phase_swa(l, grp):
            fw.begin()
            banks = [fw.ps("bk%d" % i, [128, 512]) for i in range(8)]
            r_bk = [fw.res(True) for _ in range(8)]
            wq = fw.sb("wq", [128, KD, 256], BF16)
            wqr = fw.sb("wqr", [128, KD, 256], BF16)
            wk = fw.sb("wk", [128, KD, 64], BF16)
            wkr = fw.sb("wkr", [128, KD, 64], BF16)
            wv = fw.sb("wv", [128, KD, 64], BF16)
            r_wq = fw.res()
            r_wk = fw.res()
            r_wv = fw.res()
            q0 = OFF_SQ + grp * 256
            k0 = OFF_SK + grp * 64
            v0 = OFF_SV + grp * 64
            load_w(wq[:], w_in[l, :, q0:q0 + 256], r_wq)
            load_w(wk[:], w_in[l, :, k0:k0 + 64], r_wk)
            load_w(wv[:], w_in[l, :, v0:v0 + 64], r_wv)
            rot_copy(wqr[:], wq[:], r_wq)
            rot_copy(wkr[:], wk[:], r_wk)
            cos2 = fw.sb("cos2", [64, S], F32)
            sin2 = fw.sb("sin2", [64, S], F32)
            esk = fw.sb("esk", [128, 8], F32)
            r_cs = fw.res()
            r_es = fw.res()
            fw.dma('sp', cos2[:], CS[0], writes=[r_cs])
            fw.dma('sp', sin2[:], CS[1], writes=[r_cs])
            fw.dma('sp', esk[:], swa_sink[l:l + 1, :].partition_broadcast(128), writes=[r_es])
            fw.op('act', 'activation', out=esk[:], in_=esk[:], func=AF.Exp, reads=[r_es], writes=[r_es])
            qT = fw.sb("qT", [64, 4, S], BF16)
            kT = fw.sb("kT", [64, 1, S], BF16)
            vx = fw.sb("vx", [128, NT, 1, 128], BF16)
            r_q = fw.res()
            r_k = fw.res()
            r_v = fw.res()
            proj_heads(banks, r_bk, kT, wk, 1, r_wk, r_k, rope=(cos2, sin2, r_cs), w_rot=wkr)
            proj_v(banks, r_bk, vx, wv, 1, r_wv, r_v)
            proj_heads(banks, r_bk, qT, wq, 4, r_wq, r_q, rope=(cos2, sin2, r_cs), w_rot=wqr)
            attention(banks, r_bk, 4, 64, qT, kT, lambda h: 0, vx, r_q, r_k, r_v, lambda i, h: None, r_es, True, (esk, grp * 4), (1, grp * 2))
            fw.end()

        def phase_fox_cum(l):
            fw.begin()
            wff = fw.sb("wff", [128, KD, 8], BF16)
            r_w = fw.res()
            load_w(wff[:], w_in[l, :, OFF_FF:OFF_FF + 8], r_w)
            bfb = fw.sb("bfb", [128, 8], F32)
            r_c = fw.res()
            fw.dma('sp', bfb[:], fox_bf[l:l + 1, :].partition_broadcast(128), writes=[r_c])
            psF = [fw.ps("psF%d" % i, [128, 8]) for i in range(2)]
            r_psF = [fw.res(True) for _ in range(2)]
            psC = [fw.ps("psC%d" % i, [128, 8]) for i in range(2)]
            r_psC = [fw.res(True) for _ in range(2)]
            psT = [fw.ps("psT%d" % i, [128, 8]) for i in range(2)]
            r_psT = [fw.res(True) for _ in range(2)]
            nls = fw.sb("nls", [128, NT, 8], F32)
            r_nls = [fw.res() for _ in range(NT)]
            r_cum = [fw.res() for _ in range(NT)]
            for t in range(NT):
                b = t % 2
                tok = slice(t * 128, (t + 1) * 128)
                for k in range(KD):
                    fw.op('pe', 'matmul', out=psF[b][:], lhsT=hT[:, k, tok], rhs=wff[:, k, :], start=(k == 0), stop=(k == KD - 1),
                          reads=[r_w], writes=[r_psF[b]])
                fw.op('dve', 'tensor_tensor', out=nls[:, t, :], in0=psF[b][:], in1=bfb[:], op=ALU.add, reads=[r_psF[b], r_c], writes=[r_nls[t]])
                fw.op('act', 'activation', out=nls[:, t, :], in_=nls[:, t, :], func=AF.Exp, scale=-1.0, reads=[r_nls[t]], writes=[r_nls[t]])
                fw.op('dve', 'tensor_scalar', out=nls[:, t, :], in0=nls[:, t, :], scalar1=1.0, scalar2=None, op0=ALU.add,
                      reads=[r_nls[t]], writes=[r_nls[t]])
                fw.op('act', 'activation', out=nls[:, t, :], in_=nls[:, t, :], func=AF.Ln, reads=[r_nls[t]], writes=[r_nls[t]])
                fw.op('pe', 'matmul', out=psC[b][:], lhsT=triu[:], rhs=nls[:, t, :], start=True, stop=True, reads=[r_nls[t]], writes=[r_psC[b]])
                fw.op('pe', 'matmul', out=psT[b][:], lhsT=onesf[:], rhs=nls[:, t, :], start=True, stop=True, reads=[r_nls[t]], writes=[r_psT[b]])
                if t == 0:
                    fw.op('dve', 'tensor_copy', out=ncum[:, t, :], in_=psC[b][:], reads=[r_psC[b]], writes=[r_cum[t]])
                    fw.op('dve', 'tensor_copy', out=tot[:, t, :], in_=psT[b][:], reads=[r_psT[b]], writes=[r_cum[t]])
                else:
                    fw.op('dve', 'tensor_tensor', out=ncum[:, t, :], in0=psC[b][:], in1=tot[:, t - 1, :], op=ALU.add,
                          reads=[r_psC[b], r_cum[t - 1]], writes=[r_cum[t]])
                    fw.op('dve', 'tensor_tensor', out=tot[:, t, :], in0=psT[b][:], in1=tot[:, t - 1, :], op=ALU.add,
                          reads=[r_psT[b], r_cum[t - 1]], writes=[r_cum[t]])
            r1 = fw.sb("r1", [128, NT, 8], F32)
            n8 = fw.sb("n8", [128, NT, 8], F32)
            r_h = fw.res()
            rr = [r_cum[NT - 1], r_h]
            fw.op('dve', 'memset', ap=KML[:, :, :, 3:6], constant=1.0, writes=[r_h])
            fw.op('dve', 'memset', ap=QML[:, :, :, 0:3], constant=8.0, reads=[r_h], writes=[r_h])
            fw.op('dve', 'tensor_copy', out=KML[:, :, :, 0], in_=ncum[:], reads=rr, writes=[r_h])
            fw.op('dve', 'tensor_tensor', out=r1[:], in0=ncum[:], in1=KML[:, :, :, 0], op=ALU.subtract, reads=rr, writes=[r_h])
            fw.op('dve', 'tensor_copy', out=KML[:, :, :, 1], in_=r1[:], reads=rr, writes=[r_h])
            fw.op('dve', 'tensor_tensor', out=r1[:], in0=r1[:], in1=KML[:, :, :, 1], op=ALU.subtract, reads=rr, writes=[r_h])
            fw.op('dve', 'tensor_copy', out=KML[:, :, :, 2], in_=r1[:], reads=rr, writes=[r_h])
            fw.op('dve', 'tensor_scalar', out=n8[:], in0=ncum[:], scalar1=-8.0, scalar2=None, op0=ALU.mult, reads=rr, writes=[r_h])
            fw.op('dve', 'tensor_copy', out=QML[:, :, :, 3], in_=n8[:], reads=rr, writes=[r_h])
            fw.op('dve', 'tensor_tensor', out=r1[:], in0=n8[:], in1=QML[:, :, :, 3], op=ALU.subtract, reads=rr, writes=[r_h])
            fw.op('dve', 'tensor_copy', out=QML[:, :, :, 4], in_=r1[:], reads=rr, writes=[r_h])
            fw.op('dve', 'tensor_tensor', out=r1[:], in0=r1[:], in1=QML[:, :, :, 4], op=ALU.subtract, reads=rr, writes=[r_h])
            fw.op('dve', 'tensor_copy', out=QML[:, :, :, 5], in_=r1[:], reads=rr, writes=[r_h])
            fw.end()

        def phase_fox(l, grp):
            h0 = grp * 4
            fw.begin()
            banks = [fw.ps("bk%d" % i, [128, 512]) for i in range(8)]
            r_bk = [fw.res(True) for _ in range(8)]
            wfq = fw.sb("wfq", [128, KD, 256], BF16)
            wfk = fw.sb("wfk", [128, KD, 256], BF16)
            wfv = fw.sb("wfv", [128, KD, 256], BF16)
            r_wq = fw.res()
            r_wk = fw.res()
            r_wv = fw.res()
            load_w(wfk[:], w_in[l, :, OFF_FK + h0 * 64:OFF_FK + h0 * 64 + 256], r_wk)
            load_w(wfv[:], w_in[l, :, OFF_FV + h0 * 64:OFF_FV + h0 * 64 + 256], r_wv)
            load_w(wfq[:], w_in[l, :, OFF_FQ + h0 * 64:OFF_FQ + h0 * 64 + 256], r_wq)
            qT = fw.sb("qT", [128, 4, S], BF16)
            kT = fw.sb("kT", [128, 4, S], BF16)
            vx = fw.sb("vx", [128, NT, 4, 128], BF16)
            r_q = fw.res()
            r_k = fw.res()
            r_ka = fw.res()
            r_v = fw.res()
            r_qa = fw.res()
            n = 0
            for (ML, dstT, r_a) in ((KML, kT, r_ka), (QML, qT, r_qa)):
                for h in range(4):
                    for tb in range(NB):
                        bb = 4 + n % 3
                        n += 1
                        for tt in range(4):
                            t = tb * 4 + tt
                            fw.op('pe', 'matmul', out=banks[bb][0:6, tt * 128:(tt + 1) * 128], lhsT=ML[:, t, h0 + h, :], rhs=ident[:],
                                  start=True, stop=True, writes=[r_bk[bb]])
                        fw.op('dve', 'tensor_copy', out=dstT[64:70, h, tb * 512:(tb + 1) * 512], in_=banks[bb][0:6, :],
                              reads=[r_bk[bb]], writes=[r_a])
            proj_heads(banks, r_bk, kT[0:64], wfk, 4, r_wk, r_k)
            proj_v(banks, r_bk, vx, wfv, 4, r_wv, r_v)
            proj_heads(banks, r_bk, qT[0:64], wfq, 4, r_wq, r_q)

            r_kk = fw.res()
            r_qq = fw.res()
            fw.op('pool', 'memset', ap=rec_dummy[:], constant=0.0, reads=[r_k, r_ka, r_q, r_qa], writes=[r_kk, r_qq])
            attention_blk(banks, r_bk, 4, 70, qT, kT, vx, r_qq, r_kk, r_v, (2, grp * 2))
            fw.end()

        def phase_gates(l):
            fw.begin()
            wG = [fw.sb("wG%d" % i, [128, KD, 512], BF16) for i in range(2)]
            r_wG = [fw.res() for _ in range(2)]
            psZ = [fw.ps("psZ%d" % i, [128, 512]) for i in range(4)]
            r_psZ = [fw.res(True) for _ in range(4)]
            sg = [fw.sb("sg%d" % i, [128, 512], BF16) for i in range(4)]
            r_sg = [fw.res() for _ in range(4)]
            r_SG = fw.res()
            n = 0
            for c in range(8):
                wb = c % 2
                load_w(wG[wb][:], w_in[l, :, OFF_G + c * 512:OFF_G + (c + 1) * 512], r_wG[wb])
                for t in range(NT):
                    b = n % 4
                    n += 1
                    tok = slice(t * 128, (t + 1) * 128)
                    for k in range(KD):
                        fw.op('pe', 'matmul', out=psZ[b][:], lhsT=hT[:, k, tok], rhs=wG[wb][:, k, :], start=(k == 0), stop=(k == KD - 1),
                              reads=[r_wG[wb]], writes=[r_psZ[b]])
                    fw.op('act', 'activation', out=sg[b][:], in_=psZ[b][:], func=AF.Sigmoid, reads=[r_psZ[b]], writes=[r_sg[b]])
                    fw.dma('sp', SG[tok, c * 512:(c + 1) * 512], sg[b][:], reads=[r_sg[b]], writes=[r_SG])
            fw.end()

        def phase_merge(l):
            fw.begin()
            wbr = fw.sb("wbr", [128, 16, D], BF16)
            wo = fw.sb("wo", [128, KD, D], BF16)
            r_w = fw.res()
            for n_ in range(4):
                fw.dma('pool', wbr[:, n_ * 4:(n_ + 1) * 4, :], w_branch[l, n_].rearrange("(k p) d -> p k d", p=128), writes=[r_w])
            load_w(wo[:], w_out[l], r_w)
            sgt = [fw.sb("sgt%d" % i, [128, 4096], BF16) for i in range(2)]
            r_sgt = [fw.res() for _ in range(2)]
            ot = [fw.sb("ot%d" % i, [128, 16, 128], BF16) for i in range(2)]
            r_ot = [fw.res() for _ in range(2)]
            psP = [fw.ps("psP%d" % i, [128, 512]) for i in range(3)]
            r_psP = [fw.res(True) for _ in range(3)]
            pT = fw.ps("pTm", [128, KD, 128], BF16)
            r_pT = fw.res(True)
            psY = [fw.ps("psY%d" % i, [128, 512]) for i in range(2)]
            r_psY = [fw.res(True) for _ in range(2)]
            mg = [fw.sb("mg%d" % i, [128, D], F32) for i in range(2)]
            r_mg = [fw.res() for _ in range(2)]
            tm = [fw.sb("tm%d" % i, [128, 512], F32) for i in range(2)]
            r_tm = [fw.res() for _ in range(2)]
            mb = [fw.sb("mb%d" % i, [128, D], BF16) for i in range(2)]
            r_mb = [fw.res() for _ in range(2)]
            mT = [fw.sb("mT%d" % i, [128, KD, 128], BF16) for i in range(2)]
            r_mT = [fw.res() for _ in range(2)]
            cnt = {'np': 0}

            def stageA(t):
                np_ = cnt['np']
                b = t % 2
                tok = slice(t * 128, (t + 1) * 128)
                fw.dma('sp', sgt[b][:], SG[tok, :], writes=[r_sgt[b]])
                for n_ in range(4):
                    fw.dma('sp', ot[b][:, n_ * 4:(n_ + 1) * 4, :], OT[n_, :, :, tok], writes=[r_ot[b]])
                for half in range(2):
                    hs = slice(half * 512, (half + 1) * 512)
                    for n_ in range(4):
                        pb = np_ % 3
                        np_ += 1
                        for k in range(4):
                            fw.op('pe', 'matmul', out=psP[pb][:], lhsT=ot[b][:, n_ * 4 + k, :], rhs=wbr[:, n_ * 4 + k, hs], start=(k == 0),
                                  stop=(k == 3), reads=[r_ot[b], r_w], writes=[r_psP[pb]])
                        gsl = slice(n_ * 1024 + half * 512, n_ * 1024 + (half + 1) * 512)
                        if n_ == 0:
                            fw.op('dve', 'tensor_tensor', out=mg[b][:, hs], in0=psP[pb][:], in1=sgt[b][:, gsl], op=ALU.mult,
                                  reads=[r_psP[pb], r_sgt[b]], writes=[r_mg[b]])
                        else:
                            tb = np_ % 2
                            fw.op('dve', 'tensor_tensor', out=tm[tb][:], in0=psP[pb][:], in1=sgt[b][:, gsl], op=ALU.mult,
                                  reads=[r_psP[pb], r_sgt[b]], writes=[r_tm[tb]])
                            if n_ < 3:
                                fw.op('dve', 'tensor_tensor', out=mg[b][:, hs], in0=mg[b][:, hs], in1=tm[tb][:], op=ALU.add,
                                      reads=[r_tm[tb], r_mg[b]], writes=[r_mg[b]])
                            else:
                                fw.op('dve', 'tensor_tensor', out=mb[b][:, hs], in0=mg[b][:, hs], in1=tm[tb][:], op=ALU.add,
                                      reads=[r_tm[tb], r_mg[b]], writes=[r_mb[b]])
                cnt['np'] = np_

            def stageB(t):
                b = t % 2
                for k in range(KD):
                    fw.op('pe', 'transpose', out=pT[:, k, :], in_=mb[b][:, k * 128:(k + 1) * 128], identity=ident[:],
                          reads=[r_mb[b]], writes=[r_pT])
                fw.op('act', 'activation', out=mT[b][:], in_=pT[:], func=AF.Copy, reads=[r_pT], writes=[r_mT[b]])
                for half in range(2):
                    hs = slice(half * 512, (half + 1) * 512)
                    for k in range(KD):
                        fw.op('pe', 'matmul', out=psY[half][:], lhsT=mT[b][:, k, :], rhs=wo[:, k, hs], start=(k == 0), stop=(k == KD - 1),
                              reads=[r_mT[b], r_w], writes=[r_psY[half]])
                    fw.op('dve', 'tensor_tensor', out=xs[:, t, hs], in0=psY[half][:], in1=xs[:, t, hs], op=ALU.add,
                          reads=[r_psY[half], r_xs[t]], writes=[r_xs[t]])
            stageA(0)
            for t in range(1, NT):
                stageA(t)
                stageB(t - 1)
            stageB(NT - 1)
            fw.end()

        def phase_ffn(wg_d, wu_d, wd_d, ne, gated):
            fw.begin()
            wg = [fw.sb("wg%d" % i, [128, KD, 512], BF16) for i in range(2)]
            wu = [fw.sb("wu%d" % i, [128, KD, 512], BF16) for i in range(2)]
            wd = [fw.sb("wd%d" % i, [128, 4, D], BF16) for i in range(2)]
            r_wgt = [fw.res() for _ in range(2)]
            psG = [fw.ps("fpG%d" % i, [128, 512]) for i in range(2)]
            psU = [fw.ps("fpU%d" % i, [128, 512]) for i in range(2)]
            psY = [fw.ps("fpY%d" % i, [128, 512]) for i in range(3)]
            r_psG = [fw.res(True) for _ in range(2)]
            r_psU = [fw.res(True) for _ in range(2)]
            r_psY = [fw.res(True) for _ in range(3)]
            sl = [fw.sb("sl%d" % i, [128, 512], F32) for i in range(2)]
            r_sl = [fw.res() for _ in range(2)]
            act = [fw.sb("act%d" % i, [128, 4, 512], BF16) for i in range(2)]
            r_act = [fw.res() for _ in range(2)]
            nw = 0
            nc_ = 0
            nb_ = 0
            ny = 0
            for e in range(ne):
                for r in range(NR):
                    w = nw % 2
                    nw += 1
                    fs = slice(r * 512, (r + 1) * 512)
                    load_w(wg[w][:], wg_d[e, :, fs], r_wgt[w])
                    load_w(wu[w][:], wu_d[e, :, fs], r_wgt[w])
                    fw.dma('pool', wd[w][:], wd_d[e, fs, :].rearrange("(k p) d -> p k d", p=128), writes=[r_wgt[w]])
                    for bk in range(NB):
                        ab = nb_ % 2
                        nb_ += 1
                        tokb = slice(bk * 512, (bk + 1) * 512)
                        for c in range(4):
                            pb = nc_ % 2
                            nc_ += 1
                            cs = slice(c * 128, (c + 1) * 128)
                            for k in range(KD):
                                fw.op('pe', 'matmul', out=psG[pb][:], lhsT=wg[w][:, k, cs], rhs=hT[:, k, tokb], start=(k == 0), stop=(k == KD - 1),
                                      reads=[r_wgt[w]], writes=[r_psG[pb]])
                            for k in range(KD):
                                fw.op('pe', 'matmul', out=psU[pb][:], lhsT=wu[w][:, k, cs], rhs=hT[:, k, tokb], start=(k == 0), stop=(k == KD - 1),
                                      reads=[r_wgt[w]], writes=[r_psU[pb]])
                            fw.op('act', 'activation', out=sl[pb][:], in_=psG[pb][:], func=AF.Silu, reads=[r_psG[pb]], writes=[r_sl[pb]])
                            fw.op('dve', 'tensor_tensor', out=act[ab][:, c, :], in0=psU[pb][:], in1=sl[pb][:], op=ALU.mult,
                                  reads=[r_psU[pb], r_sl[pb]], writes=[r_act[ab]])
                        for tt in range(4):
                            t = bk * 4 + tt
                            for half in range(2):
                                yb = ny % 3
                                ny += 1
                                hs = slice(half * 512, (half + 1) * 512)
                                for c in range(4):
                                    fw.op('pe', 'matmul', out=psY[yb][:], lhsT=act[ab][:, c, tt * 128:(tt + 1) * 128], rhs=wd[w][:, c, hs],
                                          start=(c == 0), stop=(c == 3), reads=[r_act[ab], r_wgt[w]], writes=[r_psY[yb]])
                                if gated:
                                    fw.op('dve', 'scalar_tensor_tensor', out=xs[:, t, hs], in0=psY[yb][:], scalar=Gt[:, t * 8 + e:t * 8 + e + 1], in1=xs[:, t, hs],
                                          op0=ALU.mult, op1=ALU.add, reads=[r_psY[yb], r_xs[t]], writes=[r_xs[t]])
                                else:
                                    fw.op('dve', 'tensor_tensor', out=xs[:, t, hs], in0=psY[yb][:], in1=xs[:, t, hs], op=ALU.add,
                                          reads=[r_psY[yb], r_xs[t]], writes=[r_xs[t]])
            fw.end()

        def phase_stash(seq):
            fw.begin()
            for t in range(NT):
                fw.dma(('sp', 'act')[t % 2], XS[seq, t * 128:(t + 1) * 128, :], xs[:, t, :], writes=[fw.res()])
            fw.end()

        def phase_route():
            fw.begin()
            X_ = mybir.AxisListType.X
            W8 = NTT * 8
            v3 = lambda ap: ap.rearrange("p (t e) -> p t e", e=8)
            Mb = fw.sb("Mb", [128, W8], BF16)
            onesb = fw.sb("onesb", [128, 128], BF16)
            r_c = fw.res()
            fw.op('pool', 'memset', ap=onesb[:], constant=1.0, writes=[r_c])
            r_M = fw.res()
            fw.op('dve', 'tensor_copy', out=Mb[:], in_=Mf[:], writes=[r_M])
            ps_rank = fw.ps("ps_rank", [128, W8])
            ps_tot = fw.ps("ps_tot", [128, W8])
            r_pr = fw.res(True)
            r_pt = fw.res(True)
            fw.op('pe', 'matmul', out=ps_rank[:], lhsT=mdiag[:], rhs=Mb[:], start=True, stop=True, reads=[r_M], writes=[r_pr])
            fw.op('pe', 'matmul', out=ps_tot[:], lhsT=onesb[:], rhs=Mb[:], start=True, stop=True, reads=[r_M, r_c], writes=[r_pt])
            tot_sb = fw.sb("tot_sb", [128, W8], F32)
            tp = fw.sb("tp", [128, W8 + 8], F32)
            r_t = fw.res()
            D_ = lambda name, **kw: fw.op('dve', name, reads=[r_t], writes=[r_t], **kw)
            fw.op('dve', 'tensor_copy', out=tot_sb[:], in_=ps_tot[:], reads=[r_pt], writes=[r_t])
            D_('memset', ap=tp[:, 0:8], constant=0.0)
            for tt in range(NTT):
                D_('tensor_tensor', out=tp[:, (tt + 1) * 8:(tt + 2) * 8], in0=tp[:, tt * 8:(tt + 1) * 8],
                   in1=tot_sb[:, tt * 8:(tt + 1) * 8], op=ALU.add)
            cnt = tp[:, W8:W8 + 8]
            nb = fw.sb("nb", [128, 8], F32)
            tmp8 = fw.sb("tmp8", [128, 8], F32)
            D_('memset', ap=nb[:], constant=0.0)
            for j in range(JMAX):
                D_('tensor_scalar', out=tmp8[:], in0=cnt, scalar1=float(j * BS), scalar2=None, op0=ALU.is_gt)
                D_('tensor_tensor', out=nb[:], in0=nb[:], in1=tmp8[:], op=ALU.add)
            bend = fw.sb("bend", [128, 8], F32)
            off = fw.sb("off", [128, 8], F32)
            D_('tensor_copy', out=bend[:, 0:1], in_=nb[:, 0:1])
            for e in range(1, 8):
                D_('tensor_tensor', out=bend[:, e:e + 1], in0=bend[:, e - 1:e], in1=nb[:, e:e + 1], op=ALU.add)
            D_('tensor_tensor', out=off[:], in0=bend[:], in1=nb[:], op=ALU.subtract)
            D_('tensor_scalar', out=off[:], in0=off[:], scalar1=float(BS), scalar2=None, op0=ALU.mult)
            slot = fw.sb("slot", [128, W8], F32)
            slotM = fw.sb("slotM", [128, W8], F32)
            valA = fw.sb("valA", [128, W8], F32)
            fw.op('dve', 'tensor_tensor', out=slot[:], in0=ps_rank[:], in1=Mf[:], op=ALU.subtract, reads=[r_pr, r_t], writes=[r_t])
            D_('tensor_tensor', out=slot[:], in0=slot[:], in1=tp[:, 0:W8], op=ALU.add)
            for tt in range(NTT):
                D_('tensor_tensor', out=slot[:, tt * 8:(tt + 1) * 8], in0=slot[:, tt * 8:(tt + 1) * 8], in1=off[:], op=ALU.add)
            D_('tensor_tensor', out=slotM[:], in0=slot[:], in1=Mf[:], op=ALU.mult)
            D_('tensor_scalar', out=valA[:], in0=Mf[:], scalar1=-1.0e6, scalar2=1.0e6, op0=ALU.mult, op1=ALU.add)
            D_('tensor_tensor', out=valA[:], in0=valA[:], in1=slot[:], op=ALU.add)
            sAf = fw.sb("sAf", [128, NTT], F32)
            sBf = fw.sb("sBf", [128, NTT], F32)
            eq = fw.sb("eq", [128, NTT], F32)
            D_('tensor_reduce', out=sAf[:], in_=v3(valA[:]), axis=X_, op=ALU.min)
            D_('tensor_reduce', out=sBf[:], in_=v3(slotM[:]), axis=X_, op=ALU.max)
            D_('tensor_copy', out=sAi[:], in_=sAf[:])
            D_('tensor_copy', out=sBi[:], in_=sBf[:])
            D_('tensor_reduce', out=gA[:], in_=v3(Gt[:]), axis=X_, op=ALU.add)
            D_('memset', ap=gB[:], constant=0.0)
            for e in range(NE):
                D_('tensor_tensor', out=eq[:], in0=v3(slotM[:])[:, :, e], in1=sBf[:], op=ALU.is_equal)
                D_('tensor_tensor', out=eq[:], in0=eq[:], in1=v3(Gt[:])[:, :, e], op=ALU.mult)
                D_('tensor_tensor', out=gB[:], in0=gB[:], in1=eq[:], op=ALU.add)
            D_('tensor_tensor', out=gA[:], in0=gA[:], in1=gB[:], op=ALU.subtract)
            jidx = fw.sb("jidx", [128, NBLK], F32)
            ej = fw.sb("ej", [128, NBLK], F32)
            tmpj = fw.sb("tmpj", [128, NBLK], F32)
            base14 = fw.sb("base14", [128, NR2], F32)
            idxf = fw.sb("idxf", [128, NBLK * NR2], F32)
            r_i = fw.res()
            fw.op('pool', 'iota', out=jidx[:], pattern=[[1, NBLK]], base=0, channel_multiplier=0,
                  allow_small_or_imprecise_dtypes=True, writes=[r_i])
            fw.op('pool', 'iota', out=base14[:], pattern=[[1, NR2]], base=0, channel_multiplier=NR2,
                  allow_small_or_imprecise_dtypes=True, writes=[r_i])
            D_('memset', ap=ej[:], constant=0.0)
            for e in range(NE):
                fw.op('dve', 'tensor_scalar', out=tmpj[:], in0=jidx[:], scalar1=bend[:, e:e + 1], scalar2=None, op0=ALU.is_ge,
                      reads=[r_t, r_i], writes=[r_t])
                D_('tensor_tensor', out=ej[:], in0=ej[:], in1=tmpj[:], op=ALU.add)
            D_('tensor_scalar', out=ej[:], in0=ej[:], scalar1=float(NE - 1), scalar2=float(128 * NR2), op0=ALU.min, op1=ALU.mult)
            for j in range(NBLK):
                fw.op('dve', 'tensor_scalar', out=idxf[:, j * NR2:(j + 1) * NR2], in0=base14[:], scalar1=ej[:, j:j + 1], scalar2=None,
                      op0=ALU.add, reads=[r_t, r_i], writes=[r_t])
            D_('tensor_copy', out=idxw[:], in_=idxf[:])
            fw.end()

        def phase_scatter():
            fw.begin()
            r_z = fw.res()
            fw.op('pool', 'memset', ap=hT[:], constant=0.0, writes=[r_z])
            rows_pp = NBLK * BS // 128
            hs_v = HS.rearrange("(p n) d -> p n d", p=128)
            zrows = (KD * S) // D
            hT_v = hT[:].rearrange("p k s -> p (k s)").rearrange("p (a d) -> p a d", d=D)
            n0 = 0
            qi = 0
            r_HS = fw.res()
            while n0 < rows_pp:
                n1 = min(rows_pp, n0 + zrows)
                fw.dma(('sp', 'act')[qi % 2], hs_v[:, n0:n1, :], hT_v[:, 0:n1 - n0, :], reads=[r_z], writes=[r_HS])
                qi += 1
                n0 = n1
            hb = [fw.sb("hb%d" % i, [128, D], BF16) for i in range(8)]
            r_hb = [fw.res() for _ in range(8)]
            for tt in range(NTT):
                b = tt % 8
                fw.dma('sp', hb[b][:], HTK[tt * 128:(tt + 1) * 128, :], writes=[r_hb[b]])
                for sI in (sAi, sBi):
                    fw.dma('pool', HS[:, :], hb[b][:], reads=[r_hb[b], r_HS], writes=[fw.res()], _meth='indirect_dma_start',
                           out_offset=IOA(ap=sI[:, tt:tt + 1], axis=0), in_offset=None)
            fw.end()

        def phase_moe_sparse():
            fw.begin()
            NWB = 3
            wg = [fw.sb("wg%d" % i, [128, KD, 512], BF16) for i in range(NWB)]
            wu = [fw.sb("wu%d" % i, [128, KD, 512], BF16) for i in range(NWB)]
            wd = [fw.sb("wd%d" % i, [128, 4, D], BF16) for i in range(NWB)]
            r_wgt = [fw.res() for _ in range(NWB)]
            r_wdn = [fw.res() for _ in range(NWB)]
            psG = [fw.ps("fpG%d" % i, [128, 512]) for i in range(2)]
            psU = [fw.ps("fpU%d" % i, [128, 512]) for i in range(2)]
            psY = [fw.ps("fpY%d" % i, [128, 512]) for i in range(3)]
            pT = fw.ps("spT", [128, KD, 128], BF16)
            r_psG = [fw.res(True) for _ in range(2)]
            r_psU = [fw.res(True) for _ in range(2)]
            r_psY = [fw.res(True) for _ in range(3)]
            r_pT = fw.res(True)
            sl = [fw.sb("sl%d" % i, [128, 512], F32) for i in range(2)]
            r_sl = [fw.res() for _ in range(2)]
            act = [fw.sb("act%d" % i, [128, 4, 512], BF16) for i in range(2)]
            r_act = [fw.res() for _ in range(2)]
            hsb = [fw.sb("hsb%d" % i, [128, D], BF16) for i in range(SB)]
            r_hsb = [fw.res() for _ in range(SB)]
            r_hTs = [fw.res() for _ in range(2)]
            r_ys = [[fw.res() for _ in range(SB)] for _ in range(2)]
            nw = 0
            nc_ = 0
            nb_ = 0
            ny = 0
            nh = 0
            NTB = BS // TBW
            TPB = TBW // 128

            def prep_load(j):
                for st in range(SB):
                    fw.dma(('sp', 'act')[st % 2], hsb[st][:], HS[j * BS + st * 128:j * BS + (st + 1) * 128, :], writes=[r_hsb[st]])

            def prep_tr(j):
                c0_ = (j % 2) * BS
                for st in range(SB):
                    for k in range(KD):
                        fw.op('pe', 'transpose', out=pT[:, k, :], in_=hsb[st][:, k * 128:(k + 1) * 128], identity=ident[:],
                              reads=[r_hsb[st]], writes=[r_pT])
                    if st % 2 == 0:
                        fw.op('dve', 'tensor_copy', out=hT[:, :, c0_ + st * 128:c0_ + (st + 1) * 128], in_=pT[:],
                              reads=[r_pT], writes=[r_hTs[j % 2]])
                    else:
                        fw.op('act', 'activation', out=hT[:, :, c0_ + st * 128:c0_ + (st + 1) * 128], in_=pT[:], func=AF.Copy,
                              reads=[r_pT], writes=[r_hTs[j % 2]])

            prep_load(0)
            prep_tr(0)
            for j in range(NBLK):
                jb = j % 2
                c0 = jb * BS
                for r in range(NR):
                    if j + 1 < NBLK:
                        if r == max(NR - 2, 0):
                            prep_load(j + 1)
                        if r == NR - 1:
                            prep_tr(j + 1)
                    w = nw % NWB
                    nw += 1
                    ic = j * NR + r
                    io = IOA(ap=idxw[:, ic:ic + 1], axis=0)
                    fw.dma('pool', wg[w][:].rearrange("p k f -> p (k f)"), exp_w_gate[:, :], writes=[r_wgt[w]],
                           _meth='indirect_dma_start', out_offset=None, in_offset=io)
                    fw.dma('pool', wu[w][:].rearrange("p k f -> p (k f)"), exp_w_up[:, :], writes=[r_wgt[w]],
                           _meth='indirect_dma_start', out_offset=None, in_offset=io)
                    fw.dma('pool', wd[w][:].rearrange("p c d -> p (c d)"), exp_w_down[:, :], writes=[r_wdn[w]],
                           _meth='indirect_dma_start', out_offset=None, in_offset=io)
                    for tb in range(NTB):
                        ab = nb_ % 2
                        nb_ += 1
                        tokb = slice(c0 + tb * TBW, c0 + (tb + 1) * TBW)
                        for c in range(4):
                            pb = nc_ % 2
                            nc_ += 1
                            cs = slice(c * 128, (c + 1) * 128)
                            for k in range(KD):
                                fw.op('pe', 'matmul', out=psG[pb][:, 0:TBW], lhsT=wg[w][:, k, cs], rhs=hT[:, k, tokb], start=(k == 0), stop=(k == KD - 1),
                                      reads=[r_wgt[w], r_hTs[jb]], writes=[r_psG[pb]])
                            for k in range(KD):
                                fw.op('pe', 'matmul', out=psU[pb][:, 0:TBW], lhsT=wu[w][:, k, cs], rhs=hT[:, k, tokb], start=(k == 0), stop=(k == KD - 1),
                                      reads=[r_wgt[w], r_hTs[jb]], writes=[r_psU[pb]])
                            fw.op('act', 'activation', out=sl[pb][:, 0:TBW], in_=psG[pb][:, 0:TBW], func=AF.Silu, reads=[r_psG[pb]], writes=[r_sl[pb]])
                            fw.op('dve', 'tensor_tensor', out=act[ab][:, c, 0:TBW], in0=psU[pb][:, 0:TBW], in1=sl[pb][:, 0:TBW], op=ALU.mult,
                                  reads=[r_psU[pb], r_sl[pb]], writes=[r_act[ab]])
                        for tt in range(TPB):
                            st = tb * TPB + tt
                            for half in range(2):
                                yb = ny % 3
                                ny += 1
                                hs = slice(half * 512, (half + 1) * 512)
                                for c in range(4):
                                    fw.op('pe', 'matmul', out=psY[yb][:], lhsT=act[ab][:, c, tt * 128:(tt + 1) * 128], rhs=wd[w][:, c, hs],
                                          start=(c == 0), stop=(c == 3), reads=[r_act[ab], r_wdn[w]], writes=[r_psY[yb]])
                                dst = xs[:, jb * SB + st, hs]
                                if r == 0:
                                    fw.op('act', 'activation', out=dst, in_=psY[yb][:], func=AF.Copy, reads=[r_psY[yb]], writes=[r_ys[jb][st]])
                                else:
                                    fw.op('dve', 'tensor_tensor', out=dst, in0=psY[yb][:], in1=dst, op=ALU.add,
                                          reads=[r_psY[yb], r_ys[jb][st]], writes=[r_ys[jb][st]])
                fw.dma('sp', YS[j * BS:(j + 1) * BS, :].rearrange("(st p) d -> p st d", p=128), xs[:, jb * SB:(jb + 1) * SB, :],
                       reads=r_ys[jb], writes=[fw.res()])
            fw.end()

        def phase_final2(seq):
            fw.begin()
            gfb = fw.sb("gfb", [128, D], F32)
            r_g = fw.res()
            fw.dma('sp', gfb[:], norm_final_g[0:1, :].partition_broadcast(128), writes=[r_g])
            ss = fw.sb("ss", [128, NT], F32)
            junk = [fw.sb("junk%d" % i, [128, D], BF16) for i in range(2)]
            yo = [fw.sb("yo%d" % i, [128, D], F32) for i in range(2)]
            NG = 6
            xt = [fw.sb("xt%d" % i, [128, D], F32) for i in range(NG)]
            ya = [fw.sb("ya%d" % i, [128, D], F32) for i in range(NG)]
            yb_ = [fw.sb("yb%d" % i, [128, D], F32) for i in range(NG)]
            r_j = [fw.res() for _ in range(2)]
            r_y = [fw.res() for _ in range(2)]
            r_xt = [fw.res() for _ in range(NG)]
            r_ya = [fw.res() for _ in range(NG)]
            r_yb = [fw.res() for _ in range(NG)]
            r_o = fw.res()
            def loads(t):
                g_ = t % NG
                tt = seq * NT + t
                fw.dma(('sp', 'act')[t % 2], xt[g_][:], XS[seq, t * 128:(t + 1) * 128, :], writes=[r_xt[g_]])
                fw.dma('pool', ya[g_][:], YS[:, :], writes=[r_ya[g_]], _meth='indirect_dma_start', out_offset=None,
                       in_offset=IOA(ap=sAi[:, tt:tt + 1], axis=0))
                fw.dma('pool', yb_[g_][:], YS[:, :], writes=[r_yb[g_]], _meth='indirect_dma_start', out_offset=None,
                       in_offset=IOA(ap=sBi[:, tt:tt + 1], axis=0))

            for t in range(min(NG - 1, NT)):
                loads(t)
            for t in range(NT):
                if t + NG - 1 < NT:
                    loads(t + NG - 1)
                b = t % 2
                g_ = t % NG
                tt = seq * NT + t
                r_s = fw.res()
                fw.op('dve', 'scalar_tensor_tensor', out=xt[g_][:], in0=ya[g_][:], scalar=gA[:, tt:tt + 1], in1=xt[g_][:],
                      op0=ALU.mult, op1=ALU.add, reads=[r_ya[g_], r_xt[g_]], writes=[r_xt[g_]])
                fw.op('dve', 'scalar_tensor_tensor', out=xt[g_][:], in0=yb_[g_][:], scalar=gB[:, tt:tt + 1], in1=xt[g_][:],
                      op0=ALU.mult, op1=ALU.add, reads=[r_yb[g_], r_xt[g_]], writes=[r_xt[g_]])
                fw.op('act', 'activation', out=junk[b][:], in_=xt[g_][:], func=AF.Square, accum_out=ss[:, t:t + 1],
                      reads=[r_xt[g_]], writes=[r_j[b], r_s])
                fw.op('act', 'activation', out=ss[:, t:t + 1], in_=ss[:, t:t + 1], func=AF.Ln, scale=1.0 / D, bias=epsc[:, 0:1],
                      reads=[r_s], writes=[r_s])
                fw.op('act', 'activation', out=ss[:, t:t + 1], in_=ss[:, t:t + 1], func=AF.Exp, scale=-0.5, reads=[r_s], writes=[r_s])
                fw.op('act', 'activation', out=yo[b][:], in_=xt[g_][:], func=AF.Copy, scale=ss[:, t:t + 1], reads=[r_s, r_xt[g_]], writes=[r_y[b]])
                fw.op('dve', 'tensor_tensor', out=yo[b][:], in0=yo[b][:], in1=gfb[:], op=ALU.mult, reads=[r_y[b], r_g], writes=[r_y[b]])
                fw.dma('sp', out_d[seq, t * 128:(t + 1) * 128, :], yo[b][:], reads=[r_y[b]], writes=[r_o])
            fw.end()

        def phase_load_x(seq):
            fw.begin()
            for t in range(NT):
                fw.dma('sp', xs[:, t, :], x_d[seq, t * 128:(t + 1) * 128, :], writes=[fw.res()])
            fw.end()

        def phase_final(seq):
            fw.begin()
            gfb = fw.sb("gfb", [128, D], F32)
            r_g = fw.res()
            fw.dma('sp', gfb[:], norm_final_g[0:1, :].partition_broadcast(128), writes=[r_g])
            ss = fw.sb("ss", [128, NT], F32)
            junk = [fw.sb("junk%d" % i, [128, D], BF16) for i in range(2)]
            yo = [fw.sb("yo%d" % i, [128, D], F32) for i in range(2)]
            r_j = [fw.res() for _ in range(2)]
            r_y = [fw.res() for _ in range(2)]
            r_o = fw.res()
            for t in range(NT):
                b = t % 2
                r_s = fw.res()
                fw.op('act', 'activation', out=junk[b][:], in_=xs[:, t, :], func=AF.Square, accum_out=ss[:, t:t + 1], writes=[r_j[b], r_s])
                fw.op('act', 'activation', out=ss[:, t:t + 1], in_=ss[:, t:t + 1], func=AF.Ln, scale=1.0 / D, bias=epsc[:, 0:1],
                      reads=[r_s], writes=[r_s])
                fw.op('act', 'activation', out=ss[:, t:t + 1], in_=ss[:, t:t + 1], func=AF.Exp, scale=-0.5, reads=[r_s], writes=[r_s])
                fw.op('act', 'activation', out=yo[b][:], in_=xs[:, t, :], func=AF.Copy, scale=ss[:, t:t + 1], reads=[r_s], writes=[r_y[b]])
                fw.op('dve', 'tensor_tensor', out=yo[b][:], in0=yo[b][:], in1=gfb[:], op=ALU.mult, reads=[r_y[b], r_g], writes=[r_y[b]])
                fw.dma('sp', out_d[seq, t * 128:(t + 1) * 128, :], yo[b][:], reads=[r_y[b]], writes=[r_o])
            fw.end()

        for seq in range(NSEQ):
            r_xs = [Res() for _ in range(NT)]
            phase_load_x(seq)
            phase_rope_tables(seq)
            for l in range(DEPTH):
                for r_ in r_xs:
                    r_.w = None
                    r_.rd = []
                phase_norm(norm_mix_g[l])
                phase_gmlp(l)
                phase_conv(l)
                phase_swa(l, 0)
                phase_swa(l, 1)
                phase_fox_cum(l)
                phase_fox(l, 0)
                phase_fox(l, 1)
                phase_gates(l)
                for r_ in r_xs:
                    r_.w = None
                    r_.rd = []
                phase_merge(l)
                for r_ in r_xs:
                    r_.w = None
                    r_.rd = []
                j = l // 2
                if l % 2 == 0:
                    phase_norm(norm_ffn_g[l])
                    for r_ in r_xs:
                        r_.w = None
                        r_.rd = []
                    phase_ffn(ffn_w_gate[j:j + 1], ffn_w_up[j:j + 1], ffn_w_down[j:j + 1], 1, False)
                elif sparse:
                    phase_norm(norm_ffn_g[l], moe_router=router_w[j], seq=seq, g_row2=norm_ffn_g[l:l + 1, :])
                    phase_stash(seq)
                else:
                    phase_norm(norm_ffn_g[l], moe_router=router_w[j])
                    for r_ in r_xs:
                        r_.w = None
                        r_.rd = []
                    phase_ffn(exp_w_gate[j], exp_w_up[j], exp_w_down[j], NE, True)
            for r_ in r_xs:
                r_.w = None
                r_.rd = []
            if not sparse:
                phase_final(seq)
        if sparse:
            LV = 9
            if LV >= 1:
                phase_route()
            if LV >= 2:
                phase_scatter()
            if LV >= 3:
                phase_moe_sparse()
            if LV >= 4:
                for seq in range(NSEQ):
                    phase_final2(seq)
        print("built: n_inst", fw.n_inst, "n_wait", fw.n_wait)
    return nc


_NC_CACHE = {}

_PARAMS = ['norm_mix_g', 'w_in', 'gmlp_ln_g', 'gmlp_ln_b', 'gmlp_ws', 'gmlp_bs', 'swa_sink', 'fox_bf', 'conv_w', 'conv_b',
           'conv_ln_g', 'conv_ln_b', 'w_branch', 'w_out', 'norm_ffn_g', 'ffn_w_gate', 'ffn_w_up', 'ffn_w_down', 'router_w',
           'exp_w_gate', 'exp_w_up', 'exp_w_down']


def relayout_experts(wg, wu, wd):
    NE, D_, DFF = wg.shape
    NR = DFF // 512

    def gu(w):
        a = np.asarray(w, dtype=np.float32).reshape(NE, 8, 128, NR, 512).transpose(0, 2, 3, 1, 4)
        return np.ascontiguousarray(a).reshape(NE * 128 * NR, 4096)

    b = np.asarray(wd, dtype=np.float32).reshape(NE, NR, 4, 128, D_).transpose(0, 3, 1, 2, 4)
    return gu(wg), gu(wu), np.ascontiguousarray(b).reshape(NE * 128 * NR, 4096)


def kernel(**inputs):
    n = 8
    x = np.ascontiguousarray(np.asarray(inputs['x'], dtype=np.float32))
    pos = np.ascontiguousarray(np.asarray(inputs['positions'], dtype=np.int32))
    B, S, _ = x.shape
    per = B // n
    if 'nc' not in _NC_CACHE:
        _NC_CACHE['nc'] = build(NSEQ=per, S=S)
    nc = _NC_CACHE['nc']
    shared = {k: np.ascontiguousarray(np.asarray(inputs[k], dtype=np.float32)) for k in _PARAMS}
    shared['norm_final_g'] = np.ascontiguousarray(np.asarray(inputs['norm_final_g'], dtype=np.float32)).reshape(1, -1)
    shared['exp_w_gate'], shared['exp_w_up'], shared['exp_w_down'] = relayout_experts(
        shared['exp_w_gate'][0], shared['exp_w_up'][0], shared['exp_w_down'][0])
    in_maps = []
    for c in range(n):
        m = dict(shared)
        m['x'] = x[c * per:(c + 1) * per]
        m['positions'] = pos[c * per:(c + 1) * per]
        in_maps.append(m)
    res = run_bass_kernel_spmd(nc, in_maps, core_ids=list(range(n)))
    return np.concatenate([r['out'] for r in res.results], axis=0).astype(np.float32)
```
